# Optimizing a Trainium2 kernel written in Bass

```python
import math
import jax
import jax.numpy as jnp
from jax import lax
import numpy as np

D_MODEL = 1024
BATCH = 16
SEQ = 4096
DEPTH = 2

N_HEADS = 4
HEAD_DIM = 64
MIX_W = N_HEADS * HEAD_DIM
N_BRANCH = 4
Q_BLOCK = 128
GDN_CHUNK = 64
CONV_K = 4
RET_CHUNK = 128
ROPE_BASE = 10000.0
NSA_CMP_LEN = 32
NSA_CMP_STRIDE = 16
NSA_CMP_HIDDEN = 256
NSA_SEL_LEN = 64
NSA_TOP_N = 16
NSA_WINDOW = 512
NSA_Q_BLOCK = 64
NSA_FORCE_SCORE = 1.0e4
N_EXPERTS = 16
N_GROUPS = 4
EXPERTS_PER_GROUP = N_EXPERTS // N_GROUPS
TOP_K = 2
D_EXPERT = 512
DN_ALPHA = (2.0 * DEPTH) ** 0.25
DN_BETA = (8.0 * DEPTH) ** -0.25
LN_EPS = 1e-5

IN_WIDTHS = (
    MIX_W, MIX_W, MIX_W, N_HEADS, N_HEADS, MIX_W,
    MIX_W, MIX_W, MIX_W, MIX_W,
    MIX_W, HEAD_DIM, HEAD_DIM, HEAD_DIM, HEAD_DIM, HEAD_DIM, HEAD_DIM, 3 * N_HEADS,
    MIX_W, MIX_W, MIX_W, N_HEADS,
)
IN_WIDTH = int(sum(IN_WIDTHS))
IN_SPLITS = tuple(int(v) for v in np.cumsum(IN_WIDTHS)[:-1])

kernel_name = 'hybrid_gdn_ret_nsa_fox_moe_block'


def layer_norm(x, g, b):
    xf = x.astype(jnp.float32)
    mu = xf.mean(-1, keepdims=True)
    var = jnp.square(xf - mu).mean(-1, keepdims=True)
    return ((xf - mu) * lax.rsqrt(var + LN_EPS)).astype(x.dtype) * g + b


def rms_normalize(x):
    xf = x.astype(jnp.float32)
    return xf * lax.rsqrt(jnp.mean(jnp.square(xf), -1, keepdims=True) + 1e-6)


def l2_normalize(x):
    xf = x.astype(jnp.float32)
    return (xf * lax.rsqrt(jnp.sum(jnp.square(xf), -1, keepdims=True) + 1e-6)).astype(x.dtype)


def split_heads(x):
    b, s, _ = x.shape
    return x.reshape(b, s, N_HEADS, HEAD_DIM).transpose(0, 2, 1, 3)


def merge_heads(x):
    b, h, s, d = x.shape
    return x.transpose(0, 2, 1, 3).reshape(b, s, h * d)


def to_chunks(t, size):
    return t.reshape(t.shape[:2] + (t.shape[2] // size, size) + t.shape[3:])


def masked_softmax(s, mask):
    s = jnp.where(mask, s.astype(jnp.float32), -jnp.inf)
    m = jnp.max(s, -1, keepdims=True)
    m = jnp.where(jnp.isfinite(m), m, 0.0)
    e = jnp.exp(s - m)
    den = e.sum(-1, keepdims=True)
    return e / jnp.where(den > 0, den, 1.0)


def causal_depthwise_conv(x, w):
    k, ch = w.shape
    return lax.conv_general_dilated(x, w[:, None, :].astype(x.dtype), window_strides=(1,), padding=[(k - 1, 0)],
                                    dimension_numbers=('NWC', 'WIO', 'NWC'), feature_group_count=ch)


def rope(x, positions):
    half = HEAD_DIM // 2
    inv_freq = ROPE_BASE ** (-jnp.arange(half, dtype=jnp.float32) / half)
    ang = positions.astype(jnp.float32)[:, None, :, None] * inv_freq
    cos, sin = jnp.cos(ang), jnp.sin(ang)
    x1, x2 = x[..., :half], x[..., half:]
    return jnp.concatenate([x1 * cos - x2 * sin, x1 * sin + x2 * cos], -1).astype(x.dtype)


def ada_modulation(c, w, b):
    mod = jax.nn.silu(c) @ w + b
    shift, scale, gate = jnp.split(mod[:, None, :], 3, axis=-1)
    return shift, scale, gate


def gated_deltanet(q, k, v, a, b, z, conv_w, a_log, dt_bias, norm_w):
    bsz, s, _ = q.shape
    qkv = jax.nn.silu(causal_depthwise_conv(jnp.concatenate([q, k, v], -1), conv_w))
    q, k, v = jnp.split(qkv, 3, axis=-1)
    q = l2_normalize(split_heads(q)) * HEAD_DIM ** -0.5
    k = l2_normalize(split_heads(k))
    v = split_heads(v)
    beta = jax.nn.sigmoid(b.astype(jnp.float32)).transpose(0, 2, 1)
    log_a = (-jnp.exp(a_log.astype(jnp.float32)) * jax.nn.softplus(a.astype(jnp.float32) + dt_bias)).transpose(0, 2, 1)
    C = GDN_CHUNK
    q, k, v, beta, log_a = (to_chunks(t, C) for t in (q, k, v, beta, log_a))
    bcum = jnp.cumsum(log_a, -1)
    incl = jnp.tril(jnp.ones((C, C), bool))
    strict = jnp.tril(jnp.ones((C, C), bool), -1)
    decay_incl = jnp.exp(jnp.where(incl, bcum[..., :, None] - bcum[..., None, :], -jnp.inf))
    decay_strict = jnp.where(strict, decay_incl, 0.0)
    lower = beta[..., :, None] * jnp.einsum('bhnid,bhnjd->bhnij', k, k) * decay_strict
    rhs = jnp.concatenate([beta[..., None] * v, (beta * jnp.exp(bcum))[..., None] * k], -1)
    sol = lax.linalg.triangular_solve(lower, rhs, left_side=True, lower=True, unit_diagonal=True)
    u0, w = jnp.split(sol, 2, axis=-1)
    attn = jnp.einsum('bhnid,bhnjd->bhnij', q, k) * decay_incl
    q_dec = q * jnp.exp(bcum)[..., None]
    k_dec = k * jnp.exp(bcum[..., -1:] - bcum)[..., None]
    chunk_decay = jnp.exp(bcum[..., -1])

    def step(state, xs):
        u0_n, w_n, attn_n, q_n, k_n, g_n = xs
        u = u0_n - jnp.einsum('bhck,bhkv->bhcv', w_n, state)
        o = jnp.einsum('bhck,bhkv->bhcv', q_n, state) + jnp.einsum('bhcs,bhsv->bhcv', attn_n, u)
        state = g_n[..., None, None] * state + jnp.einsum('bhck,bhcv->bhkv', k_n, u)
        return state, o

    xs = tuple(jnp.moveaxis(t, 2, 0) for t in (u0, w, attn, q_dec, k_dec, chunk_decay))
    state0 = jnp.zeros((bsz, N_HEADS, HEAD_DIM, HEAD_DIM), jnp.float32)
    _, o = lax.scan(step, state0, xs)
    o = jnp.moveaxis(o, 0, 2).reshape(bsz, N_HEADS, s, HEAD_DIM)
    o = merge_heads(rms_normalize(o) * norm_w) * jax.nn.silu(z)
    return o.astype(z.dtype)


def retention(q, k, v, g, positions, gn_w):
    bsz, s, _ = q.shape
    q = rope(split_heads(q), positions)
    k = rope(split_heads(k), positions) * HEAD_DIM ** -0.5
    v = split_heads(v)
    C = RET_CHUNK
    log_gamma = jnp.log1p(-(2.0 ** (-5.0 - jnp.arange(N_HEADS, dtype=jnp.float32))))
    idx = jnp.arange(C, dtype=jnp.float32)
    rel = idx[:, None] - idx[None, :]
    inner_decay = jnp.where(rel >= 0, jnp.exp(jnp.maximum(rel, 0.0) * log_gamma[:, None, None]), 0.0)
    xi = jnp.exp((idx + 1.0) * log_gamma[:, None])
    zeta = jnp.exp((C - 1.0 - idx) * log_gamma[:, None])
    chunk_decay = jnp.exp(C * log_gamma)
    qc, kc, vc = (to_chunks(t, C) for t in (q, k, v))
    scores = jnp.einsum('bhnid,bhnjd->bhnij', qc, kc) * inner_decay[:, None]
    inner = jnp.einsum('bhnij,bhnjd->bhnid', scores, vc)
    kv = jnp.einsum('bhnck,bhncv->bhnkv', kc * zeta[:, None, :, None], vc)

    def step(state, kv_n):
        return chunk_decay[None, :, None, None] * state + kv_n, state

    state0 = jnp.zeros((bsz, N_HEADS, HEAD_DIM, HEAD_DIM), jnp.float32)
    _, r_prev = lax.scan(step, state0, jnp.moveaxis(kv, 2, 0))
    r_prev = jnp.moveaxis(r_prev, 0, 2)
    cross = jnp.einsum('bhnck,bhnkv->bhncv', qc, r_prev) * xi[:, None, :, None]
    o = (inner + cross).reshape(bsz, N_HEADS, s, HEAD_DIM).astype(jnp.float32)
    mu = o.mean(-1, keepdims=True)
    var = jnp.square(o - mu).mean(-1, keepdims=True)
    o = merge_heads((o - mu) * lax.rsqrt(var + LN_EPS)) * gn_w
    return (jax.nn.silu(g) * o).astype(g.dtype)


def native_sparse_attention(q, k_cmp_in, v_cmp_in, k_sel_in, v_sel_in, k_win_in, v_win_in, gate_logits,
                            cmp_pe, ck_w1, ck_w2, cv_w1, cv_w2):
    bsz, s, _ = q.shape
    q = split_heads(q)
    scale = HEAD_DIM ** -0.5
    n_cmp = (s - NSA_CMP_LEN) // NSA_CMP_STRIDE + 1
    cmp_start = jnp.arange(n_cmp) * NSA_CMP_STRIDE
    cmp_tok = cmp_start[:, None] + jnp.arange(NSA_CMP_LEN)[None, :]

    def compress(t, w1, w2):
        blocks = (t[:, cmp_tok] + cmp_pe).reshape(bsz, n_cmp, NSA_CMP_LEN * HEAD_DIM)
        return jax.nn.silu(blocks @ w1) @ w2

    k_cmp = compress(k_cmp_in, ck_w1, ck_w2)
    v_cmp = compress(v_cmp_in, cv_w1, cv_w2)
    cmp_end = cmp_start + NSA_CMP_LEN - 1
    n_sel = s // NSA_SEL_LEN
    sel_start = jnp.arange(n_sel) * NSA_SEL_LEN
    overlap = jnp.clip(jnp.minimum(cmp_start[:, None] + NSA_CMP_LEN, sel_start[None, :] + NSA_SEL_LEN)
                       - jnp.maximum(cmp_start[:, None], sel_start[None, :]), 0, NSA_CMP_LEN).astype(jnp.float32) / NSA_CMP_LEN
    n_top = min(NSA_TOP_N, n_sel)
    k_blk = k_sel_in.reshape(bsz, n_sel, NSA_SEL_LEN, HEAD_DIM)
    v_blk = v_sel_in.reshape(bsz, n_sel, NSA_SEL_LEN, HEAD_DIM)
    k_win = jnp.pad(k_win_in, ((0, 0), (NSA_WINDOW, 0), (0, 0)))
    v_win = jnp.pad(v_win_in, ((0, 0), (NSA_WINDOW, 0), (0, 0)))
    gates = jax.nn.sigmoid(gate_logits.astype(jnp.float32)).reshape(bsz, s, N_HEADS, 3)
    blk_ids = jnp.arange(n_sel)
    blk_off = jnp.arange(NSA_SEL_LEN)
    win_off = jnp.arange(NSA_WINDOW + NSA_Q_BLOCK)
    gather = jax.vmap(lambda blocks, idx: blocks[idx])

    def block(i):
        t0 = i * NSA_Q_BLOCK
        tpos = t0 + jnp.arange(NSA_Q_BLOCK)
        qb = lax.dynamic_slice_in_dim(q, t0, NSA_Q_BLOCK, axis=2)
        p_cmp = masked_softmax(jnp.einsum('bhqd,bnd->bhqn', qb, k_cmp) * scale, cmp_end[None, :] <= tpos[:, None])
        o_cmp = jnp.einsum('bhqn,bnd->bhqd', p_cmp, v_cmp)
        importance = jnp.einsum('bhqn,nj->bqj', p_cmp, overlap)
        cur = tpos // NSA_SEL_LEN
        forced = (blk_ids[None, :] == 0) | (blk_ids[None, :] == cur[:, None]) | (blk_ids[None, :] == cur[:, None] - 1)
        causal_blk = sel_start[None, :] <= tpos[:, None]
        score = jnp.where(forced, NSA_FORCE_SCORE, jnp.where(causal_blk, importance, -1.0))
        _, sel = lax.top_k(score, n_top)
        k_g = gather(k_blk, sel).reshape(bsz, NSA_Q_BLOCK, n_top * NSA_SEL_LEN, HEAD_DIM)
        v_g = gather(v_blk, sel).reshape(bsz, NSA_Q_BLOCK, n_top * NSA_SEL_LEN, HEAD_DIM)
        kpos = (sel[..., None] * NSA_SEL_LEN + blk_off).reshape(bsz, NSA_Q_BLOCK, n_top * NSA_SEL_LEN)
        p_sel = masked_softmax(jnp.einsum('bhqd,bqkd->bhqk', qb, k_g) * scale, (kpos <= tpos[None, :, None])[:, None])
        o_sel = jnp.einsum('bhqk,bqkd->bhqd', p_sel, v_g)
        kw = lax.dynamic_slice_in_dim(k_win, t0, NSA_WINDOW + NSA_Q_BLOCK, axis=1)
        vw = lax.dynamic_slice_in_dim(v_win, t0, NSA_WINDOW + NSA_Q_BLOCK, axis=1)
        wpos = t0 - NSA_WINDOW + win_off
        wmask = (wpos[None, :] >= 0) & (wpos[None, :] <= tpos[:, None]) & (wpos[None, :] > tpos[:, None] - NSA_WINDOW)
        p_win = masked_softmax(jnp.einsum('bhqd,bkd->bhqk', qb, kw) * scale, wmask)
        o_win = jnp.einsum('bhqk,bkd->bhqd', p_win, vw)
        g = lax.dynamic_slice_in_dim(gates, t0, NSA_Q_BLOCK, axis=1).transpose(0, 2, 1, 3)
        return g[..., 0:1] * o_cmp + g[..., 1:2] * o_sel + g[..., 2:3] * o_win

    o = lax.map(block, jnp.arange(s // NSA_Q_BLOCK))
    return o.transpose(1, 0, 3, 2, 4).reshape(bsz, s, MIX_W).astype(k_cmp_in.dtype)


def forgetting_attention(q, k, v, f_logit, f_bias):
    bsz, s, _ = q.shape
    q, k, v = split_heads(q), split_heads(k), split_heads(v)
    scale = HEAD_DIM ** -0.5
    log_f = jax.nn.log_sigmoid(f_logit.astype(jnp.float32) + f_bias).transpose(0, 2, 1)
    cum = jnp.cumsum(log_f, -1)
    kpos = jnp.arange(s)

    def block(i):
        t0 = i * Q_BLOCK
        tpos = t0 + jnp.arange(Q_BLOCK)
        qb = lax.dynamic_slice_in_dim(q, t0, Q_BLOCK, axis=2)
        cb = lax.dynamic_slice_in_dim(cum, t0, Q_BLOCK, axis=2)
        logits = jnp.einsum('bhqd,bhkd->bhqk', qb, k) * scale + (cb[..., :, None] - cum[..., None, :])
        p = masked_softmax(logits, kpos[None, :] <= tpos[:, None])
        return jnp.einsum('bhqk,bhkd->bhqd', p, v)

    o = lax.map(block, jnp.arange(s // Q_BLOCK))
    return o.transpose(1, 0, 3, 2, 4).reshape(bsz, s, MIX_W).astype(v.dtype)


def hybrid_mixer(h, positions, w_in, gdn_conv_w, gdn_a_log, gdn_dt_bias, gdn_norm_w, ret_gn_w,
                 nsa_cmp_pe, nsa_ck_w1, nsa_ck_w2, nsa_cv_w1, nsa_cv_w2, fox_f_bias, branch_proj, w_gate, w_out):
    proj = jnp.einsum('bsd,de->bse', h, w_in)
    (gq, gk, gv, ga, gb, gz, rq, rk, rv, rg, nq, nkc, nvc, nks, nvs, nkw, nvw, ngate,
     fq, fk, fv, ff) = jnp.split(proj, IN_SPLITS, axis=-1)
    o_gdn = gated_deltanet(gq, gk, gv, ga, gb, gz, gdn_conv_w, gdn_a_log, gdn_dt_bias, gdn_norm_w)
    o_ret = retention(rq, rk, rv, rg, positions, ret_gn_w)
    o_nsa = native_sparse_attention(nq, nkc, nvc, nks, nvs, nkw, nvw, ngate, nsa_cmp_pe,
                                    nsa_ck_w1, nsa_ck_w2, nsa_cv_w1, nsa_cv_w2)
    o_fox = forgetting_attention(fq, fk, fv, ff, fox_f_bias)
    merged = [jax.nn.sigmoid(h @ w_gate[i]) * (o.astype(h.dtype) @ branch_proj[i])
              for i, o in enumerate((o_gdn, o_ret, o_nsa, o_fox))]
    y = merged[0] + merged[1] + merged[2] + merged[3]
    return y @ w_out


def moe(h, router_w, router_b, w1, w3, w2):
    bsz, s, _ = h.shape
    scores = jax.nn.sigmoid(jnp.einsum('bsd,de->bse', h, router_w).astype(jnp.float32))
    biased = scores + router_b.astype(jnp.float32)
    group_score = lax.top_k(biased.reshape(bsz, s, N_GROUPS, EXPERTS_PER_GROUP), TOP_K)[0].sum(-1)
    group = jnp.argmax(group_score, axis=-1)
    in_group = (jnp.arange(N_EXPERTS) // EXPERTS_PER_GROUP) == group[..., None]
    _, top_idx = lax.top_k(jnp.where(in_group, biased, -jnp.inf), TOP_K)
    top_w = jnp.take_along_axis(scores, top_idx, axis=-1)
    top_w = top_w / top_w.sum(-1, keepdims=True)
    combine = jnp.sum(jax.nn.one_hot(top_idx, N_EXPERTS, dtype=jnp.float32) * top_w[..., None], axis=-2).astype(h.dtype)

    def per_sequence(args):
        hs, cs = args
        hidden = jax.nn.silu(jnp.einsum('sd,edf->sef', hs, w1)) * jnp.einsum('sd,edf->sef', hs, w3)
        return jnp.einsum('sef,efd->sd', hidden * cs[..., None], w2)

    return lax.map(per_sequence, (h, combine))


def setup_inputs(seed: int = 0) -> dict:
    key = jax.random.key(seed)
    ks = jax.random.split(key, 32)
    f32 = jnp.float32

    def nrm(k, shape, scale):
        return jax.random.normal(k, shape, f32) * scale

    dt = jnp.exp(jax.random.uniform(ks[7], (DEPTH, N_HEADS), f32, math.log(1e-3), math.log(1e-1)))
    return {
        'x': nrm(ks[0], (BATCH, SEQ, D_MODEL), 1.0),
        'c': nrm(ks[1], (BATCH, D_MODEL), 1.0),
        'positions': jnp.tile(jnp.arange(SEQ, dtype=jnp.int32)[None, :], (BATCH, 1)),
        'ada_w': nrm(ks[2], (DEPTH, 2, D_MODEL, 3 * D_MODEL), 0.5 * D_MODEL ** -0.5),
        'ada_b': nrm(ks[3], (DEPTH, 2, 3 * D_MODEL), 0.02),
        'w_in': nrm(ks[4], (DEPTH, D_MODEL, IN_WIDTH), D_MODEL ** -0.5),
        'gdn_conv_w': nrm(ks[5], (DEPTH, CONV_K, 3 * MIX_W), CONV_K ** -0.5),
        'gdn_a_log': jnp.log(jax.random.uniform(ks[6], (DEPTH, N_HEADS), f32, 1.0, 16.0)),
        'gdn_dt_bias': dt + jnp.log(-jnp.expm1(-dt)),
        'gdn_norm_w': 1.0 + nrm(ks[8], (DEPTH, HEAD_DIM), 0.02),
        'ret_gn_w': 1.0 + nrm(ks[9], (DEPTH, MIX_W), 0.02),
        'nsa_cmp_pe': nrm(ks[10], (DEPTH, NSA_CMP_LEN, HEAD_DIM), 0.02),
        'nsa_ck_w1': nrm(ks[11], (DEPTH, NSA_CMP_LEN * HEAD_DIM, NSA_CMP_HIDDEN), (NSA_CMP_LEN * HEAD_DIM) ** -0.5),
        'nsa_ck_w2': nrm(ks[12], (DEPTH, NSA_CMP_HIDDEN, HEAD_DIM), NSA_CMP_HIDDEN ** -0.5),
        'nsa_cv_w1': nrm(ks[13], (DEPTH, NSA_CMP_LEN * HEAD_DIM, NSA_CMP_HIDDEN), (NSA_CMP_LEN * HEAD_DIM) ** -0.5),
        'nsa_cv_w2': nrm(ks[14], (DEPTH, NSA_CMP_HIDDEN, HEAD_DIM), NSA_CMP_HIDDEN ** -0.5),
        'fox_f_bias': jnp.linspace(3.0, 6.0, N_HEADS, dtype=f32)[None, :] + nrm(ks[15], (DEPTH, N_HEADS), 0.1),
        'branch_proj': nrm(ks[16], (DEPTH, N_BRANCH, MIX_W, D_MODEL), DN_BETA * MIX_W ** -0.5),
        'w_gate': nrm(ks[17], (DEPTH, N_BRANCH, D_MODEL, D_MODEL), D_MODEL ** -0.5),
        'w_out': nrm(ks[18], (DEPTH, D_MODEL, D_MODEL), DN_BETA * D_MODEL ** -0.5),
        'ln_g': 1.0 + nrm(ks[19], (DEPTH, 2, D_MODEL), 0.02),
        'ln_b': nrm(ks[20], (DEPTH, 2, D_MODEL), 0.02),
        'router_w': nrm(ks[21], (D_MODEL, N_EXPERTS), D_MODEL ** -0.5),
        'router_b': nrm(ks[22], (N_EXPERTS,), 0.01),
        'exp_w1': nrm(ks[23], (DEPTH, N_EXPERTS, D_MODEL, D_EXPERT), D_MODEL ** -0.5),
        'exp_w3': nrm(ks[24], (DEPTH, N_EXPERTS, D_MODEL, D_EXPERT), D_MODEL ** -0.5),
        'exp_w2': nrm(ks[25], (DEPTH, N_EXPERTS, D_EXPERT, D_MODEL), DN_BETA * D_EXPERT ** -0.5),
    }


def reference(x, c, positions, ada_w, ada_b, w_in, gdn_conv_w, gdn_a_log, gdn_dt_bias, gdn_norm_w,
              ret_gn_w, nsa_cmp_pe, nsa_ck_w1, nsa_ck_w2, nsa_cv_w1, nsa_cv_w2, fox_f_bias,
              branch_proj, w_gate, w_out, ln_g, ln_b, router_w, router_b, exp_w1, exp_w3, exp_w2):
    for l in range(DEPTH):
        shift, scale, gate = ada_modulation(c, ada_w[l, 0], ada_b[l, 0])
        h = x * (1.0 + scale) + shift
        y = hybrid_mixer(h, positions, w_in[l], gdn_conv_w[l], gdn_a_log[l], gdn_dt_bias[l], gdn_norm_w[l],
                         ret_gn_w[l], nsa_cmp_pe[l], nsa_ck_w1[l], nsa_ck_w2[l], nsa_cv_w1[l], nsa_cv_w2[l],
                         fox_f_bias[l], branch_proj[l], w_gate[l], w_out[l])
        x = layer_norm(DN_ALPHA * x + gate * y, ln_g[l, 0], ln_b[l, 0])
        shift, scale, gate = ada_modulation(c, ada_w[l, 1], ada_b[l, 1])
        h = x * (1.0 + scale) + shift
        y = moe(h, router_w, router_b, exp_w1[l], exp_w3[l], exp_w2[l])
        x = layer_norm(DN_ALPHA * x + gate * y, ln_g[l, 1], ln_b[l, 1])
    return x
```

```python
import math
from contextlib import ExitStack
import numpy as np
import concourse.bass as bass
import concourse.mybir as mybir
from concourse.bass_utils import run_bass_kernel_spmd

F32 = mybir.dt.float32
BF16 = mybir.dt.bfloat16
I32 = mybir.dt.int32
ALU = mybir.AluOpType
AF = mybir.ActivationFunctionType
AX = mybir.AxisListType

D = 1024
T = 4096
DEPTH = 2
NE = 16
DE = 512
ALPHA = (2.0 * DEPTH) ** 0.25
LN_EPS = 1e-5
NEG = -30000.0


class K:
    NSLOT = 8

    def __init__(self, nc):
        self.nc = nc
        self.es = ExitStack()
        self.eng = {"pe": nc.tensor, "act": nc.scalar, "dve": nc.vector, "pool": nc.gpsimd, "sp": nc.sync}
        self.sem = {}
        self.cnt = {}
        for e in self.eng:
            self.sem[e] = self.es.enter_context(nc.semaphore("sem_" + e))
            self.cnt[e] = 0
        self.slots = {}
        self.slot_idx = {}
        for q in ("sp", "pool", "act"):
            self.slots[q] = [self.es.enter_context(nc.semaphore("dq_%s_%d" % (q, i))) for i in range(self.NSLOT)]
            self.slot_idx[q] = 0
        self.slot_val = {}
        self.seen = {e: {} for e in self.eng}
        self.lastw = {}
        self.readers = {}
        self.n_ins = 0

    @staticmethod
    def key(ap):
        if isinstance(ap, tuple):
            return ap[1]
        if ap is None or isinstance(ap, (int, float)):
            return None
        t = ap.tensor
        if str(t.space).lower().find("dram") >= 0 or type(t).__name__.startswith("DRam"):
            return None
        return t.name

    @staticmethod
    def raw(ap):
        return ap[0] if isinstance(ap, tuple) else ap

    def _need(self, eng, reads, writes):
        need = {}

        def add(dep):
            semname, sem, val, owner = dep
            if owner == eng and eng == "pe":
                return
            if need.get(semname, (None, -1))[1] < val:
                need[semname] = (sem, val)

        for r in reads:
            kk = self.key(r)
            if kk is None:
                continue
            if kk in self.lastw:
                add(self.lastw[kk])
        for w in writes:
            kk = self.key(w)
            if kk is None:
                continue
            if kk in self.lastw:
                add(self.lastw[kk])
            for dep in self.readers.get(kk, {}).values():
                if dep[3] == eng and dep[0].startswith("sem_"):
                    continue
                add(dep)
        return need

    def _emit_waits(self, eng, need):
        e = self.eng[eng]
        seen = self.seen[eng]
        for semname, (sem, val) in need.items():
            if seen.get(semname, -1) >= val:
                continue
            e.wait_ge(sem, val)
            seen[semname] = val
            self.n_ins += 1

    def _record(self, dep, reads, writes):
        for r in reads:
            kk = self.key(r)
            if kk is None:
                continue
            self.readers.setdefault(kk, {})[dep[0]] = dep
        for w in writes:
            kk = self.key(w)
            if kk is None:
                continue
            self.lastw[kk] = dep
            self.readers[kk] = {}

    def op(self, eng, fn, reads, writes):
        need = self._need(eng, reads, writes)
        self._emit_waits(eng, need)
        ins = fn(self.eng[eng])
        self.cnt[eng] += 1
        ins.then_inc(self.sem[eng], 1)
        self.n_ins += 1
        dep = ("sem_" + eng, self.sem[eng], self.cnt[eng], eng)
        self._record(dep, reads, writes)
        return ins

    def dma(self, out, in_, q="sp", **kw):
        reads, writes = [in_], [out]
        need = self._need(q, reads, writes)
        i = self.slot_idx[q] % self.NSLOT
        self.slot_idx[q] += 1
        semname = "dq_%s_%d" % (q, i)
        sem = self.slots[q][i]
        prev = self.slot_val.get((q, i), 0)
        if prev > 0:
            need[semname] = (sem, prev)
        self._emit_waits(q, need)
        ins = self.eng[q].dma_start(out=self.raw(out), in_=self.raw(in_), **kw)
        val = prev + 16
        ins.then_inc(sem, 16)
        self.slot_val[(q, i)] = val
        self.n_ins += 1
        dep = (semname, sem, val, "dma_" + q)
        self._record(dep, reads, writes)
        return ins

    def barrier(self):
        need = {}
        for e in self.eng:
            if self.cnt[e] > 0:
                need["sem_" + e] = (self.sem[e], self.cnt[e])
        for (q, i), v in self.slot_val.items():
            need["dq_%s_%d" % (q, i)] = (self.slots[q][i], v)
        for e in self.eng:
            nd = {k: v for k, v in need.items() if k != "sem_" + e}
            self._emit_waits(e, nd)
        self.lastw = {}
        self.readers = {}

    def mm(self, out, lhsT, rhs, start=True, stop=True):
        return self.op("pe", lambda e: e.matmul(self.raw(out), self.raw(lhsT), self.raw(rhs), start=start, stop=stop),
                       [lhsT, rhs], [out])

    def tr(self, out, in_, ident):
        return self.op("pe", lambda e: e.transpose(self.raw(out), self.raw(in_), self.raw(ident)), [in_, ident], [out])

    def act(self, out, in_, func, bias=None, scale=1.0, eng="act"):
        rd = [in_]
        kw = {}
        if bias is not None:
            kw["bias"] = self.raw(bias)
            rd.append(bias)
        if not isinstance(scale, (int, float)):
            rd.append(scale)
            kw["scale"] = self.raw(scale)
        else:
            kw["scale"] = float(scale)
        return self.op(eng, lambda e: e.activation(out=self.raw(out), in_=self.raw(in_), func=func, **kw), rd, [out])

    def ts(self, out, in0, s1, op0, s2=None, op1=None, eng="dve"):
        rd = [in0] + [s for s in (s1, s2) if s is not None and not isinstance(s, (int, float))]
        kw = {}
        if op1 is not None:
            kw["op1"] = op1
        return self.op(eng, lambda e: e.tensor_scalar(out=self.raw(out), in0=self.raw(in0), scalar1=self.raw(s1),
                                                      scalar2=self.raw(s2), op0=op0, **kw), rd, [out])

    def tt(self, out, in0, in1, op, eng="dve"):
        return self.op(eng, lambda e: e.tensor_tensor(out=self.raw(out), in0=self.raw(in0), in1=self.raw(in1), op=op),
                       [in0, in1], [out])

    def stt(self, out, in0, scalar, in1, op0, op1):
        rd = [in0, in1] + ([scalar] if not isinstance(scalar, (int, float)) else [])
        return self.op("dve", lambda e: e.scalar_tensor_tensor(out=self.raw(out), in0=self.raw(in0), scalar=self.raw(scalar),
                                                               in1=self.raw(in1), op0=op0, op1=op1), rd, [out])

    def copy(self, out, in_, eng="dve"):
        if eng == "act":
            return self.op("act", lambda e: e.copy(out=self.raw(out), in_=self.raw(in_)), [in_], [out])
        return self.op(eng, lambda e: e.tensor_copy(out=self.raw(out), in_=self.raw(in_)), [in_], [out])

    def memset(self, ap, val, eng="pool"):
        return self.op(eng, lambda e: e.memset(self.raw(ap), val), [], [ap])

    def red(self, out, in_, op, axis=AX.X):
        return self.op("dve", lambda e: e.tensor_reduce(out=self.raw(out), in_=self.raw(in_), axis=axis, op=op), [in_], [out])

    def recip(self, out, in_):
        return self.op("dve", lambda e: e.reciprocal(out=self.raw(out), in_=self.raw(in_)), [in_], [out])

    def sb(self, es, name, shape, dt):
        self.uid = getattr(self, "uid", 0) + 1
        return es.enter_context(self.nc.sbuf_tensor("%s_u%d" % (name, self.uid), shape, dt))


class Prog:
    def __init__(self, nseq=2, layers=(0, 1), do_mixer=True, do_moe=True, debug=(), branches=(0, 1, 2, 3)):
        self.nseq = nseq
        self.branches = tuple(branches)
        self.layers = tuple(layers)
        self.do_mixer = do_mixer
        self.do_moe = do_moe
        self.debug = set(debug)
        self.nc = bass.Bass("TRN2", target_bir_lowering=False)
        self.k = K(self.nc)
        self.inputs = {}
        self.outputs = {}

    def din(self, name, shape, dt=F32):
        t = self.nc.dram_tensor(name, list(shape), dt, kind="ExternalInput")
        self.inputs[name] = (tuple(shape), dt)
        return t.ap()

    def dout(self, name, shape, dt=F32):
        t = self.nc.dram_tensor(name, list(shape), dt, kind="ExternalOutput")
        self.outputs[name] = (tuple(shape), dt)
        return t.ap()

    def dscr(self, name, shape, dt=F32):
        if name in self.debug:
            return self.dout(name, shape, dt)
        return self.nc.dram_tensor(name, list(shape), dt, kind="Internal").ap()

    def build(self):
        nc, k = self.nc, self.k
        S = self.nseq
        self.xT = self.din("xT", [S, D, T])
        self.outT = self.dout("outT", [S, D, T])
        self.cT = self.din("cT", [128, 8, S])
        self.ada_w = self.din("ada_w", [DEPTH, 2, D, 3 * D])
        self.ada_b = self.din("ada_b", [128, DEPTH * 2 * 3 * 8])
        self.ln_g = self.din("ln_g", [128, DEPTH * 2 * 8])
        self.ln_b = self.din("ln_b", [128, DEPTH * 2 * 8])
        self.router_w = self.din("router_w", [128, 8, NE])
        self.router_b = self.din("router_b", [1, NE])
        self.exp_w1 = self.din("exp_w1", [DEPTH, NE, D, DE])
        self.exp_w3 = self.din("exp_w3", [DEPTH, NE, D, DE])
        self.exp_w2 = self.din("exp_w2", [DEPTH, NE, DE, D])
        self.c_sel16 = self.din("c_sel16", [16, NE * 128])
        self.c_ident = self.din("c_ident", [128, 128])
        self.w_in = self.din("w_in", [DEPTH, D, WIN_COLS])
        self.w_gate = self.din("w_gate", [DEPTH, 4, D, D])
        self.branch_proj = self.din("branch_proj", [DEPTH, 4, 256, D])
        self.w_out = self.din("w_out", [DEPTH, D, D])
        self.fox_fb = self.din("fox_fb", [4, DEPTH])
        self.c_cmask = self.din("c_cmask", [128, 4, 512])
        self.c_invf = self.din("c_invf", [128, 32])
        self.c_gcols = self.din("c_gcols", [128, 8])
        self.c_gmsk = self.din("c_gmsk", [128, 4])
        self.c_mreset = self.din("c_mreset", [128, 1024])
        self.c_gmaskS = self.din("c_gmaskS", [64, 256])
        self.c_gmaskIT = self.din("c_gmaskIT", [64, 256])
        self.c_blk = self.din("c_blk", [128, 128])
        self.gdn_convw = self.din("gdn_convw", [128, DEPTH, 6, 4])
        self.gdn_pA = self.din("gdn_pA", [128, DEPTH])
        self.gdn_pDt = self.din("gdn_pDt", [128, DEPTH])
        self.gdn_nw = self.din("gdn_nw", [64, DEPTH, 64])
        if getattr(self, "gdn_lvl", 9) == 2.47:
            self.dbgout = self.dout("dbgout", [16, 64, 768])
        self.gqn = self.dscr("gqn", [256, T], BF16)
        self.gkn = self.dscr("gkn", [256, T], BF16)
        self.gvs = self.dscr("gvs", [256, T], BF16)
        self.c_cmpmask = self.din("c_cmpmask", [128, 5, 512])
        self.c_bmask = self.din("c_bmask", [128, 4, 512])
        self.c_E2 = self.din("c_E2", [64, 32, 128])
        self.c_ovl = self.din("c_ovl", [128, 2, 64])
        self.c_selA = self.din("c_selA", [128, 32, 64])
        self.c_selB = self.din("c_selB", [128, 32, 64])
        self.nsa_pe = self.din("nsa_pe", [128, DEPTH, 32])
        self.nsa_ck_w1 = self.din("nsa_ck_w1", [DEPTH, 2048, 256])
        self.nsa_cv_w1 = self.din("nsa_cv_w1", [DEPTH, 2048, 256])
        self.nsa_ck_w2 = self.din("nsa_ck_w2", [DEPTH, 256, 64])
        self.nsa_cv_w2 = self.din("nsa_cv_w2", [DEPTH, 256, 64])
        self.gsD = self.dscr("gsD", [12, T], F32)
        self.c_decT = self.din("c_decT", [128, 4, 128])
        self.c_xiT = self.din("c_xiT", [64, 4, 128])
        self.c_zt = self.din("c_zt", [128, 4])
        self.c_cd = self.din("c_cd", [64, 4, 64])
        self.ret_gnw = self.din("ret_gnw", [128, DEPTH * 2])
        self.posT = self.din("posT", [S, 128, 32], I32)
        self.projF = self.dscr("projF", [(NFM - 1) * 128, T], BF16)
        self.projS = self.dscr("projS", [128, T], F32)
        self.projT = self.dscr("projT", [T, NTM], BF16)
        self.obrT = self.dscr("obrT", [4, 256, T], BF16)
        self.xa = self.dscr("xa", [S, D, T])
        self.xb = self.dscr("xb", [S, D, T])

        es = self.k.es
        self.ps = [es.enter_context(nc.psum_tensor("ps%d" % i, [128, 512], F32)) for i in range(8)]
        self.modv = k.sb(es, "modv", [128, DEPTH * 2 * 3 * 8 * S], F32)
        self.lng = k.sb(es, "lng", [128, DEPTH * 2 * 8], F32)
        self.lnb = k.sb(es, "lnb", [128, DEPTH * 2 * 8], F32)
        self.onesD = k.sb(es, "onesD", [128, 128], F32)
        self.epsc = k.sb(es, "epsc", [128, 1], F32)
        self.ident = k.sb(es, "ident", [128, 128], F32)
        self.identb = k.sb(es, "identb", [128, 128], BF16)
        k.memset(self.onesD[:], 1.0 / D)
        self.ones = k.sb(es, "ones", [128, 128], F32)
        self.onec = k.sb(es, "onec", [128, 1], F32)
        k.memset(self.ones[:], 1.0)
        k.memset(self.onec[:], 1.0)
        k.memset(self.epsc[:], LN_EPS)
        self.epsln = k.sb(es, "epsln", [128, 1], F32)
        k.memset(self.epsln[:], LN_EPS / (ALPHA * ALPHA))
        k.dma(self.lng[:], self.ln_g)
        k.dma(self.lnb[:], self.ln_b)
        k.dma(self.ident[:], self.c_ident)
        k.copy(self.identb[:], self.ident[:])

        self.phase_mod()
        cur = [self.xT[s] for s in range(S)]
        for l in self.layers:
            if self.do_mixer:
                nxt = [self.xa[s] for s in range(S)]
                for s in range(S):
                    self.phase_mixer(l, s, cur[s], nxt[s])
                cur = nxt
            if self.do_moe:
                last = (l == self.layers[-1])
                nxt = [self.outT[s] if last else self.xb[s] for s in range(S)]
                self.phase_moe(l, cur, nxt)
                cur = nxt
        k.barrier()
        es.close()
        return nc

    def mcol(self, l, sub, j, kc, s):
        i = ((((l * 2 + sub) * 3 + j) * 8 + kc) * self.nseq + s)
        return self.modv[:, i:i + 1]

    def lcol(self, t, l, sub, kc):
        i = (l * 2 + sub) * 8 + kc
        return t[:, i:i + 1]

    def phase_mod(self):
        nc, k, S = self.nc, self.k, self.nseq
        with ExitStack() as es:
            ct = k.sb(es, "pm_ct", [128, 8, S], F32)
            sc = k.sb(es, "pm_sc", [128, 8, S], F32)
            adab = k.sb(es, "pm_adab", [128, DEPTH * 2 * 3 * 8], F32)
            wsl = [k.sb(es, "pm_w%d" % i, [128, 8, 1024], F32) for i in range(2)]
            k.dma(ct[:], self.cT)
            k.dma(adab[:], self.ada_b)
            k.act(sc[:], ct[:], AF.Silu)
            i = 0
            for l in range(DEPTH):
                for sub in range(2):
                    for j in range(3):
                        w = wsl[i % 2]
                        i += 1
                        src = self.ada_w[l, sub, :, j * 1024:(j + 1) * 1024].rearrange("(kc p) f -> p kc f", p=128)
                        for h in range(2):
                            k.dma(w[:, h * 4:(h + 1) * 4, :], src[:, h * 4:(h + 1) * 4, :], q="sp" if h == 0 else "pool")
                        pst = self.ps[i % 2]
                        for cc in range(8):
                            for kc in range(8):
                                k.mm(pst[:, cc * S:(cc + 1) * S], w[:, kc, cc * 128:(cc + 1) * 128], sc[:, kc, :],
                                     start=(kc == 0), stop=(kc == 7))
                        base = ((l * 2 + sub) * 3 + j) * 8
                        for cc in range(8):
                            o = self.modv[:, (base + cc) * S:(base + cc + 1) * S]
                            if j == 2:
                                k.ts(o, pst[:, cc * S:(cc + 1) * S], adab[:, base + cc:base + cc + 1], ALU.add,
                                     s2=1.0 / ALPHA, op1=ALU.mult)
                            else:
                                k.ts(o, pst[:, cc * S:(cc + 1) * S], adab[:, base + cc:base + cc + 1], ALU.add,
                                     s2=(1.0 if j == 1 else 0.0), op1=ALU.add)
            k.barrier()

    def ln_tile(self, zb, sqb, outb, msb, vsb, l, sub, pM, pQ):
        k = self.k
        for kc in range(8):
            k.act(sqb[:, kc, :], zb[:, kc, :], AF.Square)
        for kc in range(8):
            k.mm(pM[:], self.onesD[:], zb[:, kc, :], start=(kc == 0), stop=(kc == 7))
        for kc in range(8):
            k.mm(pQ[:], self.onesD[:], sqb[:, kc, :], start=(kc == 0), stop=(kc == 7))
        k.copy(msb[:], pM[:], eng="act")
        k.act(vsb[:], pM[:], AF.Square)
        k.tt(vsb[:], pQ[:], vsb[:], ALU.subtract)
        k.act(vsb[:], vsb[:], AF.Sqrt, bias=self.epsln[:])
        k.recip(vsb[:], vsb[:])
        for kc in range(8):
            k.tt(zb[:, kc, :], zb[:, kc, :], msb[:], ALU.subtract, eng="pool")
            k.tt(zb[:, kc, :], zb[:, kc, :], vsb[:], ALU.mult)
            k.act(outb[:, kc, :], zb[:, kc, :], AF.Identity, bias=self.lcol(self.lnb, l, sub, kc),
                  scale=self.lcol(self.lng, l, sub, kc))

    def phase_moe(self, l, src, dst):
        nc, k, S = self.nc, self.k, self.nseq
        ST = 1024
        NT = ST // 512
        ps = self.ps
        with ExitStack() as es:
            hT = k.sb(es, "mo_hT", [128, 8, ST], BF16)
            cTt = k.sb(es, "mo_cT", [16, ST], F32)
            yacc = k.sb(es, "mo_yacc", [128, 8, ST], F32)
            w1b = [k.sb(es, "mo_w1_%d" % i, [128, 8, DE], BF16) for i in range(2)]
            w3b = [k.sb(es, "mo_w3_%d" % i, [128, 8, DE], BF16) for i in range(2)]
            w2b = [k.sb(es, "mo_w2_%d" % i, [128, 4, D], BF16) for i in range(2)]
            xb = k.sb(es, "mo_xb", [128, 8, 512], F32)
            zb = k.sb(es, "mo_zb", [128, 8, 512], F32)
            t1 = [k.sb(es, "mo_t1_%d" % i, [128, 512], F32) for i in range(2)]
            t2 = [k.sb(es, "mo_t2_%d" % i, [128, 512], F32) for i in range(2)]
            hid = [k.sb(es, "mo_hid%d" % i, [128, 4, 512], BF16) for i in range(2)]
            bcS = [k.sb(es, "mo_bc%d" % i, [128, 512], F32) for i in range(2)]
            msb = k.sb(es, "mo_msb", [128, 512], F32)
            vsb = k.sb(es, "mo_vsb", [128, 512], F32)
            rw = k.sb(es, "mo_rw", [128, 8, NE], F32)
            rb = k.sb(es, "mo_rb", [128, NE], F32)
            sel = k.sb(es, "mo_sel", [16, NE * 128], F32)
            rt = k.sb(es, "mo_rt", [128, 12, NE], F32)
            rs = k.sb(es, "mo_rs", [128, 16], F32)
            k.dma(rw[:], self.router_w)
            k.dma(rb[:], self.router_b.broadcast_to([128, NE]))
            k.dma(sel[:], self.c_sel16)

            def load_w(e, buf):
                k.dma(w1b[buf][:], self.exp_w1[l, e].rearrange("(kc p) f -> p kc f", p=128), q="pool")
                k.dma(w3b[buf][:], self.exp_w3[l, e].rearrange("(kc p) f -> p kc f", p=128), q="pool")
                k.dma(w2b[buf][:], self.exp_w2[l, e].rearrange("(fc p) d -> p fc d", p=128), q="pool")

            load_w(0, 0)
            load_w(1, 1)
            for s in range(S):
                for st in range(T // ST):
                    for tt in range(NT):
                        t0 = st * ST + tt * 512
                        k.dma(xb[:], src[s][:, t0:t0 + 512].rearrange("(kc p) t -> p kc t", p=128))
                        for kc in range(8):
                            k.ts(zb[:, kc, :], xb[:, kc, :], self.mcol(l, 1, 1, kc, s), ALU.mult,
                                 s2=self.mcol(l, 1, 0, kc, s), op1=ALU.add)
                            k.copy(hT[:, kc, tt * 512:(tt + 1) * 512], zb[:, kc, :], eng="pool")
                        for q4 in range(4):
                            pr = ps[7]
                            for kc in range(8):
                                k.mm(pr[:, q4 * 16:(q4 + 1) * 16], zb[:, kc, q4 * 128:(q4 + 1) * 128], rw[:, kc, :],
                                     start=(kc == 0), stop=(kc == 7))
                        for q4 in range(4):
                            self.route(pr[:, q4 * 16:(q4 + 1) * 16], rb, rt, rs)
                            pt = ps[6]
                            k.tr(pt[0:16, q4 * 128:(q4 + 1) * 128], rt[:, 0, :], self.ident[:])
                        k.copy(cTt[:, tt * 512:(tt + 1) * 512], pt[0:16, :], eng="act")
                    units = [(e, tt) for e in range(NE) for tt in range(NT)]
                    ebuf = {}

                    def emit_H(u):
                        e, tt = units[u]
                        ebuf[e] = e % 2
                        buf = ebuf[e]
                        tok = slice(tt * 512, (tt + 1) * 512)
                        j = u % 2
                        k.mm(ps[0][:], sel[:, e * 128:(e + 1) * 128], cTt[:, tok])
                        k.copy(bcS[j][:], ps[0][:], eng="act")
                        for fc in range(4):
                            jj = fc % 2
                            p1, p3 = ps[1 + jj], ps[3 + jj]
                            for kc in range(8):
                                k.mm(p1[:], w1b[buf][:, kc, fc * 128:(fc + 1) * 128], hT[:, kc, tok],
                                     start=(kc == 0), stop=(kc == 7))
                            for kc in range(8):
                                k.mm(p3[:], w3b[buf][:, kc, fc * 128:(fc + 1) * 128], hT[:, kc, tok],
                                     start=(kc == 0), stop=(kc == 7))
                            k.act(t1[jj][:], p1[:], AF.Silu)
                            k.tt(t2[jj][:], t1[jj][:], bcS[j][:], ALU.mult, eng="pool")
                            k.tt(hid[j][:, fc, :], t2[jj][:], p3[:], ALU.mult)

                    def emit_Y(u):
                        e, tt = units[u]
                        buf = ebuf[e]
                        tok = slice(tt * 512, (tt + 1) * 512)
                        j = u % 2
                        for dc in range(8):
                            py = ps[5 + (u * 8 + dc) % 3]
                            for fc in range(4):
                                k.mm(py[:], w2b[buf][:, fc, dc * 128:(dc + 1) * 128], hid[j][:, fc, :],
                                     start=(fc == 0), stop=(fc == 3))
                            if e == 0:
                                k.copy(yacc[:, dc, tok], py[:])
                            else:
                                k.tt(yacc[:, dc, tok], yacc[:, dc, tok], py[:], ALU.add)

                    emit_H(0)
                    for u in range(len(units)):
                        if u + 1 < len(units):
                            emit_H(u + 1)
                        emit_Y(u)
                        e_done, tt_done = units[u]
                        if tt_done == NT - 1:
                            last_st = (st == T // ST - 1 and s == S - 1)
                            if not (last_st and e_done + 2 >= NE):
                                load_w((e_done + 2) % NE, e_done % 2)
                    for tt in range(NT):
                        t0 = st * ST + tt * 512
                        k.dma(xb[:], src[s][:, t0:t0 + 512].rearrange("(kc p) t -> p kc t", p=128))
                        for kc in range(8):
                            k.stt(zb[:, kc, :], yacc[:, kc, tt * 512:(tt + 1) * 512], self.mcol(l, 1, 2, kc, s),
                                  xb[:, kc, :], ALU.mult, ALU.add)
                        self.ln_tile(zb, xb, xb, msb, vsb, l, 1, ps[6], ps[7])
                        k.dma(dst[s][:, t0:t0 + 512].rearrange("(kc p) t -> p kc t", p=128), xb[:])
            k.barrier()

    def route(self, logits, rb, rt, rs):
        k = self.k
        s_ = rt[:, 1, :]
        a = rt[:, 2, :]
        k.act(s_, logits, AF.Sigmoid)
        k.tt(a, s_, rb[:], ALU.add)
        a3 = a.rearrange("p (g e) -> p g e", e=4)
        m1 = rs[:, 0:4]
        m2 = rs[:, 4:8]
        k.red(m1, a3, ALU.max)
        oh = rt[:, 3, :].rearrange("p (g e) -> p g e", e=4)
        k.tt(oh, a3, m1.unsqueeze(2).broadcast_to([128, 4, 4]), ALU.is_equal)
        a2 = rt[:, 4, :].rearrange("p (g e) -> p g e", e=4)
        k.stt(a2, oh, -1.0e9, a3, ALU.mult, ALU.add)
        k.red(m2, a2, ALU.max)
        gs = rs[:, 8:12]
        k.tt(gs, m1, m2, ALU.add)
        gm = rs[:, 12:13]
        k.red(gm, gs, ALU.max)
        gse = rt[:, 5, 0:4]
        k.ts(gse, gs, gm, ALU.is_equal)
        selm = rt[:, 6, :].rearrange("p (g e) -> p g e", e=4)
        k.tt(selm, a3, m2.unsqueeze(2).broadcast_to([128, 4, 4]), ALU.is_ge)
        k.tt(selm, selm, gse.unsqueeze(2).broadcast_to([128, 4, 4]), ALU.mult)
        w = rt[:, 7, :]
        k.tt(w, rt[:, 6, :], s_, ALU.mult)
        ws = rs[:, 13:14]
        k.red(ws, w, ALU.add)
        k.recip(rs[:, 14:15], ws)
        k.ts(rt[:, 0, :], w, rs[:, 14:15], ALU.mult)


FM_GQ, FM_GK, FM_GV, FM_GZ, FM_RG, FM_NQ, FM_FQ, FM_FK, FM_NC, FM_NK, FM_SM = 0, 2, 4, 6, 8, 10, 12, 14, 16, 17, 18
NFM = 19
TM_RQ, TM_RK, TM_RV, TM_FV, TM_NVS, TM_NVW = 0, 256, 512, 768, 1024, 1088
NTM = 1152
WIN_COLS = NFM * 128 + NTM


def _mixer_methods():
    def phase_proj(self, l, s, src):
        nc, k = self.nc, self.k
        ps = self.ps
        with ExitStack() as es:
            win = k.sb(es, "pj_win", [128, 8, WIN_COLS], BF16)
            xb = [k.sb(es, "pj_xb%d" % i, [128, 8, 512], F32) for i in range(2)]
            hT = [k.sb(es, "pj_hT%d" % i, [128, 8, 512], BF16) for i in range(2)]
            stF = [k.sb(es, "pj_stF%d" % i, [128, 512], BF16) for i in range(4)]
            stS = k.sb(es, "pj_stS", [128, 512], F32)
            stT = [k.sb(es, "pj_stT%d" % i, [128, NTM], BF16) for i in range(2)]
            for kc in range(8):
                k.dma(win[:, kc, :], self.w_in[l, kc * 128:(kc + 1) * 128, :], q="pool")
            NTT = T // 512
            k.dma(xb[0][:], src[:, 0:512].rearrange("(kc p) t -> p kc t", p=128))
            ev = 0
            for tt in range(NTT):
                t0 = tt * 512
                b = tt % 2
                if tt + 1 < NTT:
                    k.dma(xb[1 - b][:], src[:, t0 + 512:t0 + 1024].rearrange("(kc p) t -> p kc t", p=128))
                for kc in range(8):
                    k.ts(hT[b][:, kc, :], xb[b][:, kc, :], self.mcol(l, 0, 1, kc, s), ALU.mult,
                         s2=self.mcol(l, 0, 0, kc, s), op1=ALU.add)
                for ch in range(NFM):
                    pp = ps[ch % 4]
                    for kc in range(8):
                        k.mm(pp[:], win[:, kc, ch * 128:(ch + 1) * 128], hT[b][:, kc, :], start=(kc == 0), stop=(kc == 7))
                    if ch == FM_SM:
                        k.copy(stS[:], pp[:], eng="act")
                        k.dma(self.projS[:, t0:t0 + 512], stS[:])
                    else:
                        st = stF[ev % 4]
                        k.copy(st[:], pp[:], eng=("act" if ev % 2 == 0 else "dve"))
                        ev += 1
                        k.dma(self.projF[ch * 128:(ch + 1) * 128, t0:t0 + 512], st[:])
                for q4 in range(4):
                    st = stT[q4 % 2]
                    for gi, (c0, cw) in enumerate(((0, 512), (512, 512), (1024, 128))):
                        pp = ps[4 + (q4 * 3 + gi) % 4]
                        for kc in range(8):
                            k.mm(pp[:, 0:cw], hT[b][:, kc, q4 * 128:(q4 + 1) * 128],
                                 win[:, kc, NFM * 128 + c0:NFM * 128 + c0 + cw], start=(kc == 0), stop=(kc == 7))
                        k.copy(st[:, c0:c0 + cw], pp[:, 0:cw], eng=("act" if gi % 2 == 0 else "dve"))
                    k.dma(self.projT[t0 + q4 * 128:t0 + (q4 + 1) * 128, :], st[:])
            k.barrier()

    def attn_finalize(self, pO, rr, pB, osb, outsb, gate_row=None):
        k = self.k
        k.ts(rr[64:65, :], pO[64:65, :], 1e-30, ALU.max)
        k.recip(rr[64:65, :], rr[64:65, :])
        if gate_row is not None:
            k.tt(rr[64:65, :], rr[64:65, :], gate_row, ALU.mult)
        k.mm(pB[0:64, :], self.ones[64:65, 0:64], rr[64:65, :])
        k.copy(osb[0:64, :], pO[0:64, :], eng="act")
        k.tt(outsb, osb[0:64, :], pB[0:64, :], ALU.mult)

    def phase_fox(self, l, s):
        nc, k = self.nc, self.k
        ps = self.ps
        with ExitStack() as es:
            ff = k.sb(es, "fx_ff", [4, T], F32)
            cs = k.sb(es, "fx_cs", [4, T], F32)
            fb = k.sb(es, "fx_fb", [4, DEPTH], F32)
            nfb = k.sb(es, "fx_nfb", [4, 1], F32)
            ckT = k.sb(es, "fx_ckT", [128, 32, 4], F32)
            rhsd = k.sb(es, "fx_rhsd", [4, 8, 4], F32)
            crefB = k.sb(es, "fx_cref", [128, 8, 4], F32)
            biasall = k.sb(es, "fx_bias", [128, 8, 4, 32], F32)
            qT2 = k.sb(es, "fx_qT", [128, T], BF16)
            kT2 = k.sb(es, "fx_kT", [128, T], BF16)
            vaug = k.sb(es, "fx_v", [128, 32, 4, 65], BF16)
            cm = k.sb(es, "fx_cm", [128, 4, 512], BF16)
            Pt = [k.sb(es, "fx_P%d" % i, [128, 512], BF16) for i in range(3)]
            rr = k.sb(es, "fx_rr", [65, 512], F32)
            osb = k.sb(es, "fx_osb", [64, 512], F32)
            outsb = [k.sb(es, "fx_out%d" % i, [64, 512], BF16) for i in range(2)]
            k.dma(cm[:], self.c_cmask, q="pool")
            k.dma(ff[:], self.projS[20:24, :])
            k.dma(fb[:], self.fox_fb)
            k.ts(nfb[:], fb[0:4, l:l + 1], -1.0, ALU.mult)
            k.act(ff[:], ff[:], AF.Exp, bias=nfb[:], scale=-1.0)
            k.act(ff[:], ff[:], AF.Ln, bias=self.onec[0:4, :])
            k.op("dve", lambda e: e.tensor_tensor_scan(out=cs[:], data0=ff[:], data1=ff[:], initial=0.0,
                                                        op0=ALU.add, op1=ALU.max), [ff[:]], [cs[:]])
            pt = ps[0]
            for j in range(32):
                k.tr(pt[:, j * 4:(j + 1) * 4], cs[0:4, j * 128:(j + 1) * 128], self.ident[0:4, 0:4])
            k.copy(ckT[:].rearrange("p a b -> p (a b)"), pt[:, 0:128])
            mids = cs[0:4, :].rearrange("p (i t) -> p i t", t=512)[:, :, 255:256]
            k.tt(rhsd[:], mids.broadcast_to([4, 8, 4]), self.ident[0:4, 0:4].unsqueeze(1).broadcast_to([4, 8, 4]), ALU.mult)
            k.mm(ps[1][:, 0:32], self.ones[0:4, :], rhsd[:].rearrange("p a b -> p (a b)"))
            k.copy(crefB[:].rearrange("p a b -> p (a b)"), ps[1][:, 0:32])
            for i in range(8):
                for h in range(4):
                    k.ts(biasall[:, i, h, :], ckT[:, :, h], crefB[:, i, h:h + 1], ALU.subtract)
            k.memset(vaug[:, :, :, 64:65], 1.0)
            for h in range(4):
                k.dma(vaug[:, :, h, 0:64],
                      self.projT[:, TM_FV + h * 64:TM_FV + (h + 1) * 64].rearrange("(j p) d -> p j d", p=128))
            ob = 0
            pending = []
            for hp in range(2):
                k.dma(qT2[:], self.projF[(FM_FQ + hp) * 128:(FM_FQ + hp + 1) * 128, :])
                k.dma(kT2[:], self.projF[(FM_FK + hp) * 128:(FM_FK + hp + 1) * 128, :])
                for h2 in range(2):
                    hh = hp * 2 + h2
                    po = 64 * h2
                    for i in range(8):
                        pO = ps[4 + i % 2]
                        njt = 4 * (i + 1)
                        for j in range(njt):
                            pS = ps[1 + j % 3]
                            diag = j >= 4 * i
                            k.mm(pS[:], kT2[po:po + 64, j * 128:(j + 1) * 128], qT2[po:po + 64, i * 512:(i + 1) * 512],
                                 start=True, stop=(not diag))
                            if diag:
                                k.mm(pS[:], self.identb[:], cm[:, j - 4 * i, :], start=False, stop=True)
                            P = Pt[j % 3]
                            k.act(P[:], pS[:], AF.Exp, bias=biasall[:, i, hh, j:j + 1], scale=0.125)
                            k.mm(pO[0:65, :], vaug[:, j, hh, :], P[:], start=(j == 0), stop=(j == njt - 1))
                            if j == 1 and pending:
                                pending[0]()
                                pending.clear()

                        def fin(pO=pO, i=i, hh=hh, o=outsb[ob % 2]):
                            self.attn_finalize(pO, rr, ps[6], osb, o[:])
                            k.dma(self.obrT[3, hh * 64:(hh + 1) * 64, i * 512:(i + 1) * 512], o[:])
                        ob += 1
                        pending.append(fin)
                if pending:
                    pending[0]()
                    pending.clear()
            k.barrier()

    def phase_merge(self, l, s, src, dst):
        nc, k = self.nc, self.k
        ps = self.ps
        brs = self.branches
        with ExitStack() as es:
            wg = k.sb(es, "mg_wg", [128, 4, 8, D], BF16)
            bp = k.sb(es, "mg_bp", [128, 4, 2, D], BF16)
            wo = k.sb(es, "mg_wo", [128, 8, D], BF16)
            xb = k.sb(es, "mg_xb", [128, 8, 512], F32)
            zb = k.sb(es, "mg_zb", [128, 8, 512], F32)
            hT = k.sb(es, "mg_hT", [128, 8, 512], BF16)
            oTt = k.sb(es, "mg_oT", [128, 4, 2, 512], BF16)
            sig = [k.sb(es, "mg_sig%d" % i, [128, 512], F32) for i in range(2)]
            tmp = [k.sb(es, "mg_tmp%d" % i, [128, 512], F32) for i in range(2)]
            mer = [k.sb(es, "mg_mer%d" % i, [128, 512], F32) for i in range(2)]
            merged = k.sb(es, "mg_merged", [128, 8, 512], BF16)
            msb = k.sb(es, "mg_msb", [128, 512], F32)
            vsb = k.sb(es, "mg_vsb", [128, 512], F32)
            for br in range(4):
                for half in range(2):
                    k.dma(wg[:, br, half * 4:(half + 1) * 4, :],
                          self.w_gate[l, br, half * 512:(half + 1) * 512, :].rearrange("(kc p) f -> p kc f", p=128), q="pool")
                k.dma(bp[:, br, :, :], self.branch_proj[l, br].rearrange("(c p) f -> p c f", p=128), q="pool")
            k.dma(wo[:], self.w_out[l].rearrange("(kc p) f -> p kc f", p=128), q="pool")
            cnt = 0
            for tt in range(T // 512):
                t0 = tt * 512
                k.dma(xb[:], src[:, t0:t0 + 512].rearrange("(kc p) t -> p kc t", p=128))
                for br in brs:
                    k.dma(oTt[:, br, :, :], self.obrT[br, :, t0:t0 + 512].rearrange("(c p) t -> p c t", p=128))
                for kc in range(8):
                    k.ts(hT[:, kc, :], xb[:, kc, :], self.mcol(l, 0, 1, kc, s), ALU.mult,
                         s2=self.mcol(l, 0, 0, kc, s), op1=ALU.add)
                for dc in range(8):
                    m = mer[dc % 2]
                    for bi, br in enumerate(brs):
                        pG = ps[cnt % 2]
                        pBp = ps[2 + cnt % 2]
                        sg = sig[cnt % 2]
                        cnt += 1
                        for kc in range(8):
                            k.mm(pG[:], wg[:, br, kc, dc * 128:(dc + 1) * 128], hT[:, kc, :], start=(kc == 0), stop=(kc == 7))
                        for c in range(2):
                            k.mm(pBp[:], bp[:, br, c, dc * 128:(dc + 1) * 128], oTt[:, br, c, :], start=(c == 0), stop=(c == 1))
                        k.act(sg[:], pG[:], AF.Sigmoid)
                        if bi == 0:
                            k.tt(m[:], sg[:], pBp[:], ALU.mult)
                        else:
                            tp = tmp[bi % 2]
                            k.tt(tp[:], sg[:], pBp[:], ALU.mult)
                            k.tt(m[:], m[:], tp[:], ALU.add, eng="pool")
                    k.copy(merged[:, dc, :], m[:], eng="pool")
                for d2 in range(8):
                    pY = ps[4 + d2 % 2]
                    for dc in range(8):
                        k.mm(pY[:], wo[:, dc, d2 * 128:(d2 + 1) * 128], merged[:, dc, :], start=(dc == 0), stop=(dc == 7))
                    k.stt(zb[:, d2, :], pY[:], self.mcol(l, 0, 2, d2, s), xb[:, d2, :], ALU.mult, ALU.add)
                self.ln_tile(zb, xb, xb, msb, vsb, l, 0, ps[6], ps[7])
                k.dma(dst[:, t0:t0 + 512].rearrange("(kc p) t -> p kc t", p=128), xb[:])
            k.barrier()

    def phase_mixer(self, l, s, src, dst):
        if getattr(self, "probe_conv", 0):
            self.phase_gdn_conv(l, s)
            return
        self.phase_proj(l, s, src)
        if 0 in self.branches:
            self.phase_gdn(l, s)
        if 1 in self.branches:
            self.phase_ret(l, s)
        if 2 in self.branches:
            self.phase_nsa(l, s)
        if 3 in self.branches:
            self.phase_fox(l, s)
        self.phase_merge(l, s, src, dst)

    def phase_ret(self, l, s):
        nc, k = self.nc, self.k
        ps = self.ps
        psb = [p_[:].bitcast(BF16) for p_ in ps]
        TWO_PI = 2.0 * math.pi
        with ExitStack() as es:
            invf = k.sb(es, "rt_invf", [128, 32], F32)
            decT = k.sb(es, "rt_decT", [128, 4, 128], F32)
            xiT = k.sb(es, "rt_xiT", [64, 4, 128], F32)
            zt = k.sb(es, "rt_zt", [128, 4], F32)
            cdt = k.sb(es, "rt_cd", [64, 4, 64], F32)
            gnw = k.sb(es, "rt_gnw", [128, DEPTH * 2], F32)
            posi = k.sb(es, "rt_posi", [128, 32], I32)
            posf = k.sb(es, "rt_posf", [128, 32], F32)
            ang = k.sb(es, "rt_ang", [128, 32, 32], F32)
            tmpa = k.sb(es, "rt_tmpa", [128, 32, 32], F32)
            tmpi = k.sb(es, "rt_tmpi", [128, 32, 32], I32)
            cosT = k.sb(es, "rt_cos", [128, 32, 32], F32)
            sinT = k.sb(es, "rt_sin", [128, 32, 32], F32)
            tq = [k.sb(es, "rt_tq%d" % i, [128, 768], BF16) for i in range(2)]
            gt = [k.sb(es, "rt_gt%d" % i, [128, 2, 128], BF16) for i in range(2)]
            sg = k.sb(es, "rt_sg", [128, 2, 128], F32)
            ra = [k.sb(es, "rt_ra%d" % i, [128, 4, 32], F32) for i in range(4)]
            qr = k.sb(es, "rt_qr", [128, 4, 64], BF16)
            kr = k.sb(es, "rt_kr", [128, 4, 64], BF16)
            kz = k.sb(es, "rt_kz", [128, 4, 64], BF16)
            qrT = k.sb(es, "rt_qrT", [64, 4, 128], BF16)
            qxT = k.sb(es, "rt_qxT", [64, 4, 128], BF16)
            krT = k.sb(es, "rt_krT", [64, 4, 128], BF16)
            Sd = [k.sb(es, "rt_Sd%d" % i, [128, 128], BF16) for i in range(2)]
            st32 = k.sb(es, "rt_st32", [64, 4, 64], F32)
            stb = k.sb(es, "rt_stb", [64, 4, 64], BF16)
            o32 = k.sb(es, "rt_o32", [128, 4, 64], F32)
            st6 = k.sb(es, "rt_st6", [128, 4, 6], F32)
            mv = k.sb(es, "rt_mv", [128, 4, 2], F32)
            rstd = k.sb(es, "rt_rstd", [128, 4], F32)
            on = k.sb(es, "rt_on", [128, 4, 64], BF16)
            oT = k.sb(es, "rt_oT", [128, 2, 512], BF16)
            k.dma(invf[:], self.c_invf)
            k.dma(decT[:], self.c_decT)
            k.dma(xiT[:], self.c_xiT)
            k.dma(zt[:], self.c_zt)
            k.dma(cdt[:], self.c_cd)
            k.dma(gnw[:], self.ret_gnw)
            k.dma(posi[:], self.posT[s])
            k.copy(posf[:], posi[:])
            k.tt(ang[:], posf[:].unsqueeze(2).broadcast_to([128, 32, 32]), invf[:].unsqueeze(1).broadcast_to([128, 32, 32]), ALU.mult)

            def sin_of(dst, shift):
                k.ts(tmpa[:], ang[:], 1.0 / TWO_PI, ALU.mult, s2=shift / TWO_PI + 0.5, op1=ALU.add)
                k.copy(tmpi[:], tmpa[:])
                k.copy(tmpa[:], tmpi[:])
                k.stt(tmpa[:], tmpa[:], -TWO_PI, ang[:], ALU.mult, ALU.add)
                if shift != 0.0:
                    k.ts(tmpa[:], tmpa[:], shift, ALU.add)
                k.ts(dst, tmpa[:], -math.pi, ALU.is_lt, s2=TWO_PI, op1=ALU.mult)
                k.tt(tmpa[:], tmpa[:], dst, ALU.add)
                k.ts(dst, tmpa[:], math.pi, ALU.is_gt, s2=-TWO_PI, op1=ALU.mult)
                k.tt(tmpa[:], tmpa[:], dst, ALU.add)
                k.ts(tmpa[:], tmpa[:], math.pi, ALU.min, s2=-math.pi, op1=ALU.max)
                k.act(dst, tmpa[:], AF.Sin)

            sin_of(sinT[:], 0.0)
            sin_of(cosT[:], math.pi / 2.0)
            k.memset(st32[:], 0.0)
            k.memset(stb[:], 0.0)

            def rope(dst, src4, j):
                cb = cosT[:, j, :].unsqueeze(1).broadcast_to([128, 4, 32])
                sb_ = sinT[:, j, :].unsqueeze(1).broadcast_to([128, 4, 32])
                x1 = src4[:, :, 0:32]
                x2 = src4[:, :, 32:64]
                k.tt(ra[0][:], x1, cb, ALU.mult, eng="pool")
                k.tt(ra[1][:], x2, sb_, ALU.mult, eng="pool")
                k.tt(dst[:, :, 0:32], ra[0][:], ra[1][:], ALU.subtract)
                k.tt(ra[2][:], x1, sb_, ALU.mult, eng="pool")
                k.tt(ra[3][:], x2, cb, ALU.mult, eng="pool")
                k.tt(dst[:, :, 32:64], ra[2][:], ra[3][:], ALU.add)

            NJ = T // 128
            k.dma(tq[0][:], self.projT[0:128, 0:768])
            for j in range(NJ):
                b = j % 2
                if j + 1 < NJ:
                    k.dma(tq[1 - b][:], self.projT[(j + 1) * 128:(j + 2) * 128, 0:768])
                k.dma(gt[b][:], self.projF[FM_RG * 128:(FM_RG + 2) * 128, j * 128:(j + 1) * 128].rearrange("(c p) t -> p c t", p=128))
                q4 = tq[b][:, 0:256].rearrange("p (h d) -> p h d", d=64)
                k4 = tq[b][:, 256:512].rearrange("p (h d) -> p h d", d=64)
                v4 = tq[b][:, 512:768].rearrange("p (h d) -> p h d", d=64)
                rope(qr, q4, j)
                rope(kr, k4, j)
                k.tt(kz[:], kr[:], zt[:].unsqueeze(2).broadcast_to([128, 4, 64]), ALU.mult, eng="pool")
                pq = psb[0]
                pk = psb[1]
                for h in range(4):
                    k.tr(pq[0:64, h * 128:(h + 1) * 128], qr[:, h, :], self.identb[:])
                for h in range(4):
                    k.tr(pk[0:64, h * 128:(h + 1) * 128], kr[:, h, :], self.identb[:])
                k.copy(qrT[:].rearrange("p h t -> p (h t)"), pq[0:64, 0:512], eng="act")
                k.tt(qxT[:].rearrange("p h t -> p (h t)"), pq[0:64, 0:512], xiT[:].rearrange("p h t -> p (h t)"), ALU.mult)
                k.act(krT[:].rearrange("p h t -> p (h t)"), pk[0:64, 0:512], AF.Copy, scale=0.125)
                pO = ps[2 + b]
                pKV = ps[4]
                for h in range(4):
                    pS = ps[5 + h % 2]
                    k.mm(pS[:, 0:128], krT[:, h, :], qrT[:, h, :])
                    sd = Sd[h % 2]
                    k.tt(sd[:], pS[:, 0:128], decT[:, h, :], ALU.mult)
                    k.mm(pO[:, h * 64:(h + 1) * 64], sd[:], v4[:, h, :], start=True, stop=False)
                    k.mm(pO[:, h * 64:(h + 1) * 64], qxT[:, h, :], stb[:, h, :], start=False, stop=True)
                    k.mm(pKV[0:64, h * 64:(h + 1) * 64], kz[:, h, :], v4[:, h, :])
                k.tt(st32[:], st32[:], cdt[:], ALU.mult, eng="pool")
                k.tt(st32[:].rearrange("p h d -> p (h d)"), st32[:].rearrange("p h d -> p (h d)"), pKV[0:64, 0:256], ALU.add)
                k.copy(stb[:], st32[:], eng="pool")
                k.copy(o32[:].rearrange("p h d -> p (h d)"), pO[:, 0:256], eng="act")
                for h in range(4):
                    k.op("dve", lambda e, h=h: e.bn_stats(out=st6[:, h, :], in_=o32[:, h, :]), [o32[:]], [st6[:]])
                for h in range(4):
                    k.op("dve", lambda e, h=h: e.bn_aggr(out=mv[:, h, :], in_=st6[:, h, :]), [st6[:]], [mv[:]])
                k.act(rstd[:], mv[:, :, 1], AF.Sqrt, bias=self.epsc[:])
                k.recip(rstd[:], rstd[:])
                k.tt(o32[:], o32[:], mv[:, :, 0:1].broadcast_to([128, 4, 64]), ALU.subtract)
                k.tt(on[:], o32[:], rstd[:].unsqueeze(2).broadcast_to([128, 4, 64]), ALU.mult)
                k.act(sg[:], gt[b][:], AF.Silu)
                po = psb[7]
                for c in range(2):
                    k.tr(po[:, c * 128:(c + 1) * 128], on[:, 2 * c:2 * c + 2, :].rearrange("p h d -> p (h d)"), self.identb[:])
                jj = j % 4
                for c in range(2):
                    k.stt(oT[:, c, jj * 128:(jj + 1) * 128], po[:, c * 128:(c + 1) * 128], gnw[:, l * 2 + c:l * 2 + c + 1],
                          sg[:, c, :], ALU.mult, ALU.mult)
                if jj == 3:
                    t0 = (j - 3) * 128
                    k.dma(self.obrT[1, :, t0:t0 + 512].rearrange("(c p) t -> p c t", p=128), oT[:])
            k.barrier()

    def phase_nsa(self, l, s):
        nc, k = self.nc, self.k
        ps = self.ps
        psb = [p_[:].bitcast(BF16) for p_ in ps]
        with ExitStack() as es:
            W1 = k.sb(es, "ns_W1", [128, 32, 256], BF16)
            w2k = k.sb(es, "ns_w2k", [128, 2, 64], BF16)
            w2v = k.sb(es, "ns_w2v", [128, 2, 64], BF16)
            pe32 = k.sb(es, "ns_pe32", [128, DEPTH, 32], F32)
            peb = k.sb(es, "ns_peb", [128, 32], BF16)
            nc16 = k.sb(es, "ns_nc16", [128, T], BF16)
            ksT = k.sb(es, "ns_ksT", [64, T], BF16)
            kwT = k.sb(es, "ns_kwT", [64, T], BF16)
            qt = [k.sb(es, "ns_qt%d" % i, [64, 4, 512], BF16) for i in range(2)]
            vsa = k.sb(es, "ns_vsa", [128, 32, 65], BF16)
            vwa = k.sb(es, "ns_vwa", [128, 32, 65], BF16)
            cmpm = k.sb(es, "ns_cmpm", [128, 5, 512], BF16)
            cm = k.sb(es, "ns_cm", [128, 4, 512], BF16)
            bm = k.sb(es, "ns_bm", [128, 4, 512], BF16)
            E2 = k.sb(es, "ns_E2", [64, 32, 128], BF16)
            ovl = k.sb(es, "ns_ovl", [128, 2, 64], BF16)
            selA = k.sb(es, "ns_selA", [128, 32, 64], F32)
            selB = k.sb(es, "ns_selB", [128, 32, 64], F32)
            onesb = k.sb(es, "ns_onesb", [128, 128], BF16)
            hs = k.sb(es, "ns_hs", [128, 2, 2, 256], BF16)
            bias_h = k.sb(es, "ns_bh", [128, 4], F32)
            kcmpT = k.sb(es, "ns_kcmpT", [64, 256], BF16)
            vcmp = k.sb(es, "ns_vcmp", [128, 2, 64], BF16)
            gsp = k.sb(es, "ns_gsp", [128, 1024], F32)
            gt4 = k.sb(es, "ns_gt4", [65, 4, 3, 512], F32)
            PT = [k.sb(es, "ns_PT%d" % i, [128, 512], BF16) for i in range(3)]
            Pn = k.sb(es, "ns_Pn", [128, 4, 2, 512], BF16)
            rD = k.sb(es, "ns_rD", [128, 512], F32)
            sc = k.sb(es, "ns_sc", [128, 64], F32)
            sc2 = k.sb(es, "ns_sc2", [128, 64], F32)
            m8 = k.sb(es, "ns_m8", [128, 16], F32)
            mb = k.sb(es, "ns_mb", [128, 64], BF16)
            MbT = k.sb(es, "ns_MbT", [64, 512], BF16)
            rr = k.sb(es, "ns_rr", [65, 512], F32)
            osb = k.sb(es, "ns_osb", [64, 512], F32)
            tmpo = k.sb(es, "ns_tmpo", [64, 512], F32)
            acc = k.sb(es, "ns_acc", [64, 4, 512], F32)
            outsb = [k.sb(es, "ns_out%d" % i, [64, 512], BF16) for i in range(2)]
            k.dma(W1[0:64, :, :], self.nsa_ck_w1[l].rearrange("(l d) h -> d l h", d=64), q="pool")
            k.dma(W1[64:128, :, :], self.nsa_cv_w1[l].rearrange("(l d) h -> d l h", d=64), q="pool")
            k.dma(w2k[:], self.nsa_ck_w2[l].rearrange("(c p) d -> p c d", p=128), q="pool")
            k.dma(w2v[:], self.nsa_cv_w2[l].rearrange("(c p) d -> p c d", p=128), q="pool")
            k.dma(pe32[:], self.nsa_pe)
            k.copy(peb[:], pe32[:, l, :])
            k.dma(cmpm[:], self.c_cmpmask, q="pool")
            k.dma(cm[:], self.c_cmask, q="pool")
            k.dma(bm[:], self.c_bmask, q="pool")
            k.dma(E2[:], self.c_E2, q="pool")
            k.dma(ovl[:], self.c_ovl, q="pool")
            k.dma(selA[:], self.c_selA)
            k.dma(selB[:], self.c_selB)
            k.memset(onesb[:], 1.0)
            k.dma(nc16[:], self.projF[FM_NC * 128:(FM_NC + 1) * 128, :])
            k.dma(ksT[:], self.projF[FM_NK * 128:FM_NK * 128 + 64, :])
            k.dma(kwT[:], self.projF[FM_NK * 128 + 64:FM_NK * 128 + 128, :])
            k.memset(vsa[:, :, 64:65], 1.0)
            k.memset(vwa[:, :, 64:65], 1.0)
            k.dma(vsa[:, :, 0:64], self.projT[:, TM_NVS:TM_NVS + 64].rearrange("(j p) d -> p j d", p=128))
            k.dma(vwa[:, :, 0:64], self.projT[:, TM_NVW:TM_NVW + 64].rearrange("(j p) d -> p j d", p=128))
            for pc in range(4):
                sl = slice(pc * 1024, (pc + 1) * 1024)
                k.dma(gsp[64:76, :], self.projS[8:20, sl])
                k.act(gsp[64:76, :], gsp[64:76, :], AF.Sigmoid)
                k.dma((self.gsD[:, sl], "gsD"), gsp[64:76, :])
            ncv = nc16[:].rearrange("p (n s) -> p n s", s=16)
            for kv in range(2):
                po = 64 * kv
                for hc in range(2):
                    pb = ps[0][:, kv * 2 + hc:kv * 2 + hc + 1]
                    for l_ in range(32):
                        k.mm(pb, W1[po:po + 64, l_, hc * 128:(hc + 1) * 128], peb[po:po + 64, l_:l_ + 1],
                             start=(l_ == 0), stop=(l_ == 31))
            k.copy(bias_h[:], ps[0][:, 0:4])
            for kv in range(2):
                po = 64 * kv
                for hc in range(2):
                    ph = ps[1 + hc]
                    for l_ in range(32):
                        k.mm(ph[:, 0:255], W1[po:po + 64, l_, hc * 128:(hc + 1) * 128],
                             ncv[po:po + 64, (l_ // 16):(l_ // 16) + 255, l_ % 16], start=(l_ == 0), stop=(l_ == 31))
                    k.act(hs[:, kv, hc, 0:255], ph[:, 0:255], AF.Silu, bias=bias_h[:, kv * 2 + hc:kv * 2 + hc + 1])
            for hc in range(2):
                k.mm(ps[3][0:64, 0:255], w2k[:, hc, :], hs[:, 0, hc, 0:255], start=(hc == 0), stop=(hc == 1))
            k.copy(kcmpT[:, 0:255], ps[3][0:64, 0:255], eng="act")
            MC = (128, 127)
            for c in range(2):
                M = MC[c]
                for hc in range(2):
                    k.mm(ps[4][0:M, c * 64:(c + 1) * 64], hs[:, 1, hc, c * 128:c * 128 + M], w2v[:, hc, :],
                         start=(hc == 0), stop=(hc == 1))
                k.copy(vcmp[0:M, c, :], ps[4][0:M, c * 64:(c + 1) * 64])
            ob = 0
            pending = []
            for i in range(8):
                tok = slice(i * 512, (i + 1) * 512)
                q_ = qt[i % 2]
                k.dma(q_[:], self.projF[FM_NQ * 128:(FM_NQ + 2) * 128, tok].rearrange("(h d) t -> d h t", d=64))
                k.dma(gt4[64:65, :, :, :].rearrange("p h b t -> p (h b) t"),
                      (self.gsD[:, tok].rearrange("(o r) t -> o r t", o=1), "gsD"))
                ncs = [0] if i <= 3 else [0, 1]
                for h in range(4):
                    for c in ncs:
                        M = MC[c]
                        pS = ps[c]
                        d_ = 512 * i - 2048 * c
                        need = d_ < 2063
                        k.mm(pS[0:M, :], kcmpT[:, c * 128:c * 128 + M], q_[:, h, :], start=True, stop=(not need))
                        if need:
                            k.mm(pS[0:M, :], self.identb[:, 0:M], cmpm[:, d_ // 512, :], start=False, stop=True)
                        k.act(PT[c][0:M, :], pS[0:M, :], AF.Exp, scale=0.125)
                    for ci, c in enumerate(ncs):
                        M = MC[c]
                        k.mm(ps[2][:], onesb[0:M, :], PT[c][0:M, :], start=(ci == 0), stop=(ci == len(ncs) - 1))
                    k.ts(rD[:], ps[2][:], 1e-30, ALU.max)
                    k.recip(rD[:], rD[:])
                    for c in ncs:
                        M = MC[c]
                        k.tt(Pn[0:M, h, c, :], PT[c][0:M, :], rD[0:M, :], ALU.mult, eng="pool")
                    for ci, c in enumerate(ncs):
                        M = MC[c]
                        k.mm(ps[3][0:64, :], vcmp[0:M, c, :], Pn[0:M, h, c, :], start=(ci == 0), stop=(ci == len(ncs) - 1))
                    k.mm(ps[7][0:64, :], self.ones[64:65, 0:64], gt4[64:65, h, 0, :])
                    k.copy(osb[:], ps[3][0:64, :], eng="act")
                    k.tt(acc[:, h, :], osb[:], ps[7][0:64, :], ALU.mult)
                pT = psb[2]
                for q4 in range(4):
                    pI = ps[4]
                    n_mm = 4 * len(ncs)
                    ii = 0
                    for h in range(4):
                        for c in ncs:
                            M = MC[c]
                            k.mm(pI[:, 0:64], Pn[0:M, h, c, q4 * 128:(q4 + 1) * 128], ovl[0:M, c, :],
                                 start=(ii == 0), stop=(ii == n_mm - 1))
                            ii += 1
                    u = i * 4 + q4
                    k.tt(sc[:], pI[:, 0:64], selA[:, u, :], ALU.mult)
                    k.tt(sc[:], sc[:], selB[:, u, :], ALU.add)
                    k.op("dve", lambda e: e.max(out=m8[:, 0:8], in_=sc[:]), [sc[:]], [m8[:]])
                    k.op("dve", lambda e: e.match_replace(out=sc2[:], in_to_replace=m8[:, 0:8], in_values=sc[:], imm_value=-1.0e9),
                         [sc[:], m8[:]], [sc2[:]])
                    k.op("dve", lambda e: e.max(out=m8[:, 8:16], in_=sc2[:]), [sc2[:]], [m8[:]])
                    k.ts(mb[:], sc[:], m8[:, 15:16], ALU.is_ge, s2=1.0, op1=ALU.subtract)
                    k.tr(pT[0:64, q4 * 128:(q4 + 1) * 128], mb[:], self.identb[:])
                k.copy(MbT[:], pT[0:64, 0:512], eng="act")
                for h in range(4):
                    njt = 4 * (i + 1)
                    pO = ps[5]
                    for kt in range(njt):
                        pS = ps[kt % 4]
                        diag = kt >= 4 * i
                        k.mm(pS[:], ksT[:, kt * 128:(kt + 1) * 128], q_[:, h, :], start=True, stop=False)
                        k.mm(pS[:], E2[:, kt, :], MbT[:], start=False, stop=(not diag))
                        if diag:
                            k.mm(pS[:], self.identb[:], cm[:, kt - 4 * i, :], start=False, stop=True)
                        P = PT[kt % 3]
                        k.act(P[:], pS[:], AF.Exp, scale=0.125)
                        k.mm(pO[0:65, :], vsa[:, kt, :], P[:], start=(kt == 0), stop=(kt == njt - 1))
                        if kt == 1 and pending:
                            pending[0]()
                            pending.clear()

                    def fin_s(pO=pO, h=h):
                        self.attn_finalize(pO, rr, ps[7], osb, tmpo[:], gate_row=gt4[64:65, h, 1, :])
                        k.tt(acc[:, h, :], acc[:, h, :], tmpo[:], ALU.add, eng="pool")
                    pending.append(fin_s)
                    pO = ps[6]
                    kts = list(range(max(0, 4 * i - 4), 4 * i + 4))
                    for ki, kt in enumerate(kts):
                        pS = ps[kt % 4]
                        diag = kt >= 4 * i
                        mk = cm[:, kt - 4 * i, :] if diag else bm[:, kt - 4 * i + 4, :]
                        k.mm(pS[:], kwT[:, kt * 128:(kt + 1) * 128], q_[:, h, :], start=True, stop=False)
                        k.mm(pS[:], self.identb[:], mk, start=False, stop=True)
                        P = PT[kt % 3]
                        k.act(P[:], pS[:], AF.Exp, scale=0.125)
                        k.mm(pO[0:65, :], vwa[:, kt, :], P[:], start=(ki == 0), stop=(ki == len(kts) - 1))
                        if ki == 1 and pending:
                            pending[0]()
                            pending.clear()

                    def fin_w(pO=pO, h=h, o=outsb[ob % 2], tok=tok):
                        self.attn_finalize(pO, rr, ps[7], osb, tmpo[:], gate_row=gt4[64:65, h, 2, :])
                        k.tt(o[:], acc[:, h, :], tmpo[:], ALU.add)
                        k.dma(self.obrT[2, h * 64:(h + 1) * 64, tok], o[:])
                    ob += 1
                    pending.append(fin_w)
                if pending:
                    pending[0]()
                    pending.clear()
            k.barrier()

    def phase_gdn_conv(self, l, s):
        nc, k = self.nc, self.k
        ps = self.ps
        with ExitStack() as es:
            cw = k.sb(es, "gc_cw", [128, DEPTH, 6, 4], F32)
            blk = k.sb(es, "gc_blk", [128, 128], BF16)
            gcol = k.sb(es, "gc_gcol", [128, 8], F32)
            xpad = [k.sb(es, "gc_xp%d" % i, [128, 4 + T], F32) for i in range(2)]
            acc = k.sb(es, "gc_acc", [128, T], F32)
            ys = k.sb(es, "gc_ys", [128, T], F32)
            sq = k.sb(es, "gc_sq", [128, T], BF16)
            rin = [k.sb(es, "gc_rin%d" % i, [128, 512], F32) for i in range(2)]
            outb = [k.sb(es, "gc_out%d" % i, [128, T], BF16) for i in range(2)]
            k.dma(cw[:], self.gdn_convw)
            k.dma(blk[:], self.c_blk, q="pool")
            k.dma(gcol[:], self.c_gcols)
            lvl = getattr(self, "probe_conv", 9)
            for ch in range(6):
                xp = xpad[ch % 2]
                ob = outb[ch % 2]
                if lvl == 1 and ch > 0:
                    break
                k.memset(xp[:, 0:4], 0.0)
                k.dma(xp[:, 4:4 + T], self.projF[ch * 128:(ch + 1) * 128, :], q="pool")
                k.ts(acc[:], xp[:, 4:4 + T], cw[:, l, ch, 3:4], ALU.mult)
                for kk in (2, 1, 0):
                    k.stt(acc[:], xp[:, 1 + kk:1 + kk + T], cw[:, l, ch, kk:kk + 1], acc[:], ALU.mult, ALU.add)
                if lvl <= 2:
                    continue
                if ch >= 4:
                    k.act(ob[:], acc[:], AF.Silu)
                    k.dma(self.gvs[(ch - 4) * 128:(ch - 3) * 128, :], ob[:])
                else:
                    k.act(ys[:], acc[:], AF.Silu)
                    k.act(sq[:], ys[:], AF.Square)
                    if lvl <= 3:
                        continue
                    for tt in range(8):
                        sl = slice(tt * 512, (tt + 1) * 512)
                        pss = ps[tt % 2]
                        r = rin[tt % 2]
                        k.mm(pss[:], blk[:], sq[:, sl])
                        k.act(r[:], pss[:], AF.Sqrt, bias=gcol[:, 6:7])
                        k.recip(r[:], r[:])
                        if ch < 2:
                            k.stt(ob[:, sl], ys[:, sl], 0.125, r[:], ALU.mult, ALU.mult)
                        else:
                            k.tt(ob[:, sl], ys[:, sl], r[:], ALU.mult, eng="pool")
                    dst = self.gqn if ch < 2 else self.gkn
                    k.dma(dst[(ch % 2) * 128:(ch % 2 + 1) * 128, :], ob[:])
            k.barrier()

    def phase_gdn(self, l, s):
        self.phase_gdn_conv(l, s)
        if getattr(self, "gdn_conv_only", False):
            return
        nc, k = self.nc, self.k
        ps = self.ps
        psb = [p_[:].bitcast(BF16) for p_ in ps]
        TP = 1024
        NCH = TP // 64
        with ExitStack() as es:
            gcol = k.sb(es, "gd_gcol", [128, 8], F32)
            gmsk = k.sb(es, "gd_gmsk", [128, 4], F32)
            mreset = k.sb(es, "gd_mreset", [128, TP], F32)
            maskS = k.sb(es, "gd_maskS", [64, 256], F32)
            maskIT = k.sb(es, "gd_maskIT", [64, 256], F32)
            pA = k.sb(es, "gd_pA", [128, DEPTH], F32)
            pDt = k.sb(es, "gd_pDt", [128, DEPTH], F32)
            nA = k.sb(es, "gd_nA", [128, 1], F32)
            nw = k.sb(es, "gd_nw", [64, DEPTH, 64], F32)
            raw = k.sb(es, "gd_raw", [128, TP], F32)
            gg = k.sb(es, "gd_gg", [128, TP], F32)
            bc = k.sb(es, "gd_bc", [128, TP], F32)
            At = k.sb(es, "gd_A", [128, TP], F32)
            Bt = k.sb(es, "gd_B", [128, TP], F32)
            AtH = k.sb(es, "gd_AH", [2, 4, TP], F32)
            BtH = k.sb(es, "gd_BH", [2, 4, TP], F32)
            R1 = k.sb(es, "gd_R1", [128, TP], F32)
            R2 = k.sb(es, "gd_R2", [128, TP], F32)
            rhsd = k.sb(es, "gd_rhsd", [128, NCH, 4], F32)
            Gs = k.sb(es, "gd_Gs", [64, NCH, 4], F32)
            qn = k.sb(es, "gd_qn", [64, 4, TP], BF16)
            kn = k.sb(es, "gd_kn", [64, 4, TP], BF16)
            vv = k.sb(es, "gd_vv", [64, 4, TP], BF16)
            zT = k.sb(es, "gd_zT", [128, 2, TP], BF16)
            sz = k.sb(es, "gd_sz", [128, 2, 64], F32)
            tm1 = k.sb(es, "gd_tm1", [64, 128], F32)
            tm2 = k.sb(es, "gd_tm2", [64, 128], F32)
            sm = k.sb(es, "gd_sm", [64, 8, 4], F32)
            kvtm = k.sb(es, "gd_kvtm", [64, 512], BF16)
            kdec = k.sb(es, "gd_kdec", [64, 4, 64], BF16)
            Ds = k.sb(es, "gd_Ds", [64, 256], F32)
            DTs = k.sb(es, "gd_DTs", [64, 256], F32)
            tmpN = k.sb(es, "gd_tmpN", [64, 256], F32)
            Nb = [k.sb(es, "gd_N%d" % i, [64, 4, 64], BF16) for i in range(2)]
            Pb = [k.sb(es, "gd_P%d" % i, [64, 4, 64], BF16) for i in range(2)]
            attnT = k.sb(es, "gd_attnT", [64, 4, 64], BF16)
            y32 = k.sb(es, "gd_y32", [64, 4, 128], F32)
            yb = k.sb(es, "gd_yb", [64, 4, 128], BF16)
            wT = k.sb(es, "gd_wT", [64, 4, 64], BF16)
            ub = k.sb(es, "gd_ub", [64, 4, 64], BF16)
            S32 = k.sb(es, "gd_S32", [64, 4, 64], F32)
            Sb = k.sb(es, "gd_Sb", [64, 4, 64], BF16)
            t1 = k.sb(es, "gd_t1", [64, 4, 64], F32)
            o32 = k.sb(es, "gd_o32", [64, 4, 64], F32)
            osq = k.sb(es, "gd_osq", [64, 4, 64], F32)
            ss = k.sb(es, "gd_ss", [64, 8], F32)
            on = k.sb(es, "gd_on", [64, 4, 64], BF16)
            oT = k.sb(es, "gd_oT", [128, 2, 512], BF16)
            k.dma(gcol[:], self.c_gcols)
            k.dma(gmsk[:], self.c_gmsk)
            k.dma(mreset[:], self.c_mreset)
            k.dma(maskS[:], self.c_gmaskS)
            k.dma(maskIT[:], self.c_gmaskIT)
            k.dma(pA[:], self.gdn_pA)
            k.dma(pDt[:], self.gdn_pDt)
            k.dma(nw[:], self.gdn_nw)
            k.act(nA[:], pA[:, l:l + 1], AF.Exp)
            k.ts(nA[:], nA[:], -1.0, ALU.mult)
            k.memset(S32[:], 0.0)
            k.memset(Sb[:], 0.0)
            id64 = self.ident[0:64, 0:64]
            idb64 = self.identb[0:64, 0:64]
            for pc in range(T // TP):
                p0 = pc * TP
                k.dma(raw[:], self.projS[:, p0:p0 + TP])
                k.dma(qn[:], self.gqn[:, p0:p0 + TP].rearrange("(h d) t -> d h t", d=64))
                k.dma(kn[:], self.gkn[:, p0:p0 + TP].rearrange("(h d) t -> d h t", d=64))
                k.dma(vv[:], self.gvs[:, p0:p0 + TP].rearrange("(h d) t -> d h t", d=64))
                k.dma(zT[:], self.projF[FM_GZ * 128:(FM_GZ + 2) * 128, p0:p0 + TP].rearrange("(c p) t -> p c t", p=128))
                k.act(gg[:], raw[:], AF.Exp, bias=pDt[:, l:l + 1])
                k.act(gg[:], gg[:], AF.Ln, bias=self.onec[:])
                k.ts(gg[:], gg[:], nA[:], ALU.mult)
                k.op("dve", lambda e: e.tensor_tensor_scan(out=bc[:], data0=mreset[:], data1=gg[:], initial=0.0,
                                                            op0=ALU.mult, op1=ALU.add), [mreset[:], gg[:]], [bc[:]])
                k.ts(At[:], bc[:], gcol[:, 0:1], ALU.mult, s2=gcol[:, 1:2], op1=ALU.add)
                k.ts(Bt[:], bc[:], gcol[:, 2:3], ALU.mult, s2=gcol[:, 3:4], op1=ALU.add)
                for h in range(4):
                    k.dma(AtH[:, h, :], At[32 * h:32 * h + 2, :])
                    k.dma(BtH[:, h, :], Bt[32 * h:32 * h + 2, :])
                k.act(raw[:], raw[:], AF.Sigmoid)
                k.ts(raw[:], raw[:], gcol[:, 4:5], ALU.mult)
                k.stt(R1[:], bc[:], gcol[:, 5:6], raw[:], ALU.mult, ALU.add)
                bl = bc[:].rearrange("p (n c) -> p n c", c=64)[:, :, 63:64]
                k.tt(R2[:].rearrange("p (n c) -> p n c", c=64), bl.broadcast_to([128, NCH, 64]),
                     bc[:].rearrange("p (n c) -> p n c", c=64), ALU.subtract)
                k.act(R2[:], R2[:], AF.Exp)
                k.tt(rhsd[:], bl.broadcast_to([128, NCH, 4]), gmsk[:].unsqueeze(1).broadcast_to([128, NCH, 4]), ALU.mult)
                k.mm(ps[7][0:64, 0:NCH * 4], self.ones[:, 0:64], rhsd[:].rearrange("p n h -> p (n h)"))
                k.act(Gs[:].rearrange("p n h -> p (n h)"), ps[7][0:64, 0:NCH * 4], AF.Exp)
                glv = getattr(self, "gdn_lvl", 9)
                if glv <= 1:
                    continue
                for n in range(NCH):
                    c0 = n * 64
                    cs_ = slice(c0, c0 + 64)
                    gn = pc * NCH + n
                    k.tr(ps[0][0:64, 0:128], R1[:, cs_], self.ident[:])
                    k.tr(ps[0][0:64, 128:256], R2[:, cs_], self.ident[:])
                    k.copy(tm1[:], ps[0][0:64, 0:128], eng="act")
                    k.copy(tm2[:], ps[0][0:64, 128:256], eng="act")
                    if glv <= 2.1:
                        continue
                    tv1 = tm1[:].rearrange("p (h r) -> p h r", r=32)
                    tv2 = tm2[:].rearrange("p (h r) -> p h r", r=32)
                    b_tm = tv1[:, :, 0]
                    beta_tm = tv1[:, :, 2]
                    ekd_tm = tv2[:, :, 0]
                    nbeta, eb, beb = sm[:, 0, :], sm[:, 1, :], sm[:, 2, :]
                    k.ts(nbeta, beta_tm, -1.0, ALU.mult)
                    k.act(eb, b_tm, AF.Exp)
                    k.tt(beb, eb, beta_tm, ALU.mult)
                    if glv <= 2.2:
                        continue
                    pkv = psb[1]
                    for h in range(4):
                        k.tr(pkv[0:64, h * 64:(h + 1) * 64], kn[:, h, cs_], idb64)
                    for h in range(4):
                        k.tr(pkv[0:64, 256 + h * 64:256 + (h + 1) * 64], vv[:, h, cs_], idb64)
                    if glv <= 2.3:
                        continue
                    k.copy(kvtm[:], pkv[0:64, 0:512], eng="act")
                    kt3 = kvtm[:, 0:256].rearrange("p (h d) -> p h d", d=64)
                    vt3 = kvtm[:, 256:512].rearrange("p (h d) -> p h d", d=64)
                    k.tt(y32[:, :, 0:64], vt3, beta_tm.unsqueeze(2).broadcast_to([64, 4, 64]), ALU.mult)
                    if glv <= 2.4:
                        continue
                    k.tt(y32[:, :, 64:128], kt3, beb.unsqueeze(2).broadcast_to([64, 4, 64]), ALU.mult)
                    if glv <= 2.45:
                        continue
                    k.tt(kdec[:], kt3, ekd_tm.unsqueeze(2).broadcast_to([64, 4, 64]), ALU.mult)
                    if glv <= 2.47:
                        if glv == 2.47 and pc == 0 and n < 16:
                            k.dma(self.dbgout[n, :, 0:512], y32[:].rearrange("p h c -> p (h c)"))
                            k.dma(self.dbgout[n, :, 512:640], tm1[:])
                            k.dma(self.dbgout[n, :, 640:768], tm2[:])
                        continue
                    k.copy(yb[:], y32[:], eng="act")
                    if glv <= 2:
                        continue
                    pD = ps[2]
                    k.mm(pD[0:64, 0:256], id64, maskS[:], start=True, stop=False)
                    def AB(h):
                        return AtH[0:2, h, cs_], BtH[0:2, h, cs_]
                    import os
                    hs_ = [int(c) for c in os.environ.get("GH", "0123")]
                    for h in hs_:
                        a_, b_ = AB(h)
                        k.mm(pD[0:64, h * 64:(h + 1) * 64], a_, b_, start=False, stop=True)
                    k.mm(pD[0:64, 256:512], id64, maskIT[:], start=True, stop=False)
                    for h in hs_:
                        a_, b_ = AB(h)
                        k.mm(pD[0:64, 256 + h * 64:256 + (h + 1) * 64], b_, a_, start=False, stop=True)
                    if glv <= 2.55:
                        continue
                    k.act(Ds[:], pD[0:64, 0:256], AF.Exp)
                    k.act(DTs[:], pD[0:64, 256:512], AF.Exp)
                    if glv <= 2.6:
                        continue
                    pKK = ps[3]
                    for h in range(4):
                        k.mm(pKK[0:64, h * 64:(h + 1) * 64], kn[:, h, cs_], kn[:, h, cs_])
                    for h in range(4):
                        k.mm(pKK[0:64, 256 + h * 64:256 + (h + 1) * 64], kn[:, h, cs_], qn[:, h, cs_])
                    k.tt(tmpN[:], pKK[0:64, 0:256], Ds[:], ALU.mult)
                    N0, P0 = Nb[0], Pb[0]
                    k.tt(N0[:], tmpN[:].rearrange("p (h j) -> p h j", j=64), nbeta.unsqueeze(2).broadcast_to([64, 4, 64]), ALU.mult)
                    k.tt(attnT[:].rearrange("p h i -> p (h i)"), pKK[0:64, 256:512], DTs[:], ALU.mult)
                    if glv <= 2.7:
                        continue
                    pP = psb[4]
                    for h in range(4):
                        k.tr(pP[0:64, h * 64:(h + 1) * 64], N0[:, h, :], idb64)
                    k.copy(P0[:].rearrange("p h i -> p (h i)"), pP[0:64, 0:256], eng="act")
                    if glv <= 3:
                        continue
                    cur = 0
                    for lev in range(6):
                        Nc, Pc = Nb[cur], Pb[cur]
                        pY = ps[6]
                        for h in range(4):
                            k.mm(pY[0:64, h * 128:(h + 1) * 128], Pc[:, h, :], yb[:, h, :])
                        if lev < 5:
                            Nn, Pn_ = Nb[1 - cur], Pb[1 - cur]
                            pN = ps[5]
                            for h in range(4):
                                k.mm(pN[0:64, h * 64:(h + 1) * 64], Pc[:, h, :], Nc[:, h, :])
                            for h in range(4):
                                k.mm(pN[0:64, 256 + h * 64:256 + (h + 1) * 64], Nc[:, h, :], Pc[:, h, :])
                        k.tt(y32[:].rearrange("p h c -> p (h c)"), y32[:].rearrange("p h c -> p (h c)"), pY[0:64, :], ALU.add)
                        k.copy(yb[:], y32[:], eng="act")
                        if lev < 5:
                            k.copy(Nn[:].rearrange("p h i -> p (h i)"), pN[0:64, 0:256], eng="act")
                            k.copy(Pn_[:].rearrange("p h i -> p (h i)"), pN[0:64, 256:512], eng="act")
                            cur = 1 - cur
                    if glv <= 4:
                        continue
                    pW = psb[4]
                    for h in range(4):
                        k.tr(pW[0:64, 256 + h * 64:256 + (h + 1) * 64], yb[:, h, 64:128], idb64)
                    k.copy(wT[:].rearrange("p h i -> p (h i)"), pW[0:64, 256:512], eng="act")
                    pU = ps[7]
                    for h in range(4):
                        k.mm(pU[0:64, h * 64:(h + 1) * 64], wT[:, h, :], Sb[:, h, :])
                    for h in range(4):
                        k.mm(pU[0:64, 256 + h * 64:256 + (h + 1) * 64], qn[:, h, cs_], Sb[:, h, :])
                    k.tt(ub[:], y32[:, :, 0:64], pU[0:64, 0:256].rearrange("p (h d) -> p h d", d=64), ALU.subtract)
                    pAU = ps[4]
                    for h in range(4):
                        k.mm(pAU[0:64, 256 + h * 64:256 + (h + 1) * 64], attnT[:, h, :], ub[:, h, :])
                    pSn = ps[1]
                    for h in range(4):
                        k.mm(pSn[0:64, 256 + h * 64:256 + (h + 1) * 64], kdec[:, h, :], ub[:, h, :])
                    k.tt(t1[:], pU[0:64, 256:512].rearrange("p (h d) -> p h d", d=64), eb.unsqueeze(2).broadcast_to([64, 4, 64]), ALU.mult)
                    k.tt(o32[:].rearrange("p h d -> p (h d)"), t1[:].rearrange("p h d -> p (h d)"), pAU[0:64, 256:512], ALU.add)
                    k.tt(S32[:], S32[:], Gs[:, n, :].unsqueeze(2).broadcast_to([64, 4, 64]), ALU.mult, eng="pool")
                    k.tt(S32[:].rearrange("p h d -> p (h d)"), S32[:].rearrange("p h d -> p (h d)"), pSn[0:64, 256:512], ALU.add)
                    k.copy(Sb[:], S32[:], eng="pool")
                    if glv <= 5:
                        continue
                    k.act(osq[:], o32[:], AF.Square)
                    k.red(ss[:, 0:4], osq[:], ALU.add)
                    k.ts(ss[:, 0:4], ss[:, 0:4], 1.0 / 64.0, ALU.mult, s2=1.0e-6, op1=ALU.add)
                    k.act(ss[:, 0:4], ss[:, 0:4], AF.Sqrt)
                    k.recip(ss[:, 4:8], ss[:, 0:4])
                    k.tt(o32[:], o32[:], ss[:, 4:8].unsqueeze(2).broadcast_to([64, 4, 64]), ALU.mult)
                    k.tt(on[:], o32[:], nw[:, l, :].unsqueeze(1).broadcast_to([64, 4, 64]), ALU.mult)
                    pOT = psb[0]
                    for c in range(2):
                        k.tr(pOT[:, 512 + c * 64:512 + (c + 1) * 64], on[:, 2 * c:2 * c + 2, :].rearrange("p h d -> p (h d)"), idb64)
                    k.act(sz[:], zT[:, :, cs_], AF.Silu)
                    jj = gn % 8
                    for c in range(2):
                        k.tt(oT[:, c, jj * 64:(jj + 1) * 64], pOT[:, 512 + c * 64:512 + (c + 1) * 64], sz[:, c, :], ALU.mult)
                    if jj == 7:
                        t0 = (gn - 7) * 64
                        k.dma(self.obrT[0, :, t0:t0 + 512].rearrange("(c p) t -> p c t", p=128), oT[:])
            k.barrier()

    for f in (phase_proj, attn_finalize, phase_fox, phase_merge, phase_mixer, phase_ret, phase_nsa, phase_gdn, phase_gdn_conv):
        setattr(Prog, f.__name__, f)


_mixer_methods()

def host_consts():
    c = {}
    sel = np.zeros((16, NE * 128), np.float32)
    for e in range(NE):
        sel[e, e * 128:(e + 1) * 128] = 1.0
    c["c_sel16"] = sel
    c["c_ident"] = np.eye(128, dtype=np.float32)
    kk = np.arange(128)[:, None, None]
    oo = np.arange(4)[None, :, None]
    qq = np.arange(512)[None, None, :]
    c["c_cmask"] = np.where(oo * 128 + kk <= qq, 0.0, NEG).astype(np.float32)
    gc = np.zeros((128, 8), np.float32)
    gm = np.zeros((128, 4), np.float32)
    for h in range(4):
        gc[32 * h, 0] = 1.0; gc[32 * h + 1, 1] = 1.0; gc[32 * h + 1, 2] = -1.0; gc[32 * h, 3] = 1.0
        gc[32 * h + 2, 4] = 1.0; gc[32 * h, 5] = 1.0
        gm[32 * h, h] = 1.0
    gc[:, 6] = 1.0e-6
    c["c_gcols"] = gc
    c["c_gmsk"] = gm
    c["c_mreset"] = np.ascontiguousarray(np.broadcast_to((np.arange(1024) % 64 != 0).astype(np.float32)[None, :], (128, 1024)))
    ii = np.arange(64)
    mS = np.where(ii[:, None] > ii[None, :], 0.0, NEG).astype(np.float32)
    mIT = np.where(ii[None, :] >= ii[:, None], 0.0, NEG).astype(np.float32)
    c["c_gmaskS"] = np.ascontiguousarray(np.tile(mS, (1, 4)))
    c["c_gmaskIT"] = np.ascontiguousarray(np.tile(mIT, (1, 4)))
    blk = np.zeros((128, 128), np.float32); blk[0:64, 0:64] = 1.0; blk[64:128, 64:128] = 1.0
    c["c_blk"] = blk
    nl = np.arange(128)[:, None, None]
    di = np.arange(5)[None, :, None]
    c["c_cmpmask"] = np.where(16 * nl + 31 - qq <= 512 * di, 0.0, NEG).astype(np.float32)
    c["c_bmask"] = np.where(oo * 128 + kk > qq, 0.0, NEG).astype(np.float32)
    jj = np.arange(64)
    E2 = np.zeros((64, 32, 128), np.float32)
    for kt in range(32):
        for m_ in range(128):
            E2[2 * kt + m_ // 64, kt, m_] = -NEG
    c["c_E2"] = E2
    n_all = np.arange(256)
    ov = np.clip(np.minimum(n_all[:, None] * 16 + 32, jj[None, :] * 64 + 64) - np.maximum(n_all[:, None] * 16, jj[None, :] * 64), 0, 32) / 32.0
    ov[255:] = 0.0
    c["c_ovl"] = np.ascontiguousarray(ov.reshape(2, 128, 64).transpose(1, 0, 2)).astype(np.float32)
    tpos = (np.arange(32)[None, :] * 128 + np.arange(128)[:, None])
    cur = (tpos // 64)[:, :, None]
    jb = jj[None, None, :]
    forced = (jb == 0) | (jb == cur) | (jb == cur - 1)
    causal = jb <= cur
    c["c_selA"] = (causal & ~forced).astype(np.float32)
    c["c_selB"] = (1.0e4 * forced - 1.0 * ((~causal) & (~forced))).astype(np.float32)
    half = 32
    invf = (10000.0 ** (-np.arange(half, dtype=np.float32) / half)).astype(np.float32)
    c["c_invf"] = np.ascontiguousarray(np.broadcast_to(invf[None, :], (128, 32))).astype(np.float32)
    lg = np.log1p(-(2.0 ** (-5.0 - np.arange(4, dtype=np.float64))))
    idx = np.arange(128, dtype=np.float64)
    rel = idx[None, :] - idx[:, None]
    dec = np.where(rel[None] >= 0, np.exp(np.maximum(rel[None], 0.0) * lg[:, None, None]), 0.0)
    c["c_decT"] = np.ascontiguousarray(dec.transpose(1, 0, 2)).astype(np.float32)
    xi = np.exp((idx[None, :] + 1.0) * lg[:, None])
    c["c_xiT"] = np.ascontiguousarray(np.broadcast_to(xi[None], (64, 4, 128))).astype(np.float32)
    zeta = np.exp((127.0 - idx[None, :]) * lg[:, None]) / 8.0
    c["c_zt"] = np.ascontiguousarray(zeta.T).astype(np.float32)
    cd = np.exp(128.0 * lg)
    c["c_cd"] = np.ascontiguousarray(np.broadcast_to(cd[None, :, None], (64, 4, 64))).astype(np.float32)
    return c


_O = dict(gq=0, gk=256, gv=512, ga=768, gb=772, gz=776, rq=1032, rk=1288, rv=1544, rg=1800, nq=2056, nkc=2312,
          nvc=2376, nks=2440, nvs=2504, nkw=2568, nvw=2632, ngate=2696, fq=2708, fk=2964, fv=3220, ff=3476)


def permute_w_in(w_in):
    out = np.zeros((w_in.shape[0], D, WIN_COLS), np.float32)

    def put(dst, name, width):
        out[:, :, dst:dst + width] = w_in[:, :, _O[name]:_O[name] + width]

    put(FM_GQ * 128, "gq", 256); put(FM_GK * 128, "gk", 256); put(FM_GV * 128, "gv", 256); put(FM_GZ * 128, "gz", 256)
    put(FM_RG * 128, "rg", 256); put(FM_NQ * 128, "nq", 256); put(FM_FQ * 128, "fq", 256); put(FM_FK * 128, "fk", 256)
    put(FM_NC * 128, "nkc", 64); put(FM_NC * 128 + 64, "nvc", 64)
    put(FM_NK * 128, "nks", 64); put(FM_NK * 128 + 64, "nkw", 64)
    for h in range(4):
        for r, nm in ((0, "ga"), (1, "ga"), (2, "gb")):
            out[:, :, FM_SM * 128 + 32 * h + r] = w_in[:, :, _O[nm] + h]
    put(FM_SM * 128 + 8, "ngate", 12); put(FM_SM * 128 + 20, "ff", 4)
    b = NFM * 128
    put(b + TM_RQ, "rq", 256); put(b + TM_RK, "rk", 256); put(b + TM_RV, "rv", 256); put(b + TM_FV, "fv", 256)
    put(b + TM_NVS, "nvs", 64); put(b + TM_NVW, "nvw", 64)
    return out


def pcol(v):
    v = np.asarray(v)
    sh = v.shape[:-1]
    n = v.shape[-1] // 128
    v = v.reshape(sh + (n, 128))
    v = np.moveaxis(v, -1, 0)
    return np.ascontiguousarray(v)


def prep_core(inp, seqs):
    S = len(seqs)
    m = {}
    m["xT"] = np.ascontiguousarray(np.transpose(inp["x"][seqs], (0, 2, 1)))
    m["cT"] = np.ascontiguousarray(np.transpose(pcol(inp["c"][seqs]), (0, 2, 1)))
    m["ada_w"] = np.ascontiguousarray(inp["ada_w"])
    m["ada_b"] = pcol(inp["ada_b"].reshape(DEPTH, 2, 3, D)).reshape(128, -1)
    m["ln_g"] = pcol(inp["ln_g"]).reshape(128, -1)
    m["ln_b"] = pcol(inp["ln_b"]).reshape(128, -1)
    m["router_w"] = np.ascontiguousarray(inp["router_w"].reshape(8, 128, NE).transpose(1, 0, 2))
    m["router_b"] = np.ascontiguousarray(inp["router_b"].reshape(1, NE))
    for n in ("exp_w1", "exp_w3", "exp_w2", "w_gate", "branch_proj", "w_out"):
        m[n] = np.ascontiguousarray(inp[n])
    m["w_in"] = permute_w_in(inp["w_in"])
    m["fox_fb"] = np.ascontiguousarray(inp["fox_f_bias"].T)
    m["gdn_convw"] = np.ascontiguousarray(inp["gdn_conv_w"].reshape(DEPTH, 4, 6, 128).transpose(3, 0, 2, 1))
    pA = np.zeros((128, DEPTH), np.float32); pDt = np.zeros((128, DEPTH), np.float32)
    for h in range(4):
        for r in (0, 1):
            pA[32 * h + r, :] = inp["gdn_a_log"][:, h]
            pDt[32 * h + r, :] = inp["gdn_dt_bias"][:, h]
    m["gdn_pA"] = pA
    m["gdn_pDt"] = pDt
    m["gdn_nw"] = np.ascontiguousarray(np.broadcast_to(inp["gdn_norm_w"][None], (64, DEPTH, 64))).astype(np.float32)
    pe = np.transpose(inp["nsa_cmp_pe"], (2, 0, 1))
    m["nsa_pe"] = np.ascontiguousarray(np.concatenate([pe, pe], axis=0))
    for n in ("nsa_ck_w1", "nsa_cv_w1", "nsa_ck_w2", "nsa_cv_w2"):
        m[n] = np.ascontiguousarray(inp[n])
    m["ret_gnw"] = pcol(inp["ret_gn_w"]).reshape(128, -1)
    m["posT"] = np.ascontiguousarray(inp["positions"][seqs].reshape(S, 32, 128).transpose(0, 2, 1)).astype(np.int32)
    return m


_CACHE = {}


def kernel(**inputs):
    inp = {k_: np.asarray(v) for k_, v in inputs.items()}
    ncores = 8
    S = inp["x"].shape[0] // ncores
    if "prog" not in _CACHE:
        p = Prog(nseq=S)
        p.build()
        _CACHE["prog"] = p
    p = _CACHE["prog"]
    consts = host_consts()
    in_maps = []
    for c in range(ncores):
        m = prep_core(inp, list(range(c * S, (c + 1) * S)))
        m.update(consts)
        in_maps.append({n: m[n] for n in p.inputs})
    res = run_bass_kernel_spmd(p.nc, in_maps, core_ids=list(range(ncores)))
    outs = [np.transpose(r["outT"], (0, 2, 1)) for r in res.results]
    return np.ascontiguousarray(np.concatenate(outs, axis=0)).astype(np.float32)
```

```python
import math
from contextlib import ExitStack
import numpy as np
import concourse.bass as bass
import concourse.mybir as mybir
from concourse.bass_utils import run_bass_kernel_spmd

F32 = mybir.dt.float32
BF16 = mybir.dt.bfloat16
I32 = mybir.dt.int32
ALU = mybir.AluOpType
AF = mybir.ActivationFunctionType
AX = mybir.AxisListType

D = 1024
T = 4096
DEPTH = 2
NE = 16
DE = 512
ALPHA = (2.0 * DEPTH) ** 0.25
LN_EPS = 1e-5
NEG = -30000.0


class K:
    NSLOT = 8

    def __init__(self, nc):
        self.nc = nc
        self.es = ExitStack()
        self.eng = {"pe": nc.tensor, "act": nc.scalar, "dve": nc.vector, "pool": nc.gpsimd, "sp": nc.sync}
        self.sem = {}
        self.cnt = {}
        for e in self.eng:
            self.sem[e] = self.es.enter_context(nc.semaphore("sem_" + e))
            self.cnt[e] = 0
        self.slots = {}
        self.slot_idx = {}
        for q in ("sp", "pool", "act"):
            self.slots[q] = [self.es.enter_context(nc.semaphore("dq_%s_%d" % (q, i))) for i in range(self.NSLOT)]
            self.slot_idx[q] = 0
        self.slot_val = {}
        self.seen = {e: {} for e in self.eng}
        self.lastw = {}
        self.readers = {}
        self.n_ins = 0

    @staticmethod
    def key(ap):
        if isinstance(ap, tuple):
            return ap[1]
        if ap is None or isinstance(ap, (int, float)):
            return None
        t = ap.tensor
        if str(t.space).lower().find("dram") >= 0 or type(t).__name__.startswith("DRam"):
            return None
        return t.name

    @staticmethod
    def raw(ap):
        return ap[0] if isinstance(ap, tuple) else ap

    def _need(self, eng, reads, writes):
        need = {}

        def add(dep):
            semname, sem, val, owner = dep
            if owner == eng and eng == "pe":
                return
            if need.get(semname, (None, -1))[1] < val:
                need[semname] = (sem, val)

        for r in reads:
            kk = self.key(r)
            if kk is None:
                continue
            if kk in self.lastw:
                add(self.lastw[kk])
        for w in writes:
            kk = self.key(w)
            if kk is None:
                continue
            if kk in self.lastw:
                add(self.lastw[kk])
            for dep in self.readers.get(kk, {}).values():
                if dep[3] == eng and dep[0].startswith("sem_"):
                    continue
                add(dep)
        return need

    def _emit_waits(self, eng, need):
        e = self.eng[eng]
        seen = self.seen[eng]
        for semname, (sem, val) in need.items():
            if seen.get(semname, -1) >= val:
                continue
            e.wait_ge(sem, val)
            seen[semname] = val
            self.n_ins += 1

    def _record(self, dep, reads, writes):
        for r in reads:
            kk = self.key(r)
            if kk is None:
                continue
            self.readers.setdefault(kk, {})[dep[0]] = dep
        for w in writes:
            kk = self.key(w)
            if kk is None:
                continue
            self.lastw[kk] = dep
            self.readers[kk] = {}

    def op(self, eng, fn, reads, writes):
        need = self._need(eng, reads, writes)
        self._emit_waits(eng, need)
        ins = fn(self.eng[eng])
        self.cnt[eng] += 1
        ins.then_inc(self.sem[eng], 1)
        self.n_ins += 1
        dep = ("sem_" + eng, self.sem[eng], self.cnt[eng], eng)
        self._record(dep, reads, writes)
        return ins

    def dma(self, out, in_, q="sp", **kw):
        reads, writes = [in_], [out]
        need = self._need(q, reads, writes)
        i = self.slot_idx[q] % self.NSLOT
        self.slot_idx[q] += 1
        semname = "dq_%s_%d" % (q, i)
        sem = self.slots[q][i]
        prev = self.slot_val.get((q, i), 0)
        if prev > 0:
            need[semname] = (sem, prev)
        self._emit_waits(q, need)
        ins = self.eng[q].dma_start(out=self.raw(out), in_=self.raw(in_), **kw)
        val = prev + 16
        ins.then_inc(sem, 16)
        self.slot_val[(q, i)] = val
        self.n_ins += 1
        dep = (semname, sem, val, "dma_" + q)
        self._record(dep, reads, writes)
        return ins

    def barrier(self):
        need = {}
        for e in self.eng:
            if self.cnt[e] > 0:
                need["sem_" + e] = (self.sem[e], self.cnt[e])
        for (q, i), v in self.slot_val.items():
            need["dq_%s_%d" % (q, i)] = (self.slots[q][i], v)
        for e in self.eng:
            nd = {k: v for k, v in need.items() if k != "sem_" + e}
            self._emit_waits(e, nd)
        self.lastw = {}
        self.readers = {}

    def mm(self, out, lhsT, rhs, start=True, stop=True):
        return self.op("pe", lambda e: e.matmul(self.raw(out), self.raw(lhsT), self.raw(rhs), start=start, stop=stop),
                       [lhsT, rhs], [out])

    def tr(self, out, in_, ident):
        return self.op("pe", lambda e: e.transpose(self.raw(out), self.raw(in_), self.raw(ident)), [in_, ident], [out])

    def act(self, out, in_, func, bias=None, scale=1.0, eng="act"):
        rd = [in_]
        kw = {}
        if bias is not None:
            kw["bias"] = self.raw(bias)
            rd.append(bias)
        if not isinstance(scale, (int, float)):
            rd.append(scale)
            kw["scale"] = self.raw(scale)
        else:
            kw["scale"] = float(scale)
        return self.op(eng, lambda e: e.activation(out=self.raw(out), in_=self.raw(in_), func=func, **kw), rd, [out])

    def ts(self, out, in0, s1, op0, s2=None, op1=None, eng="dve"):
        rd = [in0] + [s for s in (s1, s2) if s is not None and not isinstance(s, (int, float))]
        kw = {}
        if op1 is not None:
            kw["op1"] = op1
        return self.op(eng, lambda e: e.tensor_scalar(out=self.raw(out), in0=self.raw(in0), scalar1=self.raw(s1),
                                                      scalar2=self.raw(s2), op0=op0, **kw), rd, [out])

    def tt(self, out, in0, in1, op, eng="dve"):
        return self.op(eng, lambda e: e.tensor_tensor(out=self.raw(out), in0=self.raw(in0), in1=self.raw(in1), op=op),
                       [in0, in1], [out])

    def stt(self, out, in0, scalar, in1, op0, op1):
        rd = [in0, in1] + ([scalar] if not isinstance(scalar, (int, float)) else [])
        return self.op("dve", lambda e: e.scalar_tensor_tensor(out=self.raw(out), in0=self.raw(in0), scalar=self.raw(scalar),
                                                               in1=self.raw(in1), op0=op0, op1=op1), rd, [out])

    def copy(self, out, in_, eng="dve"):
        if eng == "act":
            return self.op("act", lambda e: e.copy(out=self.raw(out), in_=self.raw(in_)), [in_], [out])
        return self.op(eng, lambda e: e.tensor_copy(out=self.raw(out), in_=self.raw(in_)), [in_], [out])

    def memset(self, ap, val, eng="pool"):
        return self.op(eng, lambda e: e.memset(self.raw(ap), val), [], [ap])

    def red(self, out, in_, op, axis=AX.X):
        return self.op("dve", lambda e: e.tensor_reduce(out=self.raw(out), in_=self.raw(in_), axis=axis, op=op), [in_], [out])

    def recip(self, out, in_):
        return self.op("dve", lambda e: e.reciprocal(out=self.raw(out), in_=self.raw(in_)), [in_], [out])

    def sb(self, es, name, shape, dt):
        self.uid = getattr(self, "uid", 0) + 1
        return es.enter_context(self.nc.sbuf_tensor("%s_u%d" % (name, self.uid), shape, dt))


class Prog:
    def __init__(self, nseq=2, layers=(0, 1), do_mixer=True, do_moe=True, debug=(), branches=(0, 1, 2, 3)):
        self.nseq = nseq
        self.branches = tuple(branches)
        self.layers = tuple(layers)
        self.do_mixer = do_mixer
        self.do_moe = do_moe
        self.debug = set(debug)
        self.nc = bass.Bass("TRN2", target_bir_lowering=False)
        self.k = K(self.nc)
        self.inputs = {}
        self.outputs = {}

    def din(self, name, shape, dt=F32):
        t = self.nc.dram_tensor(name, list(shape), dt, kind="ExternalInput")
        self.inputs[name] = (tuple(shape), dt)
        return t.ap()

    def dout(self, name, shape, dt=F32):
        t = self.nc.dram_tensor(name, list(shape), dt, kind="ExternalOutput")
        self.outputs[name] = (tuple(shape), dt)
        return t.ap()

    def dscr(self, name, shape, dt=F32):
        if name in self.debug:
            return self.dout(name, shape, dt)
        return self.nc.dram_tensor(name, list(shape), dt, kind="Internal").ap()

    def build(self):
        nc, k = self.nc, self.k
        S = self.nseq
        self.xT = self.din("xT", [S, D, T])
        self.outT = self.dout("outT", [S, D, T])
        self.cT = self.din("cT", [128, 8, S])
        self.ada_w = self.din("ada_w", [DEPTH, 2, D, 3 * D])
        self.ada_b = self.din("ada_b", [128, DEPTH * 2 * 3 * 8])
        self.ln_g = self.din("ln_g", [128, DEPTH * 2 * 8])
        self.ln_b = self.din("ln_b", [128, DEPTH * 2 * 8])
        self.router_w = self.din("router_w", [128, 8, NE])
        self.router_b = self.din("router_b", [1, NE])
        self.exp_w1 = self.din("exp_w1", [DEPTH, NE, D, DE])
        self.exp_w3 = self.din("exp_w3", [DEPTH, NE, D, DE])
        self.exp_w2 = self.din("exp_w2", [DEPTH, NE, DE, D])
        self.c_sel16 = self.din("c_sel16", [16, NE * 128])
        self.c_ident = self.din("c_ident", [128, 128])
        self.w_in = self.din("w_in", [DEPTH, D, WIN_COLS])
        self.w_gate = self.din("w_gate", [DEPTH, 4, D, D])
        self.branch_proj = self.din("branch_proj", [DEPTH, 4, 256, D])
        self.w_out = self.din("w_out", [DEPTH, D, D])
        self.fox_fb = self.din("fox_fb", [4, DEPTH])
        self.c_cmask = self.din("c_cmask", [128, 4, 512])
        self.c_invf = self.din("c_invf", [128, 32])
        self.c_gcols = self.din("c_gcols", [128, 8])
        self.c_gmsk = self.din("c_gmsk", [128, 4])
        self.c_mreset = self.din("c_mreset", [128, 1024])
        self.c_gmaskS = self.din("c_gmaskS", [64, 256])
        self.c_gmaskIT = self.din("c_gmaskIT", [64, 256])
        self.c_blk = self.din("c_blk", [128, 128])
        self.gdn_convw = self.din("gdn_convw", [128, DEPTH, 6, 4])
        self.gdn_pA = self.din("gdn_pA", [128, DEPTH])
        self.gdn_pDt = self.din("gdn_pDt", [128, DEPTH])
        self.gdn_nw = self.din("gdn_nw", [64, DEPTH, 64])
        if getattr(self, "gdn_lvl", 9) == 2.47:
            self.dbgout = self.dout("dbgout", [16, 64, 768])
        self.gqn = self.dscr("gqn", [256, T], BF16)
        self.gkn = self.dscr("gkn", [256, T], BF16)
        self.gvs = self.dscr("gvs", [256, T], BF16)
        self.c_cmpmask = self.din("c_cmpmask", [128, 5, 512])
        self.c_bmask = self.din("c_bmask", [128, 4, 512])
        self.c_E2 = self.din("c_E2", [64, 32, 128])
        self.c_ovl = self.din("c_ovl", [128, 2, 64])
        self.c_selA = self.din("c_selA", [128, 32, 64])
        self.c_selB = self.din("c_selB", [128, 32, 64])
        self.nsa_pe = self.din("nsa_pe", [128, DEPTH, 32])
        self.nsa_ck_w1 = self.din("nsa_ck_w1", [DEPTH, 2048, 256])
        self.nsa_cv_w1 = self.din("nsa_cv_w1", [DEPTH, 2048, 256])
        self.nsa_ck_w2 = self.din("nsa_ck_w2", [DEPTH, 256, 64])
        self.nsa_cv_w2 = self.din("nsa_cv_w2", [DEPTH, 256, 64])
        self.gsD = self.dscr("gsD", [12, T], F32)
        self.c_decT = self.din("c_decT", [128, 4, 128])
        self.c_xiT = self.din("c_xiT", [64, 4, 128])
        self.c_zt = self.din("c_zt", [128, 4])
        self.c_cd = self.din("c_cd", [64, 4, 64])
        self.ret_gnw = self.din("ret_gnw", [128, DEPTH * 2])
        self.posT = self.din("posT", [S, 128, 32], I32)
        self.projF = self.dscr("projF", [(NFM - 1) * 128, T], BF16)
        self.projS = self.dscr("projS", [128, T], F32)
        self.projT = self.dscr("projT", [T, NTM], BF16)
        self.obrT = self.dscr("obrT", [4, 256, T], BF16)
        self.xa = self.dscr("xa", [S, D, T])
        self.xb = self.dscr("xb", [S, D, T])

        es = self.k.es
        self.ps = [es.enter_context(nc.psum_tensor("ps%d" % i, [128, 512], F32)) for i in range(8)]
        self.modv = k.sb(es, "modv", [128, DEPTH * 2 * 3 * 8 * S], F32)
        self.lng = k.sb(es, "lng", [128, DEPTH * 2 * 8], F32)
        self.lnb = k.sb(es, "lnb", [128, DEPTH * 2 * 8], F32)
        self.onesD = k.sb(es, "onesD", [128, 128], F32)
        self.epsc = k.sb(es, "epsc", [128, 1], F32)
        self.ident = k.sb(es, "ident", [128, 128], F32)
        self.identb = k.sb(es, "identb", [128, 128], BF16)
        k.memset(self.onesD[:], 1.0 / D)
        self.ones = k.sb(es, "ones", [128, 128], F32)
        self.onec = k.sb(es, "onec", [128, 1], F32)
        k.memset(self.ones[:], 1.0)
        k.memset(self.onec[:], 1.0)
        k.memset(self.epsc[:], LN_EPS)
        self.epsln = k.sb(es, "epsln", [128, 1], F32)
        k.memset(self.epsln[:], LN_EPS / (ALPHA * ALPHA))
        k.dma(self.lng[:], self.ln_g)
        k.dma(self.lnb[:], self.ln_b)
        k.dma(self.ident[:], self.c_ident)
        k.copy(self.identb[:], self.ident[:])

        self.phase_mod()
        cur = [self.xT[s] for s in range(S)]
        for l in self.layers:
            if self.do_mixer:
                nxt = [self.xa[s] for s in range(S)]
                for s in range(S):
                    self.phase_mixer(l, s, cur[s], nxt[s])
                cur = nxt
            if self.do_moe:
                last = (l == self.layers[-1])
                nxt = [self.outT[s] if last else self.xb[s] for s in range(S)]
                self.phase_moe(l, cur, nxt)
                cur = nxt
        k.barrier()
        es.close()
        return nc

    def mcol(self, l, sub, j, kc, s):
        i = ((((l * 2 + sub) * 3 + j) * 8 + kc) * self.nseq + s)
        return self.modv[:, i:i + 1]

    def lcol(self, t, l, sub, kc):
        i = (l * 2 + sub) * 8 + kc
        return t[:, i:i + 1]

    def phase_mod(self):
        nc, k, S = self.nc, self.k, self.nseq
        with ExitStack() as es:
            ct = k.sb(es, "pm_ct", [128, 8, S], F32)
            sc = k.sb(es, "pm_sc", [128, 8, S], F32)
            adab = k.sb(es, "pm_adab", [128, DEPTH * 2 * 3 * 8], F32)
            wsl = [k.sb(es, "pm_w%d" % i, [128, 8, 1024], F32) for i in range(2)]
            k.dma(ct[:], self.cT)
            k.dma(adab[:], self.ada_b)
            k.act(sc[:], ct[:], AF.Silu)
            i = 0
            for l in range(DEPTH):
                for sub in range(2):
                    for j in range(3):
                        w = wsl[i % 2]
                        i += 1
                        src = self.ada_w[l, sub, :, j * 1024:(j + 1) * 1024].rearrange("(kc p) f -> p kc f", p=128)
                        for h in range(2):
                            k.dma(w[:, h * 4:(h + 1) * 4, :], src[:, h * 4:(h + 1) * 4, :], q="sp" if h == 0 else "pool")
                        pst = self.ps[i % 2]
                        for cc in range(8):
                            for kc in range(8):
                                k.mm(pst[:, cc * S:(cc + 1) * S], w[:, kc, cc * 128:(cc + 1) * 128], sc[:, kc, :],
                                     start=(kc == 0), stop=(kc == 7))
                        base = ((l * 2 + sub) * 3 + j) * 8
                        for cc in range(8):
                            o = self.modv[:, (base + cc) * S:(base + cc + 1) * S]
                            if j == 2:
                                k.ts(o, pst[:, cc * S:(cc + 1) * S], adab[:, base + cc:base + cc + 1], ALU.add,
                                     s2=1.0 / ALPHA, op1=ALU.mult)
                            else:
                                k.ts(o, pst[:, cc * S:(cc + 1) * S], adab[:, base + cc:base + cc + 1], ALU.add,
                                     s2=(1.0 if j == 1 else 0.0), op1=ALU.add)
            k.barrier()

    def ln_tile(self, zb, sqb, outb, msb, vsb, l, sub, pM, pQ):
        k = self.k
        for kc in range(8):
            k.act(sqb[:, kc, :], zb[:, kc, :], AF.Square)
        for kc in range(8):
            k.mm(pM[:], self.onesD[:], zb[:, kc, :], start=(kc == 0), stop=(kc == 7))
        for kc in range(8):
            k.mm(pQ[:], self.onesD[:], sqb[:, kc, :], start=(kc == 0), stop=(kc == 7))
        k.copy(msb[:], pM[:], eng="act")
        k.act(vsb[:], pM[:], AF.Square)
        k.tt(vsb[:], pQ[:], vsb[:], ALU.subtract)
        k.act(vsb[:], vsb[:], AF.Sqrt, bias=self.epsln[:])
        k.recip(vsb[:], vsb[:])
        for kc in range(8):
            k.tt(zb[:, kc, :], zb[:, kc, :], msb[:], ALU.subtract, eng="pool")
            k.tt(zb[:, kc, :], zb[:, kc, :], vsb[:], ALU.mult)
            k.act(outb[:, kc, :], zb[:, kc, :], AF.Identity, bias=self.lcol(self.lnb, l, sub, kc),
                  scale=self.lcol(self.lng, l, sub, kc))

    def phase_moe(self, l, src, dst):
        nc, k, S = self.nc, self.k, self.nseq
        ST = 1024
        NT = ST // 512
        ps = self.ps
        with ExitStack() as es:
            hT = k.sb(es, "mo_hT", [128, 8, ST], BF16)
            cTt = k.sb(es, "mo_cT", [16, ST], F32)
            hT2 = k.sb(es, "mo_hT2", [128, 8, ST], BF16)
            cTt2 = k.sb(es, "mo_cT2", [16, ST], F32)
            rt4 = [k.sb(es, "mo_rt4_%d" % i, [128, 12, NE], F32) for i in range(4)]
            rs4 = [k.sb(es, "mo_rs4_%d" % i, [128, 16], F32) for i in range(4)]
            yacc = k.sb(es, "mo_yacc", [128, 8, ST], F32)
            w1b = [k.sb(es, "mo_w1_%d" % i, [128, 8, DE], BF16) for i in range(2)]
            w3b = [k.sb(es, "mo_w3_%d" % i, [128, 8, DE], BF16) for i in range(2)]
            w2b = [k.sb(es, "mo_w2_%d" % i, [128, 4, D], BF16) for i in range(2)]
            xb = k.sb(es, "mo_xb", [128, 8, 512], F32)
            zb = k.sb(es, "mo_zb", [128, 8, 512], F32)
            t1 = [k.sb(es, "mo_t1_%d" % i, [128, 512], F32) for i in range(2)]
            t2 = [k.sb(es, "mo_t2_%d" % i, [128, 512], F32) for i in range(2)]
            hid = [k.sb(es, "mo_hid%d" % i, [128, 4, 512], BF16) for i in range(2)]
            bcS = [k.sb(es, "mo_bc%d" % i, [128, 512], F32) for i in range(2)]
            msb = k.sb(es, "mo_msb", [128, 512], F32)
            vsb = k.sb(es, "mo_vsb", [128, 512], F32)
            rw = k.sb(es, "mo_rw", [128, 8, NE], F32)
            rb = k.sb(es, "mo_rb", [128, NE], F32)
            sel = k.sb(es, "mo_sel", [16, NE * 128], F32)
            rt = k.sb(es, "mo_rt", [128, 12, NE], F32)
            rs = k.sb(es, "mo_rs", [128, 16], F32)
            k.dma(rw[:], self.router_w)
            k.dma(rb[:], self.router_b.broadcast_to([128, NE]))
            k.dma(sel[:], self.c_sel16)

            def load_w(e, buf):
                k.dma(w1b[buf][:], self.exp_w1[l, e].rearrange("(kc p) f -> p kc f", p=128), q="pool")
                k.dma(w3b[buf][:], self.exp_w3[l, e].rearrange("(kc p) f -> p kc f", p=128), q="pool")
                k.dma(w2b[buf][:], self.exp_w2[l, e].rearrange("(fc p) d -> p fc d", p=128), q="pool")

            load_w(0, 0)
            load_w(1, 1)
            hTb = [hT, hT2]
            cTb = [cTt, cTt2]
            stiles = [(s, st) for s in range(S) for st in range(T // ST)]

            def emit_prologue(idx):
                s, st = stiles[idx]
                hT_, cT_ = hTb[idx % 2], cTb[idx % 2]
                for tt in range(NT):
                    t0 = st * ST + tt * 512
                    k.dma(xb[:], src[s][:, t0:t0 + 512].rearrange("(kc p) t -> p kc t", p=128))
                    for kc in range(8):
                        k.ts(zb[:, kc, :], xb[:, kc, :], self.mcol(l, 1, 1, kc, s), ALU.mult,
                             s2=self.mcol(l, 1, 0, kc, s), op1=ALU.add)
                        k.copy(hT_[:, kc, tt * 512:(tt + 1) * 512], zb[:, kc, :], eng="pool")
                    pr = ps[0]
                    for q4 in range(4):
                        for kc in range(8):
                            k.mm(pr[:, q4 * 16:(q4 + 1) * 16], zb[:, kc, q4 * 128:(q4 + 1) * 128], rw[:, kc, :],
                                 start=(kc == 0), stop=(kc == 7))
                    pt = ps[0]
                    for q4 in range(4):
                        self.route(pr[:, q4 * 16:(q4 + 1) * 16], rb, rt4[q4], rs4[q4])
                    for q4 in range(4):
                        k.tr(pt[0:16, q4 * 128:(q4 + 1) * 128], rt4[q4][:, 0, :], self.ident[:])
                    k.copy(cT_[:, tt * 512:(tt + 1) * 512], pt[0:16, :], eng="act")

            def emit_epilogue(idx):
                s, st = stiles[idx]
                for tt in range(NT):
                    t0 = st * ST + tt * 512
                    k.dma(xb[:], src[s][:, t0:t0 + 512].rearrange("(kc p) t -> p kc t", p=128))
                    for kc in range(8):
                        k.stt(zb[:, kc, :], yacc[:, kc, tt * 512:(tt + 1) * 512], self.mcol(l, 1, 2, kc, s),
                              xb[:, kc, :], ALU.mult, ALU.add)
                    self.ln_tile(zb, xb, xb, msb, vsb, l, 1, ps[6], ps[7])
                    k.dma(dst[s][:, t0:t0 + 512].rearrange("(kc p) t -> p kc t", p=128), xb[:])

            units = [(e, tt) for e in range(NE) for tt in range(NT)]

            def emit_H(idx, u):
                e, tt = units[u]
                buf = e % 2
                hT_, cT_ = hTb[idx % 2], cTb[idx % 2]
                tok = slice(tt * 512, (tt + 1) * 512)
                j = u % 2
                k.mm(ps[0][:], sel[:, e * 128:(e + 1) * 128], cT_[:, tok])
                k.copy(bcS[j][:], ps[0][:], eng="act")
                for fc in range(4):
                    jj = fc % 2
                    p1, p3 = ps[1 + jj], ps[3 + jj]
                    for kc in range(8):
                        k.mm(p1[:], w1b[buf][:, kc, fc * 128:(fc + 1) * 128], hT_[:, kc, tok],
                             start=(kc == 0), stop=(kc == 7))
                    for kc in range(8):
                        k.mm(p3[:], w3b[buf][:, kc, fc * 128:(fc + 1) * 128], hT_[:, kc, tok],
                             start=(kc == 0), stop=(kc == 7))
                    k.act(t1[jj][:], p1[:], AF.Silu)
                    k.tt(t2[jj][:], t1[jj][:], bcS[j][:], ALU.mult, eng="pool")
                    k.tt(hid[j][:, fc, :], t2[jj][:], p3[:], ALU.mult)

            def emit_Y(idx, u):
                e, tt = units[u]
                buf = e % 2
                tok = slice(tt * 512, (tt + 1) * 512)
                j = u % 2
                for dc in range(8):
                    py = ps[5 + (u * 8 + dc) % 3]
                    for fc in range(4):
                        k.mm(py[:], w2b[buf][:, fc, dc * 128:(dc + 1) * 128], hid[j][:, fc, :],
                             start=(fc == 0), stop=(fc == 3))
                    if e == 0:
                        k.copy(yacc[:, dc, tok], py[:])
                    else:
                        k.tt(yacc[:, dc, tok], yacc[:, dc, tok], py[:], ALU.add)

            emit_prologue(0)
            for idx in range(len(stiles)):
                last_st = (idx == len(stiles) - 1)
                emit_H(idx, 0)
                for u in range(len(units)):
                    if u + 1 < len(units):
                        emit_H(idx, u + 1)
                    emit_Y(idx, u)
                    e_done, tt_done = units[u]
                    if tt_done == NT - 1:
                        if not (last_st and e_done + 2 >= NE):
                            load_w((e_done + 2) % NE, e_done % 2)
                    if u == len(units) // 2 and not last_st:
                        emit_prologue(idx + 1)
                emit_epilogue(idx)
            k.barrier()

    def route(self, logits, rb, rt, rs):
        k = self.k
        s_ = rt[:, 1, :]
        a = rt[:, 2, :]
        k.act(s_, logits, AF.Sigmoid)
        k.tt(a, s_, rb[:], ALU.add)
        a3 = a.rearrange("p (g e) -> p g e", e=4)
        m1 = rs[:, 0:4]
        m2 = rs[:, 4:8]
        k.red(m1, a3, ALU.max)
        oh = rt[:, 3, :].rearrange("p (g e) -> p g e", e=4)
        k.tt(oh, a3, m1.unsqueeze(2).broadcast_to([128, 4, 4]), ALU.is_equal)
        a2 = rt[:, 4, :].rearrange("p (g e) -> p g e", e=4)
        k.stt(a2, oh, -1.0e9, a3, ALU.mult, ALU.add)
        k.red(m2, a2, ALU.max)
        gs = rs[:, 8:12]
        k.tt(gs, m1, m2, ALU.add)
        gm = rs[:, 12:13]
        k.red(gm, gs, ALU.max)
        gse = rt[:, 5, 0:4]
        k.ts(gse, gs, gm, ALU.is_equal)
        selm = rt[:, 6, :].rearrange("p (g e) -> p g e", e=4)
        k.tt(selm, a3, m2.unsqueeze(2).broadcast_to([128, 4, 4]), ALU.is_ge)
        k.tt(selm, selm, gse.unsqueeze(2).broadcast_to([128, 4, 4]), ALU.mult)
        w = rt[:, 7, :]
        k.tt(w, rt[:, 6, :], s_, ALU.mult)
        ws = rs[:, 13:14]
        k.red(ws, w, ALU.add)
        k.recip(rs[:, 14:15], ws)
        k.ts(rt[:, 0, :], w, rs[:, 14:15], ALU.mult)


FM_GQ, FM_GK, FM_GV, FM_GZ, FM_RG, FM_NQ, FM_FQ, FM_FK, FM_NC, FM_NK, FM_SM = 0, 2, 4, 6, 8, 10, 12, 14, 16, 17, 18
NFM = 19
TM_RQ, TM_RK, TM_RV, TM_FV, TM_NVS, TM_NVW = 0, 256, 512, 768, 1024, 1088
NTM = 1152
WIN_COLS = NFM * 128 + NTM


def _mixer_methods():
    def phase_proj(self, l, s, src):
        nc, k = self.nc, self.k
        ps = self.ps
        with ExitStack() as es:
            win = k.sb(es, "pj_win", [128, 8, WIN_COLS], BF16)
            xb = [k.sb(es, "pj_xb%d" % i, [128, 8, 512], F32) for i in range(2)]
            hT = [k.sb(es, "pj_hT%d" % i, [128, 8, 512], BF16) for i in range(2)]
            stF = [k.sb(es, "pj_stF%d" % i, [128, 512], BF16) for i in range(4)]
            stS = k.sb(es, "pj_stS", [128, 512], F32)
            stT = [k.sb(es, "pj_stT%d" % i, [128, NTM], BF16) for i in range(2)]
            for kc in range(8):
                k.dma(win[:, kc, :], self.w_in[l, kc * 128:(kc + 1) * 128, :], q="pool")
            NTT = T // 512
            k.dma(xb[0][:], src[:, 0:512].rearrange("(kc p) t -> p kc t", p=128))
            ev = 0
            for tt in range(NTT):
                t0 = tt * 512
                b = tt % 2
                if tt + 1 < NTT:
                    k.dma(xb[1 - b][:], src[:, t0 + 512:t0 + 1024].rearrange("(kc p) t -> p kc t", p=128))
                for kc in range(8):
                    k.ts(hT[b][:, kc, :], xb[b][:, kc, :], self.mcol(l, 0, 1, kc, s), ALU.mult,
                         s2=self.mcol(l, 0, 0, kc, s), op1=ALU.add)
                for ch in range(NFM):
                    pp = ps[ch % 4]
                    for kc in range(8):
                        k.mm(pp[:], win[:, kc, ch * 128:(ch + 1) * 128], hT[b][:, kc, :], start=(kc == 0), stop=(kc == 7))
                    if ch == FM_SM:
                        k.copy(stS[:], pp[:], eng="act")
                        k.dma(self.projS[:, t0:t0 + 512], stS[:])
                    else:
                        st = stF[ev % 4]
                        k.copy(st[:], pp[:], eng=("act" if ev % 2 == 0 else "dve"))
                        ev += 1
                        k.dma(self.projF[ch * 128:(ch + 1) * 128, t0:t0 + 512], st[:])
                for q4 in range(4):
                    st = stT[q4 % 2]
                    for gi, (c0, cw) in enumerate(((0, 512), (512, 512), (1024, 128))):
                        pp = ps[4 + (q4 * 3 + gi) % 4]
                        for kc in range(8):
                            k.mm(pp[:, 0:cw], hT[b][:, kc, q4 * 128:(q4 + 1) * 128],
                                 win[:, kc, NFM * 128 + c0:NFM * 128 + c0 + cw], start=(kc == 0), stop=(kc == 7))
                        k.copy(st[:, c0:c0 + cw], pp[:, 0:cw], eng=("act" if gi % 2 == 0 else "dve"))
                    k.dma(self.projT[t0 + q4 * 128:t0 + (q4 + 1) * 128, :], st[:])
            k.barrier()

    def attn_finalize(self, pO, rr, pB, osb, outsb, gate_row=None):
        k = self.k
        k.ts(rr[64:65, :], pO[64:65, :], 1e-30, ALU.max)
        k.recip(rr[64:65, :], rr[64:65, :])
        if gate_row is not None:
            k.tt(rr[64:65, :], rr[64:65, :], gate_row, ALU.mult)
        k.mm(pB[0:64, :], self.ones[64:65, 0:64], rr[64:65, :])
        k.copy(osb[0:64, :], pO[0:64, :], eng="act")
        k.tt(outsb, osb[0:64, :], pB[0:64, :], ALU.mult)

    def run_attn_loops(self, loops, look=2):
        flat = []
        for li, L in enumerate(loops):
            for j in range(L["n"]):
                flat.append((li, j))
        n = len(flat)
        pend = []
        for t in range(n + look):
            if t < n:
                li, j = flat[t]
                loops[li]["qk"](j, t)
            g = t - look
            if g >= 0:
                li, j = flat[g]
                loops[li]["pv"](j, g)
                if j == loops[li]["n"] - 1:
                    pend.append(loops[li]["fin"])
                elif j == 1 and pend:
                    for f in pend:
                        f()
                    pend.clear()
        for f in pend:
            f()

    def phase_fox(self, l, s):
        nc, k = self.nc, self.k
        ps = self.ps
        with ExitStack() as es:
            ff = k.sb(es, "fx_ff", [4, T], F32)
            cs = k.sb(es, "fx_cs", [4, T], F32)
            fb = k.sb(es, "fx_fb", [4, DEPTH], F32)
            nfb = k.sb(es, "fx_nfb", [4, 1], F32)
            ckT = k.sb(es, "fx_ckT", [128, 32, 4], F32)
            rhsd = k.sb(es, "fx_rhsd", [4, 8, 4], F32)
            crefB = k.sb(es, "fx_cref", [128, 8, 4], F32)
            biasall = k.sb(es, "fx_bias", [128, 8, 4, 32], F32)
            qT2 = k.sb(es, "fx_qT", [128, T], BF16)
            kT2 = k.sb(es, "fx_kT", [128, T], BF16)
            vaug = k.sb(es, "fx_v", [128, 32, 4, 128], BF16)
            kTz = [k.sb(es, "fx_kTz%d" % i, [128, T], BF16) for i in range(2)]
            cm = k.sb(es, "fx_cm", [128, 4, 512], BF16)
            Pt = [k.sb(es, "fx_P%d" % i, [128, 512], BF16) for i in range(3)]
            rr = k.sb(es, "fx_rr", [65, 512], F32)
            osb = k.sb(es, "fx_osb", [64, 512], F32)
            outsb = [k.sb(es, "fx_out%d" % i, [64, 512], BF16) for i in range(2)]
            k.dma(cm[:], self.c_cmask, q="pool")
            k.dma(ff[:], self.projS[20:24, :])
            k.dma(fb[:], self.fox_fb)
            k.ts(nfb[:], fb[0:4, l:l + 1], -1.0, ALU.mult)
            k.act(ff[:], ff[:], AF.Exp, bias=nfb[:], scale=-1.0)
            k.act(ff[:], ff[:], AF.Ln, bias=self.onec[0:4, :])
            k.op("dve", lambda e: e.tensor_tensor_scan(out=cs[:], data0=ff[:], data1=ff[:], initial=0.0,
                                                        op0=ALU.add, op1=ALU.max), [ff[:]], [cs[:]])
            pt = ps[0]
            for j in range(32):
                k.tr(pt[:, j * 4:(j + 1) * 4], cs[0:4, j * 128:(j + 1) * 128], self.ident[0:4, 0:4])
            k.copy(ckT[:].rearrange("p a b -> p (a b)"), pt[:, 0:128])
            mids = cs[0:4, :].rearrange("p (i t) -> p i t", t=512)[:, :, 255:256]
            k.tt(rhsd[:], mids.broadcast_to([4, 8, 4]), self.ident[0:4, 0:4].unsqueeze(1).broadcast_to([4, 8, 4]), ALU.mult)
            k.mm(ps[1][:, 0:32], self.ones[0:4, :], rhsd[:].rearrange("p a b -> p (a b)"))
            k.copy(crefB[:].rearrange("p a b -> p (a b)"), ps[1][:, 0:32])
            for i in range(8):
                for h in range(4):
                    k.ts(biasall[:, i, h, :], ckT[:, :, h], crefB[:, i, h:h + 1], ALU.subtract)
            k.memset(vaug[:, :, :, 64:128], 0.0)
            k.memset(vaug[:, :, :, 64:65], 1.0)
            k.memset(kTz[0][64:128, :], 0.0)
            k.memset(kTz[1][0:64, :], 0.0)
            for h in range(4):
                k.dma(vaug[:, :, h, 0:64],
                      self.projT[:, TM_FV + h * 64:TM_FV + (h + 1) * 64].rearrange("(j p) d -> p j d", p=128))
            ob = 0
            for hp in range(2):
                k.dma(qT2[:], self.projF[(FM_FQ + hp) * 128:(FM_FQ + hp + 1) * 128, :])
                k.dma(kT2[:], self.projF[(FM_FK + hp) * 128:(FM_FK + hp + 1) * 128, :])
                k.copy(kTz[0][0:64, :], kT2[0:64, :], eng="pool")
                k.copy(kTz[1][64:128, :], kT2[64:128, :], eng="pool")
                loops = []
                for h2 in range(2):
                    hh = hp * 2 + h2
                    po = 64 * h2
                    for i in range(8):
                        njt = 4 * (i + 1)
                        pO = ps[4 + len(loops) % 2]

                        def qk(j, t, i=i, hh=hh, po=po):
                            pS = ps[1 + t % 3]
                            diag = j >= 4 * i
                            k.mm(pS[:], kTz[po // 64][:, j * 128:(j + 1) * 128], qT2[:, i * 512:(i + 1) * 512],
                                 start=True, stop=(not diag))
                            if diag:
                                k.mm(pS[:], self.identb[:], cm[:, j - 4 * i, :], start=False, stop=True)
                            k.act(Pt[t % 3][:], pS[:], AF.Exp, bias=biasall[:, i, hh, j:j + 1], scale=0.125)

                        def pv(j, t, hh=hh, pO=pO, njt=njt):
                            k.mm(pO[:, :], vaug[:, j, hh, :], Pt[t % 3][:], start=(j == 0), stop=(j == njt - 1))

                        def fin(pO=pO, i=i, hh=hh, o=outsb[ob % 2]):
                            self.attn_finalize(pO, rr, ps[6], osb, o[:])
                            k.dma(self.obrT[3, hh * 64:(hh + 1) * 64, i * 512:(i + 1) * 512], o[:])
                        ob += 1
                        loops.append(dict(n=njt, qk=qk, pv=pv, fin=fin))
                self.run_attn_loops(loops)
            k.barrier()

    def phase_merge(self, l, s, src, dst):
        nc, k = self.nc, self.k
        ps = self.ps
        brs = self.branches
        with ExitStack() as es:
            wg = k.sb(es, "mg_wg", [128, 4, 8, D], BF16)
            bp = k.sb(es, "mg_bp", [128, 4, 2, D], BF16)
            wo = k.sb(es, "mg_wo", [128, 8, D], BF16)
            xb = k.sb(es, "mg_xb", [128, 8, 512], F32)
            zb = k.sb(es, "mg_zb", [128, 8, 512], F32)
            hT = k.sb(es, "mg_hT", [128, 8, 512], BF16)
            oTt = k.sb(es, "mg_oT", [128, 4, 2, 512], BF16)
            sig = [k.sb(es, "mg_sig%d" % i, [128, 512], F32) for i in range(2)]
            tmp = [k.sb(es, "mg_tmp%d" % i, [128, 512], F32) for i in range(2)]
            mer = [k.sb(es, "mg_mer%d" % i, [128, 512], F32) for i in range(2)]
            merged = k.sb(es, "mg_merged", [128, 8, 512], BF16)
            msb = k.sb(es, "mg_msb", [128, 512], F32)
            vsb = k.sb(es, "mg_vsb", [128, 512], F32)
            for br in range(4):
                for half in range(2):
                    k.dma(wg[:, br, half * 4:(half + 1) * 4, :],
                          self.w_gate[l, br, half * 512:(half + 1) * 512, :].rearrange("(kc p) f -> p kc f", p=128), q="pool")
                k.dma(bp[:, br, :, :], self.branch_proj[l, br].rearrange("(c p) f -> p c f", p=128), q="pool")
            k.dma(wo[:], self.w_out[l].rearrange("(kc p) f -> p kc f", p=128), q="pool")
            cnt = 0
            for tt in range(T // 512):
                t0 = tt * 512
                k.dma(xb[:], src[:, t0:t0 + 512].rearrange("(kc p) t -> p kc t", p=128))
                for br in brs:
                    k.dma(oTt[:, br, :, :], self.obrT[br, :, t0:t0 + 512].rearrange("(c p) t -> p c t", p=128))
                for kc in range(8):
                    k.ts(hT[:, kc, :], xb[:, kc, :], self.mcol(l, 0, 1, kc, s), ALU.mult,
                         s2=self.mcol(l, 0, 0, kc, s), op1=ALU.add)
                for dc in range(8):
                    m = mer[dc % 2]
                    for bi, br in enumerate(brs):
                        pG = ps[cnt % 2]
                        pBp = ps[2 + cnt % 2]
                        sg = sig[cnt % 2]
                        cnt += 1
                        for kc in range(8):
                            k.mm(pG[:], wg[:, br, kc, dc * 128:(dc + 1) * 128], hT[:, kc, :], start=(kc == 0), stop=(kc == 7))
                        for c in range(2):
                            k.mm(pBp[:], bp[:, br, c, dc * 128:(dc + 1) * 128], oTt[:, br, c, :], start=(c == 0), stop=(c == 1))
                        k.act(sg[:], pG[:], AF.Sigmoid)
                        if bi == 0:
                            k.tt(m[:], sg[:], pBp[:], ALU.mult)
                        else:
                            tp = tmp[bi % 2]
                            k.tt(tp[:], sg[:], pBp[:], ALU.mult)
                            k.tt(m[:], m[:], tp[:], ALU.add, eng="pool")
                    k.copy(merged[:, dc, :], m[:], eng="pool")
                for d2 in range(8):
                    pY = ps[4 + d2 % 2]
                    for dc in range(8):
                        k.mm(pY[:], wo[:, dc, d2 * 128:(d2 + 1) * 128], merged[:, dc, :], start=(dc == 0), stop=(dc == 7))
                    k.stt(zb[:, d2, :], pY[:], self.mcol(l, 0, 2, d2, s), xb[:, d2, :], ALU.mult, ALU.add)
                self.ln_tile(zb, xb, xb, msb, vsb, l, 0, ps[6], ps[7])
                k.dma(dst[:, t0:t0 + 512].rearrange("(kc p) t -> p kc t", p=128), xb[:])
            k.barrier()

    def phase_mixer(self, l, s, src, dst):
        if getattr(self, "probe_conv", 0):
            self.phase_gdn_conv(l, s)
            return
        self.phase_proj(l, s, src)
        if 0 in self.branches:
            self.phase_gdn(l, s)
        if 1 in self.branches:
            self.phase_ret(l, s)
        if 2 in self.branches:
            self.phase_nsa(l, s)
        if 3 in self.branches:
            self.phase_fox(l, s)
        self.phase_merge(l, s, src, dst)

    def phase_ret(self, l, s):
        nc, k = self.nc, self.k
        ps = self.ps
        psb = [p_[:].bitcast(BF16) for p_ in ps]
        TWO_PI = 2.0 * math.pi
        with ExitStack() as es:
            invf = k.sb(es, "rt_invf", [128, 32], F32)
            decT = k.sb(es, "rt_decT", [128, 4, 128], F32)
            xiT = k.sb(es, "rt_xiT", [64, 4, 128], F32)
            zt = k.sb(es, "rt_zt", [128, 4], F32)
            cdt = k.sb(es, "rt_cd", [64, 4, 64], F32)
            gnw = k.sb(es, "rt_gnw", [128, DEPTH * 2], F32)
            posi = k.sb(es, "rt_posi", [128, 32], I32)
            posf = k.sb(es, "rt_posf", [128, 32], F32)
            ang = k.sb(es, "rt_ang", [128, 32, 32], F32)
            tmpa = k.sb(es, "rt_tmpa", [128, 32, 32], F32)
            tmpi = k.sb(es, "rt_tmpi", [128, 32, 32], I32)
            cosT = k.sb(es, "rt_cos", [128, 32, 32], F32)
            sinT = k.sb(es, "rt_sin", [128, 32, 32], F32)
            tq = [k.sb(es, "rt_tq%d" % i, [128, 768], BF16) for i in range(2)]
            gt = [k.sb(es, "rt_gt%d" % i, [128, 2, 128], BF16) for i in range(2)]
            sg = k.sb(es, "rt_sg", [128, 2, 128], F32)
            ra = [k.sb(es, "rt_ra%d" % i, [128, 4, 32], F32) for i in range(4)]
            qr = k.sb(es, "rt_qr", [128, 4, 64], BF16)
            kr = k.sb(es, "rt_kr", [128, 4, 64], BF16)
            kz = k.sb(es, "rt_kz", [128, 4, 64], BF16)
            qrT = k.sb(es, "rt_qrT", [64, 4, 128], BF16)
            qxT = k.sb(es, "rt_qxT", [64, 4, 128], BF16)
            krT = k.sb(es, "rt_krT", [64, 4, 128], BF16)
            Sd = [k.sb(es, "rt_Sd%d" % i, [128, 128], BF16) for i in range(2)]
            st32 = k.sb(es, "rt_st32", [64, 4, 64], F32)
            stb = k.sb(es, "rt_stb", [64, 4, 64], BF16)
            o32 = k.sb(es, "rt_o32", [128, 4, 64], F32)
            st6 = k.sb(es, "rt_st6", [128, 4, 6], F32)
            mv = k.sb(es, "rt_mv", [128, 4, 2], F32)
            rstd = k.sb(es, "rt_rstd", [128, 4], F32)
            on = k.sb(es, "rt_on", [128, 4, 64], BF16)
            oT = k.sb(es, "rt_oT", [128, 2, 512], BF16)
            k.dma(invf[:], self.c_invf)
            k.dma(decT[:], self.c_decT)
            k.dma(xiT[:], self.c_xiT)
            k.dma(zt[:], self.c_zt)
            k.dma(cdt[:], self.c_cd)
            k.dma(gnw[:], self.ret_gnw)
            k.dma(posi[:], self.posT[s])
            k.copy(posf[:], posi[:])
            k.tt(ang[:], posf[:].unsqueeze(2).broadcast_to([128, 32, 32]), invf[:].unsqueeze(1).broadcast_to([128, 32, 32]), ALU.mult)

            def sin_of(dst, shift):
                k.ts(tmpa[:], ang[:], 1.0 / TWO_PI, ALU.mult, s2=shift / TWO_PI + 0.5, op1=ALU.add)
                k.copy(tmpi[:], tmpa[:])
                k.copy(tmpa[:], tmpi[:])
                k.stt(tmpa[:], tmpa[:], -TWO_PI, ang[:], ALU.mult, ALU.add)
                if shift != 0.0:
                    k.ts(tmpa[:], tmpa[:], shift, ALU.add)
                k.ts(dst, tmpa[:], -math.pi, ALU.is_lt, s2=TWO_PI, op1=ALU.mult)
                k.tt(tmpa[:], tmpa[:], dst, ALU.add)
                k.ts(dst, tmpa[:], math.pi, ALU.is_gt, s2=-TWO_PI, op1=ALU.mult)
                k.tt(tmpa[:], tmpa[:], dst, ALU.add)
                k.ts(tmpa[:], tmpa[:], math.pi, ALU.min, s2=-math.pi, op1=ALU.max)
                k.act(dst, tmpa[:], AF.Sin)

            sin_of(sinT[:], 0.0)
            sin_of(cosT[:], math.pi / 2.0)
            k.memset(st32[:], 0.0)
            k.memset(stb[:], 0.0)

            def rope(dst, src4, j):
                cb = cosT[:, j, :].unsqueeze(1).broadcast_to([128, 4, 32])
                sb_ = sinT[:, j, :].unsqueeze(1).broadcast_to([128, 4, 32])
                x1 = src4[:, :, 0:32]
                x2 = src4[:, :, 32:64]
                k.tt(ra[0][:], x1, cb, ALU.mult, eng="pool")
                k.tt(ra[1][:], x2, sb_, ALU.mult, eng="pool")
                k.tt(dst[:, :, 0:32], ra[0][:], ra[1][:], ALU.subtract)
                k.tt(ra[2][:], x1, sb_, ALU.mult, eng="pool")
                k.tt(ra[3][:], x2, cb, ALU.mult, eng="pool")
                k.tt(dst[:, :, 32:64], ra[2][:], ra[3][:], ALU.add)

            NJ = T // 128
            k.dma(tq[0][:], self.projT[0:128, 0:768])
            for j in range(NJ):
                b = j % 2
                if j + 1 < NJ:
                    k.dma(tq[1 - b][:], self.projT[(j + 1) * 128:(j + 2) * 128, 0:768])
                k.dma(gt[b][:], self.projF[FM_RG * 128:(FM_RG + 2) * 128, j * 128:(j + 1) * 128].rearrange("(c p) t -> p c t", p=128))
                q4 = tq[b][:, 0:256].rearrange("p (h d) -> p h d", d=64)
                k4 = tq[b][:, 256:512].rearrange("p (h d) -> p h d", d=64)
                v4 = tq[b][:, 512:768].rearrange("p (h d) -> p h d", d=64)
                rope(qr, q4, j)
                rope(kr, k4, j)
                k.tt(kz[:], kr[:], zt[:].unsqueeze(2).broadcast_to([128, 4, 64]), ALU.mult, eng="pool")
                pq = psb[0]
                pk = psb[1]
                for h in range(4):
                    k.tr(pq[0:64, h * 128:(h + 1) * 128], qr[:, h, :], self.identb[:])
                for h in range(4):
                    k.tr(pk[0:64, h * 128:(h + 1) * 128], kr[:, h, :], self.identb[:])
                k.copy(qrT[:].rearrange("p h t -> p (h t)"), pq[0:64, 0:512], eng="act")
                k.tt(qxT[:].rearrange("p h t -> p (h t)"), pq[0:64, 0:512], xiT[:].rearrange("p h t -> p (h t)"), ALU.mult)
                k.act(krT[:].rearrange("p h t -> p (h t)"), pk[0:64, 0:512], AF.Copy, scale=0.125)
                pO = ps[2 + b]
                pKV = ps[4]
                for h in range(4):
                    pS = ps[5 + h % 2]
                    k.mm(pS[:, 0:128], krT[:, h, :], qrT[:, h, :])
                    sd = Sd[h % 2]
                    k.tt(sd[:], pS[:, 0:128], decT[:, h, :], ALU.mult)
                    k.mm(pO[:, h * 64:(h + 1) * 64], sd[:], v4[:, h, :], start=True, stop=False)
                    k.mm(pO[:, h * 64:(h + 1) * 64], qxT[:, h, :], stb[:, h, :], start=False, stop=True)
                    k.mm(pKV[0:64, h * 64:(h + 1) * 64], kz[:, h, :], v4[:, h, :])
                k.tt(st32[:], st32[:], cdt[:], ALU.mult, eng="pool")
                k.tt(st32[:].rearrange("p h d -> p (h d)"), st32[:].rearrange("p h d -> p (h d)"), pKV[0:64, 0:256], ALU.add)
                k.copy(stb[:], st32[:], eng="pool")
                k.copy(o32[:].rearrange("p h d -> p (h d)"), pO[:, 0:256], eng="act")
                for h in range(4):
                    k.op("dve", lambda e, h=h: e.bn_stats(out=st6[:, h, :], in_=o32[:, h, :]), [o32[:]], [st6[:]])
                for h in range(4):
                    k.op("dve", lambda e, h=h: e.bn_aggr(out=mv[:, h, :], in_=st6[:, h, :]), [st6[:]], [mv[:]])
                k.act(rstd[:], mv[:, :, 1], AF.Sqrt, bias=self.epsc[:])
                k.recip(rstd[:], rstd[:])
                k.tt(o32[:], o32[:], mv[:, :, 0:1].broadcast_to([128, 4, 64]), ALU.subtract)
                k.tt(on[:], o32[:], rstd[:].unsqueeze(2).broadcast_to([128, 4, 64]), ALU.mult)
                k.act(sg[:], gt[b][:], AF.Silu)
                po = psb[7]
                for c in range(2):
                    k.tr(po[:, c * 128:(c + 1) * 128], on[:, 2 * c:2 * c + 2, :].rearrange("p h d -> p (h d)"), self.identb[:])
                jj = j % 4
                for c in range(2):
                    k.stt(oT[:, c, jj * 128:(jj + 1) * 128], po[:, c * 128:(c + 1) * 128], gnw[:, l * 2 + c:l * 2 + c + 1],
                          sg[:, c, :], ALU.mult, ALU.mult)
                if jj == 3:
                    t0 = (j - 3) * 128
                    k.dma(self.obrT[1, :, t0:t0 + 512].rearrange("(c p) t -> p c t", p=128), oT[:])
            k.barrier()

    def phase_nsa(self, l, s):
        nc, k = self.nc, self.k
        ps = self.ps
        psb = [p_[:].bitcast(BF16) for p_ in ps]
        with ExitStack() as es:
            W1 = k.sb(es, "ns_W1", [128, 32, 256], BF16)
            w2k = k.sb(es, "ns_w2k", [128, 2, 64], BF16)
            w2v = k.sb(es, "ns_w2v", [128, 2, 64], BF16)
            pe32 = k.sb(es, "ns_pe32", [128, DEPTH, 32], F32)
            peb = k.sb(es, "ns_peb", [128, 32], BF16)
            nc16 = k.sb(es, "ns_nc16", [128, T], BF16)
            ksT = k.sb(es, "ns_ksT", [128, T], BF16)
            kwT = k.sb(es, "ns_kwT", [128, T], BF16)
            qt = [k.sb(es, "ns_qt%d" % i, [128, 4, 512], BF16) for i in range(2)]
            vsa = k.sb(es, "ns_vsa", [128, 32, 128], BF16)
            vwa = k.sb(es, "ns_vwa", [128, 32, 128], BF16)
            cmpm = k.sb(es, "ns_cmpm", [128, 5, 512], BF16)
            cm = k.sb(es, "ns_cm", [128, 4, 512], BF16)
            bm = k.sb(es, "ns_bm", [128, 4, 512], BF16)
            E2 = k.sb(es, "ns_E2", [128, 32, 128], BF16)
            ovl = k.sb(es, "ns_ovl", [128, 2, 64], BF16)
            selA = k.sb(es, "ns_selA", [128, 32, 64], F32)
            selB = k.sb(es, "ns_selB", [128, 32, 64], F32)
            onesb = k.sb(es, "ns_onesb", [128, 128], BF16)
            hs = k.sb(es, "ns_hs", [128, 2, 2, 256], BF16)
            bias_h = k.sb(es, "ns_bh", [128, 4], F32)
            kcmpT = k.sb(es, "ns_kcmpT", [128, 256], BF16)
            vcmp = k.sb(es, "ns_vcmp", [128, 2, 128], BF16)
            gsp = k.sb(es, "ns_gsp", [128, 1024], F32)
            gt4 = k.sb(es, "ns_gt4", [65, 4, 3, 512], F32)
            PT = [k.sb(es, "ns_PT%d" % i, [128, 512], BF16) for i in range(3)]
            Pn = k.sb(es, "ns_Pn", [128, 4, 2, 512], BF16)
            rD = k.sb(es, "ns_rD", [128, 512], F32)
            sc = k.sb(es, "ns_sc", [128, 64], F32)
            sc2 = k.sb(es, "ns_sc2", [128, 64], F32)
            m8 = k.sb(es, "ns_m8", [128, 16], F32)
            mb = k.sb(es, "ns_mb", [128, 64], BF16)
            MbT = k.sb(es, "ns_MbT", [128, 512], BF16)
            rr = k.sb(es, "ns_rr", [65, 512], F32)
            osb = k.sb(es, "ns_osb", [64, 512], F32)
            tmpo = k.sb(es, "ns_tmpo", [64, 512], F32)
            acc = k.sb(es, "ns_acc", [64, 4, 512], F32)
            outsb = [k.sb(es, "ns_out%d" % i, [64, 512], BF16) for i in range(2)]
            k.dma(W1[0:64, :, :], self.nsa_ck_w1[l].rearrange("(l d) h -> d l h", d=64), q="pool")
            k.dma(W1[64:128, :, :], self.nsa_cv_w1[l].rearrange("(l d) h -> d l h", d=64), q="pool")
            k.dma(w2k[:], self.nsa_ck_w2[l].rearrange("(c p) d -> p c d", p=128), q="pool")
            k.dma(w2v[:], self.nsa_cv_w2[l].rearrange("(c p) d -> p c d", p=128), q="pool")
            k.dma(pe32[:], self.nsa_pe)
            k.copy(peb[:], pe32[:, l, :])
            k.dma(cmpm[:], self.c_cmpmask, q="pool")
            k.dma(cm[:], self.c_cmask, q="pool")
            k.dma(bm[:], self.c_bmask, q="pool")
            k.dma(E2[0:64, :, :], self.c_E2, q="pool")
            k.memset(E2[64:128, :, :], 0.0)
            k.memset(MbT[64:128, :], 0.0)
            k.memset(kcmpT[:], 0.0)
            k.memset(vcmp[:], 0.0)
            for q__ in qt:
                k.memset(q__[64:128, :, :], 0.0)
            k.dma(ovl[:], self.c_ovl, q="pool")
            k.dma(selA[:], self.c_selA)
            k.dma(selB[:], self.c_selB)
            k.memset(onesb[:], 1.0)
            k.dma(nc16[:], self.projF[FM_NC * 128:(FM_NC + 1) * 128, :])
            k.memset(ksT[64:128, :], 0.0)
            k.memset(kwT[64:128, :], 0.0)
            k.dma(ksT[0:64, :], self.projF[FM_NK * 128:FM_NK * 128 + 64, :])
            k.dma(kwT[0:64, :], self.projF[FM_NK * 128 + 64:FM_NK * 128 + 128, :])
            k.memset(vsa[:, :, 64:128], 0.0)
            k.memset(vsa[:, :, 64:65], 1.0)
            k.memset(vwa[:, :, 64:128], 0.0)
            k.memset(vwa[:, :, 64:65], 1.0)
            k.dma(vsa[:, :, 0:64], self.projT[:, TM_NVS:TM_NVS + 64].rearrange("(j p) d -> p j d", p=128))
            k.dma(vwa[:, :, 0:64], self.projT[:, TM_NVW:TM_NVW + 64].rearrange("(j p) d -> p j d", p=128))
            for pc in range(4):
                sl = slice(pc * 1024, (pc + 1) * 1024)
                k.dma(gsp[64:76, :], self.projS[8:20, sl])
                k.act(gsp[64:76, :], gsp[64:76, :], AF.Sigmoid)
                k.dma((self.gsD[:, sl], "gsD"), gsp[64:76, :])
            ncv = nc16[:].rearrange("p (n s) -> p n s", s=16)
            for kv in range(2):
                po = 64 * kv
                for hc in range(2):
                    pb = ps[0][:, kv * 2 + hc:kv * 2 + hc + 1]
                    for l_ in range(32):
                        k.mm(pb, W1[po:po + 64, l_, hc * 128:(hc + 1) * 128], peb[po:po + 64, l_:l_ + 1],
                             start=(l_ == 0), stop=(l_ == 31))
            k.copy(bias_h[:], ps[0][:, 0:4])
            for kv in range(2):
                po = 64 * kv
                for hc in range(2):
                    ph = ps[1 + hc]
                    for l_ in range(32):
                        k.mm(ph[:, 0:255], W1[po:po + 64, l_, hc * 128:(hc + 1) * 128],
                             ncv[po:po + 64, (l_ // 16):(l_ // 16) + 255, l_ % 16], start=(l_ == 0), stop=(l_ == 31))
                    k.act(hs[:, kv, hc, 0:255], ph[:, 0:255], AF.Silu, bias=bias_h[:, kv * 2 + hc:kv * 2 + hc + 1])
            for hc in range(2):
                k.mm(ps[3][0:64, 0:255], w2k[:, hc, :], hs[:, 0, hc, 0:255], start=(hc == 0), stop=(hc == 1))
            k.copy(kcmpT[0:64, 0:255], ps[3][0:64, 0:255], eng="act")
            MC = (128, 127)
            for c in range(2):
                M = MC[c]
                for hc in range(2):
                    k.mm(ps[4][0:M, c * 64:(c + 1) * 64], hs[:, 1, hc, c * 128:c * 128 + M], w2v[:, hc, :],
                         start=(hc == 0), stop=(hc == 1))
                k.copy(vcmp[0:M, c, 0:64], ps[4][0:M, c * 64:(c + 1) * 64])
            ob = 0
            pending = []
            for i in range(8):
                tok = slice(i * 512, (i + 1) * 512)
                q_ = qt[i % 2]
                k.dma(q_[0:64, :, :], self.projF[FM_NQ * 128:(FM_NQ + 2) * 128, tok].rearrange("(h d) t -> d h t", d=64))
                k.dma(gt4[64:65, :, :, :].rearrange("p h b t -> p (h b) t"),
                      (self.gsD[:, tok].rearrange("(o r) t -> o r t", o=1), "gsD"))
                ncs = [0] if i <= 3 else [0, 1]
                for h in range(4):
                    for c in ncs:
                        M = MC[c]
                        pS = ps[c]
                        d_ = 512 * i - 2048 * c
                        need = d_ < 2063
                        k.mm(pS[0:M, :], kcmpT[:, c * 128:c * 128 + M], q_[:, h, :], start=True, stop=(not need))
                        if need:
                            k.mm(pS[0:M, :], self.identb[:, 0:M], cmpm[:, d_ // 512, :], start=False, stop=True)
                        k.act(PT[c][0:M, :], pS[0:M, :], AF.Exp, scale=0.125)
                    for ci, c in enumerate(ncs):
                        M = MC[c]
                        k.mm(ps[2][:], onesb[0:M, :], PT[c][0:M, :], start=(ci == 0), stop=(ci == len(ncs) - 1))
                    k.ts(rD[:], ps[2][:], 1e-30, ALU.max)
                    k.recip(rD[:], rD[:])
                    for c in ncs:
                        M = MC[c]
                        k.tt(Pn[0:M, h, c, :], PT[c][0:M, :], rD[0:M, :], ALU.mult, eng="pool")
                    for ci, c in enumerate(ncs):
                        M = MC[c]
                        k.mm(ps[3][:, :], vcmp[0:M, c, :], Pn[0:M, h, c, :], start=(ci == 0), stop=(ci == len(ncs) - 1))
                    k.mm(ps[7][0:64, :], self.ones[64:65, 0:64], gt4[64:65, h, 0, :])
                    k.copy(osb[:], ps[3][0:64, :], eng="act")
                    k.tt(acc[:, h, :], osb[:], ps[7][0:64, :], ALU.mult)
                pT = psb[2]
                pI = ps[4]
                for q4 in range(4):
                    n_mm = 4 * len(ncs)
                    ii = 0
                    for h in range(4):
                        for c in ncs:
                            M = MC[c]
                            k.mm(pI[:, q4 * 64:(q4 + 1) * 64], Pn[0:M, h, c, q4 * 128:(q4 + 1) * 128], ovl[0:M, c, :],
                                 start=(ii == 0), stop=(ii == n_mm - 1))
                            ii += 1
                for q4 in range(4):
                    u = i * 4 + q4
                    k.tt(sc[:], pI[:, q4 * 64:(q4 + 1) * 64], selA[:, u, :], ALU.mult)
                    k.tt(sc[:], sc[:], selB[:, u, :], ALU.add)
                    k.op("dve", lambda e: e.max(out=m8[:, 0:8], in_=sc[:]), [sc[:]], [m8[:]])
                    k.op("dve", lambda e: e.match_replace(out=sc2[:], in_to_replace=m8[:, 0:8], in_values=sc[:], imm_value=-1.0e9),
                         [sc[:], m8[:]], [sc2[:]])
                    k.op("dve", lambda e: e.max(out=m8[:, 8:16], in_=sc2[:]), [sc2[:]], [m8[:]])
                    k.ts(mb[:], sc[:], m8[:, 15:16], ALU.is_ge, s2=1.0, op1=ALU.subtract)
                    k.tr(pT[0:64, q4 * 128:(q4 + 1) * 128], mb[:], self.identb[:])
                k.copy(MbT[0:64, :], pT[0:64, 0:512], eng="act")
                loops = []
                for h in range(4):
                    njt = 4 * (i + 1)

                    def qk_s(j, t, h=h, i=i, q_=q_):
                        pS = ps[t % 4]
                        diag = j >= 4 * i
                        k.mm(pS[:], ksT[:, j * 128:(j + 1) * 128], q_[:, h, :], start=True, stop=False)
                        k.mm(pS[:], E2[:, j, :], MbT[:], start=False, stop=(not diag))
                        if diag:
                            k.mm(pS[:], self.identb[:], cm[:, j - 4 * i, :], start=False, stop=True)
                        k.act(PT[t % 3][:], pS[:], AF.Exp, scale=0.125)

                    def pv_s(j, t, njt=njt):
                        k.mm(ps[5][:, :], vsa[:, j, :], PT[t % 3][:], start=(j == 0), stop=(j == njt - 1))

                    def fin_s(h=h):
                        self.attn_finalize(ps[5], rr, ps[7], osb, tmpo[:], gate_row=gt4[64:65, h, 1, :])
                        k.tt(acc[:, h, :], acc[:, h, :], tmpo[:], ALU.add, eng="pool")
                    loops.append(dict(n=njt, qk=qk_s, pv=pv_s, fin=fin_s))
                    kts = list(range(max(0, 4 * i - 4), 4 * i + 4))

                    def qk_w(j, t, h=h, i=i, q_=q_, kts=kts):
                        kt = kts[j]
                        pS = ps[t % 4]
                        diag = kt >= 4 * i
                        mk = cm[:, kt - 4 * i, :] if diag else bm[:, kt - 4 * i + 4, :]
                        k.mm(pS[:], kwT[:, kt * 128:(kt + 1) * 128], q_[:, h, :], start=True, stop=False)
                        k.mm(pS[:], self.identb[:], mk, start=False, stop=True)
                        k.act(PT[t % 3][:], pS[:], AF.Exp, scale=0.125)

                    def pv_w(j, t, kts=kts):
                        k.mm(ps[6][:, :], vwa[:, kts[j], :], PT[t % 3][:], start=(j == 0), stop=(j == len(kts) - 1))

                    def fin_w(h=h, o=outsb[ob % 2], tok=tok):
                        self.attn_finalize(ps[6], rr, ps[7], osb, tmpo[:], gate_row=gt4[64:65, h, 2, :])
                        k.tt(o[:], acc[:, h, :], tmpo[:], ALU.add)
                        k.dma(self.obrT[2, h * 64:(h + 1) * 64, tok], o[:])
                    ob += 1
                    loops.append(dict(n=len(kts), qk=qk_w, pv=pv_w, fin=fin_w))
                self.run_attn_loops(loops)
            k.barrier()

    def phase_gdn_conv(self, l, s):
        nc, k = self.nc, self.k
        ps = self.ps
        with ExitStack() as es:
            cw = k.sb(es, "gc_cw", [128, DEPTH, 6, 4], F32)
            blk = k.sb(es, "gc_blk", [128, 128], BF16)
            gcol = k.sb(es, "gc_gcol", [128, 8], F32)
            xpad = [k.sb(es, "gc_xp%d" % i, [128, 4 + T], F32) for i in range(2)]
            acc = k.sb(es, "gc_acc", [128, T], F32)
            ys = k.sb(es, "gc_ys", [128, T], F32)
            sq = k.sb(es, "gc_sq", [128, T], BF16)
            rin = [k.sb(es, "gc_rin%d" % i, [128, 512], F32) for i in range(2)]
            outb = [k.sb(es, "gc_out%d" % i, [128, T], BF16) for i in range(2)]
            k.dma(cw[:], self.gdn_convw)
            k.dma(blk[:], self.c_blk, q="pool")
            k.dma(gcol[:], self.c_gcols)
            lvl = getattr(self, "probe_conv", 9)
            for ch in range(6):
                xp = xpad[ch % 2]
                ob = outb[ch % 2]
                if lvl == 1 and ch > 0:
                    break
                k.memset(xp[:, 0:4], 0.0)
                k.dma(xp[:, 4:4 + T], self.projF[ch * 128:(ch + 1) * 128, :], q="pool")
                k.ts(acc[:], xp[:, 4:4 + T], cw[:, l, ch, 3:4], ALU.mult)
                for kk in (2, 1, 0):
                    k.stt(acc[:], xp[:, 1 + kk:1 + kk + T], cw[:, l, ch, kk:kk + 1], acc[:], ALU.mult, ALU.add)
                if lvl <= 2:
                    continue
                if ch >= 4:
                    k.act(ob[:], acc[:], AF.Silu)
                    k.dma(self.gvs[(ch - 4) * 128:(ch - 3) * 128, :], ob[:])
                else:
                    k.act(ys[:], acc[:], AF.Silu)
                    k.act(sq[:], ys[:], AF.Square)
                    if lvl <= 3:
                        continue
                    for tt in range(8):
                        sl = slice(tt * 512, (tt + 1) * 512)
                        pss = ps[tt % 2]
                        r = rin[tt % 2]
                        k.mm(pss[:], blk[:], sq[:, sl])
                        k.act(r[:], pss[:], AF.Sqrt, bias=gcol[:, 6:7])
                        k.recip(r[:], r[:])
                        if ch < 2:
                            k.stt(ob[:, sl], ys[:, sl], 0.125, r[:], ALU.mult, ALU.mult)
                        else:
                            k.tt(ob[:, sl], ys[:, sl], r[:], ALU.mult, eng="pool")
                    dst = self.gqn if ch < 2 else self.gkn
                    k.dma(dst[(ch % 2) * 128:(ch % 2 + 1) * 128, :], ob[:])
            k.barrier()

    def phase_gdn(self, l, s):
        self.phase_gdn_conv(l, s)
        if getattr(self, "gdn_conv_only", False):
            return
        nc, k = self.nc, self.k
        ps = self.ps
        psb = [p_[:].bitcast(BF16) for p_ in ps]
        TP = 1024
        NCH = TP // 64
        with ExitStack() as es:
            gcol = k.sb(es, "gd_gcol", [128, 8], F32)
            gmsk = k.sb(es, "gd_gmsk", [128, 4], F32)
            mreset = k.sb(es, "gd_mreset", [128, TP], F32)
            maskS = k.sb(es, "gd_maskS", [64, 256], F32)
            maskIT = k.sb(es, "gd_maskIT", [64, 256], F32)
            pA = k.sb(es, "gd_pA", [128, DEPTH], F32)
            pDt = k.sb(es, "gd_pDt", [128, DEPTH], F32)
            nA = k.sb(es, "gd_nA", [128, 1], F32)
            nw = k.sb(es, "gd_nw", [64, DEPTH, 64], F32)
            raw = k.sb(es, "gd_raw", [128, TP], F32)
            gg = k.sb(es, "gd_gg", [128, TP], F32)
            bc = k.sb(es, "gd_bc", [128, TP], F32)
            At = k.sb(es, "gd_A", [128, TP], F32)
            Bt = k.sb(es, "gd_B", [128, TP], F32)
            AtH = k.sb(es, "gd_AH", [2, 4, TP], F32)
            BtH = k.sb(es, "gd_BH", [2, 4, TP], F32)
            R1 = k.sb(es, "gd_R1", [128, TP], F32)
            R2 = k.sb(es, "gd_R2", [128, TP], F32)
            rhsd = k.sb(es, "gd_rhsd", [128, NCH, 4], F32)
            Gs = k.sb(es, "gd_Gs", [64, NCH, 4], F32)
            qn = k.sb(es, "gd_qn", [64, 4, TP], BF16)
            kn = k.sb(es, "gd_kn", [64, 4, TP], BF16)
            vv = k.sb(es, "gd_vv", [64, 4, TP], BF16)
            zT = k.sb(es, "gd_zT", [128, 2, TP], BF16)
            sz = k.sb(es, "gd_sz", [128, 2, 64], F32)
            tm1 = k.sb(es, "gd_tm1", [64, 128], F32)
            tm2 = k.sb(es, "gd_tm2", [64, 128], F32)
            smS = [k.sb(es, "gd_sm%d" % i, [64, 8, 4], F32) for i in range(2)]
            y32S = [k.sb(es, "gd_y32_%d" % i, [64, 4, 128], F32) for i in range(2)]
            ybS = [k.sb(es, "gd_yb_%d" % i, [64, 4, 128], BF16) for i in range(2)]
            kdecS = [k.sb(es, "gd_kdec_%d" % i, [64, 4, 64], BF16) for i in range(2)]
            attnTS = [k.sb(es, "gd_attnT_%d" % i, [64, 4, 64], BF16) for i in range(2)]
            NbS = [[k.sb(es, "gd_N%d_%d" % (j, i), [64, 4, 64], BF16) for i in range(2)] for j in range(2)]
            PbS = [[k.sb(es, "gd_P%d_%d" % (j, i), [64, 4, 64], BF16) for i in range(2)] for j in range(2)]
            kvtm = k.sb(es, "gd_kvtm", [64, 512], BF16)
            kdec = k.sb(es, "gd_kdec", [64, 4, 64], BF16)
            Ds = k.sb(es, "gd_Ds", [64, 256], F32)
            DTs = k.sb(es, "gd_DTs", [64, 256], F32)
            tmpN = k.sb(es, "gd_tmpN", [64, 256], F32)
            Nb = [k.sb(es, "gd_N%d" % i, [64, 4, 64], BF16) for i in range(2)]
            Pb = [k.sb(es, "gd_P%d" % i, [64, 4, 64], BF16) for i in range(2)]
            attnT = k.sb(es, "gd_attnT", [64, 4, 64], BF16)
            y32 = k.sb(es, "gd_y32", [64, 4, 128], F32)
            yb = k.sb(es, "gd_yb", [64, 4, 128], BF16)
            wT = k.sb(es, "gd_wT", [64, 4, 64], BF16)
            ub = k.sb(es, "gd_ub", [64, 4, 64], BF16)
            S32 = k.sb(es, "gd_S32", [64, 4, 64], F32)
            Sb = k.sb(es, "gd_Sb", [64, 4, 64], BF16)
            t1 = k.sb(es, "gd_t1", [64, 4, 64], F32)
            o32 = k.sb(es, "gd_o32", [64, 4, 64], F32)
            osq = k.sb(es, "gd_osq", [64, 4, 64], F32)
            ss = k.sb(es, "gd_ss", [64, 8], F32)
            on = k.sb(es, "gd_on", [64, 4, 64], BF16)
            oT = k.sb(es, "gd_oT", [128, 2, 512], BF16)
            k.dma(gcol[:], self.c_gcols)
            k.dma(gmsk[:], self.c_gmsk)
            k.dma(mreset[:], self.c_mreset)
            k.dma(maskS[:], self.c_gmaskS)
            k.dma(maskIT[:], self.c_gmaskIT)
            k.dma(pA[:], self.gdn_pA)
            k.dma(pDt[:], self.gdn_pDt)
            k.dma(nw[:], self.gdn_nw)
            k.act(nA[:], pA[:, l:l + 1], AF.Exp)
            k.ts(nA[:], nA[:], -1.0, ALU.mult)
            k.memset(S32[:], 0.0)
            k.memset(Sb[:], 0.0)
            id64 = self.ident[0:64, 0:64]
            idb64 = self.identb[0:64, 0:64]
            for pc in range(T // TP):
                p0 = pc * TP
                k.dma(raw[:], self.projS[:, p0:p0 + TP])
                k.dma(qn[:], self.gqn[:, p0:p0 + TP].rearrange("(h d) t -> d h t", d=64))
                k.dma(kn[:], self.gkn[:, p0:p0 + TP].rearrange("(h d) t -> d h t", d=64))
                k.dma(vv[:], self.gvs[:, p0:p0 + TP].rearrange("(h d) t -> d h t", d=64))
                k.dma(zT[:], self.projF[FM_GZ * 128:(FM_GZ + 2) * 128, p0:p0 + TP].rearrange("(c p) t -> p c t", p=128))
                k.act(gg[:], raw[:], AF.Exp, bias=pDt[:, l:l + 1])
                k.act(gg[:], gg[:], AF.Ln, bias=self.onec[:])
                k.ts(gg[:], gg[:], nA[:], ALU.mult)
                k.op("dve", lambda e: e.tensor_tensor_scan(out=bc[:], data0=mreset[:], data1=gg[:], initial=0.0,
                                                            op0=ALU.mult, op1=ALU.add), [mreset[:], gg[:]], [bc[:]])
                k.ts(At[:], bc[:], gcol[:, 0:1], ALU.mult, s2=gcol[:, 1:2], op1=ALU.add)
                k.ts(Bt[:], bc[:], gcol[:, 2:3], ALU.mult, s2=gcol[:, 3:4], op1=ALU.add)
                for h in range(4):
                    k.dma(AtH[:, h, :], At[32 * h:32 * h + 2, :])
                    k.dma(BtH[:, h, :], Bt[32 * h:32 * h + 2, :])
                k.act(raw[:], raw[:], AF.Sigmoid)
                k.ts(raw[:], raw[:], gcol[:, 4:5], ALU.mult)
                k.stt(R1[:], bc[:], gcol[:, 5:6], raw[:], ALU.mult, ALU.add)
                bl = bc[:].rearrange("p (n c) -> p n c", c=64)[:, :, 63:64]
                k.tt(R2[:].rearrange("p (n c) -> p n c", c=64), bl.broadcast_to([128, NCH, 64]),
                     bc[:].rearrange("p (n c) -> p n c", c=64), ALU.subtract)
                k.act(R2[:], R2[:], AF.Exp)
                k.tt(rhsd[:], bl.broadcast_to([128, NCH, 4]), gmsk[:].unsqueeze(1).broadcast_to([128, NCH, 4]), ALU.mult)
                k.mm(ps[7][0:64, 0:NCH * 4], self.ones[:, 0:64], rhsd[:].rearrange("p n h -> p (n h)"))
                k.act(Gs[:].rearrange("p n h -> p (n h)"), ps[7][0:64, 0:NCH * 4], AF.Exp)
                def chunk(n):
                    sl_ = n % 2
                    sm_, y32_, yb_, kdec_, attnT_ = smS[sl_], y32S[sl_], ybS[sl_], kdecS[sl_], attnTS[sl_]
                    Nb_, Pb_ = NbS[sl_], PbS[sl_]
                    c0 = n * 64
                    cs_ = slice(c0, c0 + 64)
                    gn = pc * NCH + n
                    k.tr(ps[0][0:64, 0:128], R1[:, cs_], self.ident[:])
                    k.tr(ps[0][0:64, 128:256], R2[:, cs_], self.ident[:])
                    k.copy(tm1[:], ps[0][0:64, 0:128], eng="act")
                    k.copy(tm2[:], ps[0][0:64, 128:256], eng="act")
                    tv1 = tm1[:].rearrange("p (h r) -> p h r", r=32)
                    tv2 = tm2[:].rearrange("p (h r) -> p h r", r=32)
                    b_tm = tv1[:, :, 0]
                    beta_tm = tv1[:, :, 2]
                    ekd_tm = tv2[:, :, 0]
                    nbeta, eb, beb = sm_[:, 0, :], sm_[:, 1, :], sm_[:, 2, :]
                    k.ts(nbeta, beta_tm, -1.0, ALU.mult)
                    k.act(eb, b_tm, AF.Exp)
                    k.tt(beb, eb, beta_tm, ALU.mult)
                    yield "prep"
                    pkv = psb[1]
                    for h in range(4):
                        k.tr(pkv[0:64, h * 64:(h + 1) * 64], kn[:, h, cs_], idb64)
                    for h in range(4):
                        k.tr(pkv[0:64, 256 + h * 64:256 + (h + 1) * 64], vv[:, h, cs_], idb64)
                    k.copy(kvtm[:], pkv[0:64, 0:512], eng="act")
                    kt3 = kvtm[:, 0:256].rearrange("p (h d) -> p h d", d=64)
                    vt3 = kvtm[:, 256:512].rearrange("p (h d) -> p h d", d=64)
                    k.tt(y32_[:, :, 0:64], vt3, beta_tm.unsqueeze(2).broadcast_to([64, 4, 64]), ALU.mult)
                    k.tt(y32_[:, :, 64:128], kt3, beb.unsqueeze(2).broadcast_to([64, 4, 64]), ALU.mult)
                    k.tt(kdec_[:], kt3, ekd_tm.unsqueeze(2).broadcast_to([64, 4, 64]), ALU.mult)
                    k.copy(yb_[:], y32_[:], eng="act")
                    yield "prep"
                    pD = ps[2]
                    k.mm(pD[0:64, 0:256], id64, maskS[:], start=True, stop=False)
                    def AB(h):
                        return AtH[0:2, h, cs_], BtH[0:2, h, cs_]
                    import os
                    hs_ = [int(c) for c in os.environ.get("GH", "0123")]
                    for h in hs_:
                        a_, b_ = AB(h)
                        k.mm(pD[0:64, h * 64:(h + 1) * 64], a_, b_, start=False, stop=True)
                    k.mm(pD[0:64, 256:512], id64, maskIT[:], start=True, stop=False)
                    for h in hs_:
                        a_, b_ = AB(h)
                        k.mm(pD[0:64, 256 + h * 64:256 + (h + 1) * 64], b_, a_, start=False, stop=True)
                    k.act(Ds[:], pD[0:64, 0:256], AF.Exp)
                    k.act(DTs[:], pD[0:64, 256:512], AF.Exp)
                    yield "prep"
                    pKK = ps[3]
                    for h in range(4):
                        k.mm(pKK[0:64, h * 64:(h + 1) * 64], kn[:, h, cs_], kn[:, h, cs_])
                    for h in range(4):
                        k.mm(pKK[0:64, 256 + h * 64:256 + (h + 1) * 64], kn[:, h, cs_], qn[:, h, cs_])
                    k.tt(tmpN[:], pKK[0:64, 0:256], Ds[:], ALU.mult)
                    N0, P0 = Nb_[0], Pb_[0]
                    k.tt(N0[:], tmpN[:].rearrange("p (h j) -> p h j", j=64), nbeta.unsqueeze(2).broadcast_to([64, 4, 64]), ALU.mult)
                    k.tt(attnT_[:].rearrange("p h i -> p (h i)"), pKK[0:64, 256:512], DTs[:], ALU.mult)
                    pP = psb[4]
                    for h in range(4):
                        k.tr(pP[0:64, h * 64:(h + 1) * 64], N0[:, h, :], idb64)
                    k.copy(P0[:].rearrange("p h i -> p (h i)"), pP[0:64, 0:256], eng="act")
                    yield "prepdone"
                    cur = 0
                    for lev in range(6):
                        Nc, Pc = Nb_[cur], Pb_[cur]
                        pY = ps[6]
                        for h in range(4):
                            k.mm(pY[0:64, h * 128:(h + 1) * 128], Pc[:, h, :], yb_[:, h, :])
                        if lev < 5:
                            Nn, Pn_ = Nb_[1 - cur], Pb_[1 - cur]
                            pN = ps[5]
                            for h in range(4):
                                k.mm(pN[0:64, h * 64:(h + 1) * 64], Pc[:, h, :], Nc[:, h, :])
                            for h in range(4):
                                k.mm(pN[0:64, 256 + h * 64:256 + (h + 1) * 64], Nc[:, h, :], Pc[:, h, :])
                        k.tt(y32_[:].rearrange("p h c -> p (h c)"), y32_[:].rearrange("p h c -> p (h c)"), pY[0:64, :], ALU.add)
                        k.copy(yb_[:], y32_[:], eng="act")
                        if lev < 5:
                            k.copy(Nn[:].rearrange("p h i -> p (h i)"), pN[0:64, 0:256], eng="act")
                            k.copy(Pn_[:].rearrange("p h i -> p (h i)"), pN[0:64, 256:512], eng="act")
                            cur = 1 - cur
                        yield "lev"
                    pW = psb[4]
                    for h in range(4):
                        k.tr(pW[0:64, 256 + h * 64:256 + (h + 1) * 64], yb_[:, h, 64:128], idb64)
                    k.copy(wT[:].rearrange("p h i -> p (h i)"), pW[0:64, 256:512], eng="act")
                    pU = ps[7]
                    for h in range(4):
                        k.mm(pU[0:64, h * 64:(h + 1) * 64], wT[:, h, :], Sb[:, h, :])
                    for h in range(4):
                        k.mm(pU[0:64, 256 + h * 64:256 + (h + 1) * 64], qn[:, h, cs_], Sb[:, h, :])
                    k.tt(ub[:], y32_[:, :, 0:64], pU[0:64, 0:256].rearrange("p (h d) -> p h d", d=64), ALU.subtract)
                    pAU = ps[4]
                    for h in range(4):
                        k.mm(pAU[0:64, 256 + h * 64:256 + (h + 1) * 64], attnT_[:, h, :], ub[:, h, :])
                    pSn = ps[1]
                    for h in range(4):
                        k.mm(pSn[0:64, 256 + h * 64:256 + (h + 1) * 64], kdec_[:, h, :], ub[:, h, :])
                    k.tt(t1[:], pU[0:64, 256:512].rearrange("p (h d) -> p h d", d=64), eb.unsqueeze(2).broadcast_to([64, 4, 64]), ALU.mult)
                    k.tt(o32[:].rearrange("p h d -> p (h d)"), t1[:].rearrange("p h d -> p (h d)"), pAU[0:64, 256:512], ALU.add)
                    k.tt(S32[:], S32[:], Gs[:, n, :].unsqueeze(2).broadcast_to([64, 4, 64]), ALU.mult, eng="pool")
                    k.tt(S32[:].rearrange("p h d -> p (h d)"), S32[:].rearrange("p h d -> p (h d)"), pSn[0:64, 256:512], ALU.add)
                    k.copy(Sb[:], S32[:], eng="pool")
                    k.act(osq[:], o32[:], AF.Square)
                    k.red(ss[:, 0:4], osq[:], ALU.add)
                    k.ts(ss[:, 0:4], ss[:, 0:4], 1.0 / 64.0, ALU.mult, s2=1.0e-6, op1=ALU.add)
                    k.act(ss[:, 0:4], ss[:, 0:4], AF.Sqrt)
                    k.recip(ss[:, 4:8], ss[:, 0:4])
                    k.tt(o32[:], o32[:], ss[:, 4:8].unsqueeze(2).broadcast_to([64, 4, 64]), ALU.mult)
                    k.tt(on[:], o32[:], nw[:, l, :].unsqueeze(1).broadcast_to([64, 4, 64]), ALU.mult)
                    pOT = psb[0]
                    for c in range(2):
                        k.tr(pOT[:, 512 + c * 64:512 + (c + 1) * 64], on[:, 2 * c:2 * c + 2, :].rearrange("p h d -> p (h d)"), idb64)
                    k.act(sz[:], zT[:, :, cs_], AF.Silu)
                    jj = gn % 8
                    for c in range(2):
                        k.tt(oT[:, c, jj * 64:(jj + 1) * 64], pOT[:, 512 + c * 64:512 + (c + 1) * 64], sz[:, c, :], ALU.mult)
                    if jj == 7:
                        t0 = (gn - 7) * 64
                        k.dma(self.obrT[0, :, t0:t0 + 512].rearrange("(c p) t -> p c t", p=128), oT[:])

                gens = [chunk(n) for n in range(NCH)]

                def run_until(g, tag):
                    for t_ in g:
                        if t_ == tag:
                            return True
                    return False

                run_until(gens[0], "prepdone")
                for n in range(NCH):
                    for lev in range(6):
                        next(gens[n])
                        if lev < 4 and n + 1 < NCH:
                            next(gens[n + 1])
                    for _ in gens[n]:
                        pass

            k.barrier()

    for f in (run_attn_loops, phase_proj, attn_finalize, phase_fox, phase_merge, phase_mixer, phase_ret, phase_nsa, phase_gdn, phase_gdn_conv):
        setattr(Prog, f.__name__, f)


_mixer_methods()

def host_consts():
    c = {}
    sel = np.zeros((16, NE * 128), np.float32)
    for e in range(NE):
        sel[e, e * 128:(e + 1) * 128] = 1.0
    c["c_sel16"] = sel
    c["c_ident"] = np.eye(128, dtype=np.float32)
    kk = np.arange(128)[:, None, None]
    oo = np.arange(4)[None, :, None]
    qq = np.arange(512)[None, None, :]
    c["c_cmask"] = np.where(oo * 128 + kk <= qq, 0.0, NEG).astype(np.float32)
    gc = np.zeros((128, 8), np.float32)
    gm = np.zeros((128, 4), np.float32)
    for h in range(4):
        gc[32 * h, 0] = 1.0; gc[32 * h + 1, 1] = 1.0; gc[32 * h + 1, 2] = -1.0; gc[32 * h, 3] = 1.0
        gc[32 * h + 2, 4] = 1.0; gc[32 * h, 5] = 1.0
        gm[32 * h, h] = 1.0
    gc[:, 6] = 1.0e-6
    c["c_gcols"] = gc
    c["c_gmsk"] = gm
    c["c_mreset"] = np.ascontiguousarray(np.broadcast_to((np.arange(1024) % 64 != 0).astype(np.float32)[None, :], (128, 1024)))
    ii = np.arange(64)
    mS = np.where(ii[:, None] > ii[None, :], 0.0, NEG).astype(np.float32)
    mIT = np.where(ii[None, :] >= ii[:, None], 0.0, NEG).astype(np.float32)
    c["c_gmaskS"] = np.ascontiguousarray(np.tile(mS, (1, 4)))
    c["c_gmaskIT"] = np.ascontiguousarray(np.tile(mIT, (1, 4)))
    blk = np.zeros((128, 128), np.float32); blk[0:64, 0:64] = 1.0; blk[64:128, 64:128] = 1.0
    c["c_blk"] = blk
    nl = np.arange(128)[:, None, None]
    di = np.arange(5)[None, :, None]
    c["c_cmpmask"] = np.where(16 * nl + 31 - qq <= 512 * di, 0.0, NEG).astype(np.float32)
    c["c_bmask"] = np.where(oo * 128 + kk > qq, 0.0, NEG).astype(np.float32)
    jj = np.arange(64)
    E2 = np.zeros((64, 32, 128), np.float32)
    for kt in range(32):
        for m_ in range(128):
            E2[2 * kt + m_ // 64, kt, m_] = -NEG
    c["c_E2"] = E2
    n_all = np.arange(256)
    ov = np.clip(np.minimum(n_all[:, None] * 16 + 32, jj[None, :] * 64 + 64) - np.maximum(n_all[:, None] * 16, jj[None, :] * 64), 0, 32) / 32.0
    ov[255:] = 0.0
    c["c_ovl"] = np.ascontiguousarray(ov.reshape(2, 128, 64).transpose(1, 0, 2)).astype(np.float32)
    tpos = (np.arange(32)[None, :] * 128 + np.arange(128)[:, None])
    cur = (tpos // 64)[:, :, None]
    jb = jj[None, None, :]
    forced = (jb == 0) | (jb == cur) | (jb == cur - 1)
    causal = jb <= cur
    c["c_selA"] = (causal & ~forced).astype(np.float32)
    c["c_selB"] = (1.0e4 * forced - 1.0 * ((~causal) & (~forced))).astype(np.float32)
    half = 32
    invf = (10000.0 ** (-np.arange(half, dtype=np.float32) / half)).astype(np.float32)
    c["c_invf"] = np.ascontiguousarray(np.broadcast_to(invf[None, :], (128, 32))).astype(np.float32)
    lg = np.log1p(-(2.0 ** (-5.0 - np.arange(4, dtype=np.float64))))
    idx = np.arange(128, dtype=np.float64)
    rel = idx[None, :] - idx[:, None]
    dec = np.where(rel[None] >= 0, np.exp(np.maximum(rel[None], 0.0) * lg[:, None, None]), 0.0)
    c["c_decT"] = np.ascontiguousarray(dec.transpose(1, 0, 2)).astype(np.float32)
    xi = np.exp((idx[None, :] + 1.0) * lg[:, None])
    c["c_xiT"] = np.ascontiguousarray(np.broadcast_to(xi[None], (64, 4, 128))).astype(np.float32)
    zeta = np.exp((127.0 - idx[None, :]) * lg[:, None]) / 8.0
    c["c_zt"] = np.ascontiguousarray(zeta.T).astype(np.float32)
    cd = np.exp(128.0 * lg)
    c["c_cd"] = np.ascontiguousarray(np.broadcast_to(cd[None, :, None], (64, 4, 64))).astype(np.float32)
    return c


_O = dict(gq=0, gk=256, gv=512, ga=768, gb=772, gz=776, rq=1032, rk=1288, rv=1544, rg=1800, nq=2056, nkc=2312,
          nvc=2376, nks=2440, nvs=2504, nkw=2568, nvw=2632, ngate=2696, fq=2708, fk=2964, fv=3220, ff=3476)


def permute_w_in(w_in):
    out = np.zeros((w_in.shape[0], D, WIN_COLS), np.float32)

    def put(dst, name, width):
        out[:, :, dst:dst + width] = w_in[:, :, _O[name]:_O[name] + width]

    put(FM_GQ * 128, "gq", 256); put(FM_GK * 128, "gk", 256); put(FM_GV * 128, "gv", 256); put(FM_GZ * 128, "gz", 256)
    put(FM_RG * 128, "rg", 256); put(FM_NQ * 128, "nq", 256); put(FM_FQ * 128, "fq", 256); put(FM_FK * 128, "fk", 256)
    put(FM_NC * 128, "nkc", 64); put(FM_NC * 128 + 64, "nvc", 64)
    put(FM_NK * 128, "nks", 64); put(FM_NK * 128 + 64, "nkw", 64)
    for h in range(4):
        for r, nm in ((0, "ga"), (1, "ga"), (2, "gb")):
            out[:, :, FM_SM * 128 + 32 * h + r] = w_in[:, :, _O[nm] + h]
    put(FM_SM * 128 + 8, "ngate", 12); put(FM_SM * 128 + 20, "ff", 4)
    b = NFM * 128
    put(b + TM_RQ, "rq", 256); put(b + TM_RK, "rk", 256); put(b + TM_RV, "rv", 256); put(b + TM_FV, "fv", 256)
    put(b + TM_NVS, "nvs", 64); put(b + TM_NVW, "nvw", 64)
    return out


def pcol(v):
    v = np.asarray(v)
    sh = v.shape[:-1]
    n = v.shape[-1] // 128
    v = v.reshape(sh + (n, 128))
    v = np.moveaxis(v, -1, 0)
    return np.ascontiguousarray(v)


def prep_core(inp, seqs):
    S = len(seqs)
    m = {}
    m["xT"] = np.ascontiguousarray(np.transpose(inp["x"][seqs], (0, 2, 1)))
    m["cT"] = np.ascontiguousarray(np.transpose(pcol(inp["c"][seqs]), (0, 2, 1)))
    m["ada_w"] = np.ascontiguousarray(inp["ada_w"])
    m["ada_b"] = pcol(inp["ada_b"].reshape(DEPTH, 2, 3, D)).reshape(128, -1)
    m["ln_g"] = pcol(inp["ln_g"]).reshape(128, -1)
    m["ln_b"] = pcol(inp["ln_b"]).reshape(128, -1)
    m["router_w"] = np.ascontiguousarray(inp["router_w"].reshape(8, 128, NE).transpose(1, 0, 2))
    m["router_b"] = np.ascontiguousarray(inp["router_b"].reshape(1, NE))
    for n in ("exp_w1", "exp_w3", "exp_w2", "w_gate", "branch_proj", "w_out"):
        m[n] = np.ascontiguousarray(inp[n])
    m["w_in"] = permute_w_in(inp["w_in"])
    m["fox_fb"] = np.ascontiguousarray(inp["fox_f_bias"].T)
    m["gdn_convw"] = np.ascontiguousarray(inp["gdn_conv_w"].reshape(DEPTH, 4, 6, 128).transpose(3, 0, 2, 1))
    pA = np.zeros((128, DEPTH), np.float32); pDt = np.zeros((128, DEPTH), np.float32)
    for h in range(4):
        for r in (0, 1):
            pA[32 * h + r, :] = inp["gdn_a_log"][:, h]
            pDt[32 * h + r, :] = inp["gdn_dt_bias"][:, h]
    m["gdn_pA"] = pA
    m["gdn_pDt"] = pDt
    m["gdn_nw"] = np.ascontiguousarray(np.broadcast_to(inp["gdn_norm_w"][None], (64, DEPTH, 64))).astype(np.float32)
    pe = np.transpose(inp["nsa_cmp_pe"], (2, 0, 1))
    m["nsa_pe"] = np.ascontiguousarray(np.concatenate([pe, pe], axis=0))
    for n in ("nsa_ck_w1", "nsa_cv_w1", "nsa_ck_w2", "nsa_cv_w2"):
        m[n] = np.ascontiguousarray(inp[n])
    m["ret_gnw"] = pcol(inp["ret_gn_w"]).reshape(128, -1)
    m["posT"] = np.ascontiguousarray(inp["positions"][seqs].reshape(S, 32, 128).transpose(0, 2, 1)).astype(np.int32)
    return m


_CACHE = {}


def kernel(**inputs):
    inp = {k_: np.asarray(v) for k_, v in inputs.items()}
    ncores = 8
    S = inp["x"].shape[0] // ncores
    if "prog" not in _CACHE:
        p = Prog(nseq=S)
        p.build()
        _CACHE["prog"] = p
    p = _CACHE["prog"]
    consts = host_consts()
    in_maps = []
    for c in range(ncores):
        m = prep_core(inp, list(range(c * S, (c + 1) * S)))
        m.update(consts)
        in_maps.append({n: m[n] for n in p.inputs})
    res = run_bass_kernel_spmd(p.nc, in_maps, core_ids=list(range(ncores)))
    outs = [np.transpose(r["outT"], (0, 2, 1)) for r in res.results]
    return np.ascontiguousarray(np.concatenate(outs, axis=0)).astype(np.float32)
```

```python
import math
from contextlib import ExitStack
import numpy as np
import concourse.bass as bass
import concourse.mybir as mybir
from concourse.bass_utils import run_bass_kernel_spmd

F32 = mybir.dt.float32
BF16 = mybir.dt.bfloat16
I32 = mybir.dt.int32
ALU = mybir.AluOpType
AF = mybir.ActivationFunctionType
AX = mybir.AxisListType

D = 1024
T = 4096
DEPTH = 2
NE = 16
DE = 512
ALPHA = (2.0 * DEPTH) ** 0.25
LN_EPS = 1e-5
NEG = -30000.0


class K:
    NSLOT = 8

    def __init__(self, nc):
        self.nc = nc
        self.es = ExitStack()
        self.eng = {"pe": nc.tensor, "act": nc.scalar, "dve": nc.vector, "pool": nc.gpsimd, "sp": nc.sync}
        self.sem = {}
        self.cnt = {}
        for e in self.eng:
            self.sem[e] = self.es.enter_context(nc.semaphore("sem_" + e))
            self.cnt[e] = 0
        self.slots = {}
        self.slot_idx = {}
        for q in ("sp", "pool", "act"):
            self.slots[q] = [self.es.enter_context(nc.semaphore("dq_%s_%d" % (q, i))) for i in range(self.NSLOT)]
            self.slot_idx[q] = 0
        self.slot_val = {}
        self.seen = {e: {} for e in self.eng}
        self.lastw = {}
        self.readers = {}
        self.n_ins = 0

    @staticmethod
    def key(ap):
        if isinstance(ap, tuple):
            return ap[1]
        if ap is None or isinstance(ap, (int, float)):
            return None
        t = ap.tensor
        if str(t.space).lower().find("dram") >= 0 or type(t).__name__.startswith("DRam"):
            return None
        return t.name

    @staticmethod
    def raw(ap):
        return ap[0] if isinstance(ap, tuple) else ap

    def _need(self, eng, reads, writes):
        need = {}

        def add(dep):
            semname, sem, val, owner = dep
            if owner == eng and eng == "pe":
                return
            if need.get(semname, (None, -1))[1] < val:
                need[semname] = (sem, val)

        for r in reads:
            kk = self.key(r)
            if kk is None:
                continue
            if kk in self.lastw:
                add(self.lastw[kk])
        for w in writes:
            kk = self.key(w)
            if kk is None:
                continue
            if kk in self.lastw:
                add(self.lastw[kk])
            for dep in self.readers.get(kk, {}).values():
                if dep[3] == eng and dep[0].startswith("sem_"):
                    continue
                add(dep)
        return need

    def _emit_waits(self, eng, need):
        e = self.eng[eng]
        seen = self.seen[eng]
        for semname, (sem, val) in need.items():
            if seen.get(semname, -1) >= val:
                continue
            e.wait_ge(sem, val)
            seen[semname] = val
            self.n_ins += 1

    def _record(self, dep, reads, writes):
        for r in reads:
            kk = self.key(r)
            if kk is None:
                continue
            self.readers.setdefault(kk, {})[dep[0]] = dep
        for w in writes:
            kk = self.key(w)
            if kk is None:
                continue
            self.lastw[kk] = dep
            self.readers[kk] = {}

    def op(self, eng, fn, reads, writes):
        need = self._need(eng, reads, writes)
        self._emit_waits(eng, need)
        ins = fn(self.eng[eng])
        self.cnt[eng] += 1
        ins.then_inc(self.sem[eng], 1)
        self.n_ins += 1
        dep = ("sem_" + eng, self.sem[eng], self.cnt[eng], eng)
        self._record(dep, reads, writes)
        return ins

    def dma(self, out, in_, q="sp", **kw):
        reads, writes = [in_], [out]
        need = self._need(q, reads, writes)
        i = self.slot_idx[q] % self.NSLOT
        self.slot_idx[q] += 1
        semname = "dq_%s_%d" % (q, i)
        sem = self.slots[q][i]
        prev = self.slot_val.get((q, i), 0)
        if prev > 0:
            need[semname] = (sem, prev)
        self._emit_waits(q, need)
        ins = self.eng[q].dma_start(out=self.raw(out), in_=self.raw(in_), **kw)
        val = prev + 16
        ins.then_inc(sem, 16)
        self.slot_val[(q, i)] = val
        self.n_ins += 1
        dep = (semname, sem, val, "dma_" + q)
        self._record(dep, reads, writes)
        return ins

    def barrier(self):
        need = {}
        for e in self.eng:
            if self.cnt[e] > 0:
                need["sem_" + e] = (self.sem[e], self.cnt[e])
        for (q, i), v in self.slot_val.items():
            need["dq_%s_%d" % (q, i)] = (self.slots[q][i], v)
        for e in self.eng:
            nd = {k: v for k, v in need.items() if k != "sem_" + e}
            self._emit_waits(e, nd)
        self.lastw = {}
        self.readers = {}

    def mm(self, out, lhsT, rhs, start=True, stop=True):
        return self.op("pe", lambda e: e.matmul(self.raw(out), self.raw(lhsT), self.raw(rhs), start=start, stop=stop),
                       [lhsT, rhs], [out])

    def tr(self, out, in_, ident):
        return self.op("pe", lambda e: e.transpose(self.raw(out), self.raw(in_), self.raw(ident)), [in_, ident], [out])

    def act(self, out, in_, func, bias=None, scale=1.0, eng="act"):
        rd = [in_]
        kw = {}
        if bias is not None:
            kw["bias"] = self.raw(bias)
            rd.append(bias)
        if not isinstance(scale, (int, float)):
            rd.append(scale)
            kw["scale"] = self.raw(scale)
        else:
            kw["scale"] = float(scale)
        return self.op(eng, lambda e: e.activation(out=self.raw(out), in_=self.raw(in_), func=func, **kw), rd, [out])

    def ts(self, out, in0, s1, op0, s2=None, op1=None, eng="dve"):
        rd = [in0] + [s for s in (s1, s2) if s is not None and not isinstance(s, (int, float))]
        kw = {}
        if op1 is not None:
            kw["op1"] = op1
        return self.op(eng, lambda e: e.tensor_scalar(out=self.raw(out), in0=self.raw(in0), scalar1=self.raw(s1),
                                                      scalar2=self.raw(s2), op0=op0, **kw), rd, [out])

    def tt(self, out, in0, in1, op, eng="dve"):
        return self.op(eng, lambda e: e.tensor_tensor(out=self.raw(out), in0=self.raw(in0), in1=self.raw(in1), op=op),
                       [in0, in1], [out])

    def stt(self, out, in0, scalar, in1, op0, op1):
        rd = [in0, in1] + ([scalar] if not isinstance(scalar, (int, float)) else [])
        return self.op("dve", lambda e: e.scalar_tensor_tensor(out=self.raw(out), in0=self.raw(in0), scalar=self.raw(scalar),
                                                               in1=self.raw(in1), op0=op0, op1=op1), rd, [out])

    def copy(self, out, in_, eng="dve"):
        if eng == "act":
            return self.op("act", lambda e: e.copy(out=self.raw(out), in_=self.raw(in_)), [in_], [out])
        return self.op(eng, lambda e: e.tensor_copy(out=self.raw(out), in_=self.raw(in_)), [in_], [out])

    def memset(self, ap, val, eng="pool"):
        return self.op(eng, lambda e: e.memset(self.raw(ap), val), [], [ap])

    def red(self, out, in_, op, axis=AX.X):
        return self.op("dve", lambda e: e.tensor_reduce(out=self.raw(out), in_=self.raw(in_), axis=axis, op=op), [in_], [out])

    def recip(self, out, in_):
        return self.op("dve", lambda e: e.reciprocal(out=self.raw(out), in_=self.raw(in_)), [in_], [out])

    def sb(self, es, name, shape, dt):
        self.uid = getattr(self, "uid", 0) + 1
        return es.enter_context(self.nc.sbuf_tensor("%s_u%d" % (name, self.uid), shape, dt))


class Prog:
    def __init__(self, nseq=2, layers=(0, 1), do_mixer=True, do_moe=True, debug=(), branches=(0, 1, 2, 3)):
        self.nseq = nseq
        self.branches = tuple(branches)
        self.layers = tuple(layers)
        self.do_mixer = do_mixer
        self.do_moe = do_moe
        self.debug = set(debug)
        self.nc = bass.Bass("TRN2", target_bir_lowering=False)
        self.k = K(self.nc)
        self.inputs = {}
        self.outputs = {}

    def din(self, name, shape, dt=F32):
        t = self.nc.dram_tensor(name, list(shape), dt, kind="ExternalInput")
        self.inputs[name] = (tuple(shape), dt)
        return t.ap()

    def dout(self, name, shape, dt=F32):
        t = self.nc.dram_tensor(name, list(shape), dt, kind="ExternalOutput")
        self.outputs[name] = (tuple(shape), dt)
        return t.ap()

    def dscr(self, name, shape, dt=F32):
        if name in self.debug:
            return self.dout(name, shape, dt)
        return self.nc.dram_tensor(name, list(shape), dt, kind="Internal").ap()

    def build(self):
        nc, k = self.nc, self.k
        S = self.nseq
        self.xT = self.din("xT", [S, D, T])
        self.outT = self.dout("outT", [S, D, T])
        self.cT = self.din("cT", [128, 8, S])
        self.ada_w = self.din("ada_w", [DEPTH, 2, D, 3 * D])
        self.ada_b = self.din("ada_b", [128, DEPTH * 2 * 3 * 8])
        self.ln_g = self.din("ln_g", [128, DEPTH * 2 * 8])
        self.ln_b = self.din("ln_b", [128, DEPTH * 2 * 8])
        self.router_w = self.din("router_w", [128, 8, NE])
        self.router_b = self.din("router_b", [1, NE])
        self.exp_w1 = self.din("exp_w1", [DEPTH, NE, D, DE])
        self.exp_w3 = self.din("exp_w3", [DEPTH, NE, D, DE])
        self.exp_w2 = self.din("exp_w2", [DEPTH, NE, DE, D])
        self.c_sel16 = self.din("c_sel16", [16, NE * 128])
        self.c_ident = self.din("c_ident", [128, 128])
        self.w_in = self.din("w_in", [DEPTH, D, WIN_COLS])
        self.w_gate = self.din("w_gate", [DEPTH, 4, D, D])
        self.branch_proj = self.din("branch_proj", [DEPTH, 4, 256, D])
        self.w_out = self.din("w_out", [DEPTH, D, D])
        self.fox_fb = self.din("fox_fb", [4, DEPTH])
        self.c_cmask = self.din("c_cmask", [128, 4, 512])
        self.c_invf = self.din("c_invf", [128, 32])
        self.c_gcols = self.din("c_gcols", [128, 8])
        self.c_gmsk = self.din("c_gmsk", [128, 4])
        self.c_mreset = self.din("c_mreset", [128, 1024])
        self.c_gmaskS = self.din("c_gmaskS", [64, 256])
        self.c_gmaskIT = self.din("c_gmaskIT", [64, 256])
        self.c_blk = self.din("c_blk", [128, 128])
        self.gdn_convw = self.din("gdn_convw", [128, DEPTH, 6, 4])
        self.gdn_pA = self.din("gdn_pA", [128, DEPTH])
        self.gdn_pDt = self.din("gdn_pDt", [128, DEPTH])
        self.gdn_nw = self.din("gdn_nw", [64, DEPTH, 64])
        if getattr(self, "gdn_lvl", 9) == 2.47:
            self.dbgout = self.dout("dbgout", [16, 64, 768])
        self.gqn = self.dscr("gqn", [256, T], BF16)
        self.gkn = self.dscr("gkn", [256, T], BF16)
        self.gvs = self.dscr("gvs", [256, T], BF16)
        self.c_cmpmask = self.din("c_cmpmask", [128, 5, 512])
        self.c_bmask = self.din("c_bmask", [128, 4, 512])
        self.c_E2 = self.din("c_E2", [64, 32, 128])
        self.c_ovl = self.din("c_ovl", [128, 2, 64])
        self.c_selA = self.din("c_selA", [128, 32, 64])
        self.c_selB = self.din("c_selB", [128, 32, 64])
        self.nsa_pe = self.din("nsa_pe", [128, DEPTH, 32])
        self.nsa_ck_w1 = self.din("nsa_ck_w1", [DEPTH, 2048, 256])
        self.nsa_cv_w1 = self.din("nsa_cv_w1", [DEPTH, 2048, 256])
        self.nsa_ck_w2 = self.din("nsa_ck_w2", [DEPTH, 256, 64])
        self.nsa_cv_w2 = self.din("nsa_cv_w2", [DEPTH, 256, 64])
        self.gsD = self.dscr("gsD", [12, T], F32)
        self.c_decT = self.din("c_decT", [128, 4, 128])
        self.c_xiT = self.din("c_xiT", [64, 4, 128])
        self.c_zt = self.din("c_zt", [128, 4])
        self.c_cd = self.din("c_cd", [64, 4, 64])
        self.ret_gnw = self.din("ret_gnw", [128, DEPTH * 2])
        self.posT = self.din("posT", [S, 128, 32], I32)
        self.projF = self.dscr("projF", [(NFM - 1) * 128, T], BF16)
        self.projS = self.dscr("projS", [128, T], F32)
        self.projT = self.dscr("projT", [T, NTM], BF16)
        self.obrT = self.dscr("obrT", [4, 256, T], BF16)
        self.xa = self.dscr("xa", [S, D, T])
        self.xb = self.dscr("xb", [S, D, T])

        es = self.k.es
        self.ps = [es.enter_context(nc.psum_tensor("ps%d" % i, [128, 512], F32)) for i in range(8)]
        self.modv = k.sb(es, "modv", [128, DEPTH * 2 * 3 * 8 * S], F32)
        self.lng = k.sb(es, "lng", [128, DEPTH * 2 * 8], F32)
        self.lnb = k.sb(es, "lnb", [128, DEPTH * 2 * 8], F32)
        self.onesD = k.sb(es, "onesD", [128, 128], F32)
        self.epsc = k.sb(es, "epsc", [128, 1], F32)
        self.ident = k.sb(es, "ident", [128, 128], F32)
        self.identb = k.sb(es, "identb", [128, 128], BF16)
        k.memset(self.onesD[:], 1.0 / D)
        self.ones = k.sb(es, "ones", [128, 128], F32)
        self.onec = k.sb(es, "onec", [128, 1], F32)
        k.memset(self.ones[:], 1.0)
        k.memset(self.onec[:], 1.0)
        k.memset(self.epsc[:], LN_EPS)
        self.epsln = k.sb(es, "epsln", [128, 1], F32)
        k.memset(self.epsln[:], LN_EPS / (ALPHA * ALPHA))
        k.dma(self.lng[:], self.ln_g)
        k.dma(self.lnb[:], self.ln_b)
        k.dma(self.ident[:], self.c_ident)
        k.copy(self.identb[:], self.ident[:])

        self.phase_mod()
        cur = [self.xT[s] for s in range(S)]
        for l in self.layers:
            if self.do_mixer:
                nxt = [self.xa[s] for s in range(S)]
                for s in range(S):
                    self.phase_mixer(l, s, cur[s], nxt[s])
                cur = nxt
            if self.do_moe:
                last = (l == self.layers[-1])
                nxt = [self.outT[s] if last else self.xb[s] for s in range(S)]
                self.phase_moe(l, cur, nxt)
                cur = nxt
        k.barrier()
        es.close()
        return nc

    def mcol(self, l, sub, j, kc, s):
        i = ((((l * 2 + sub) * 3 + j) * 8 + kc) * self.nseq + s)
        return self.modv[:, i:i + 1]

    def lcol(self, t, l, sub, kc):
        i = (l * 2 + sub) * 8 + kc
        return t[:, i:i + 1]

    def phase_mod(self):
        nc, k, S = self.nc, self.k, self.nseq
        with ExitStack() as es:
            ct = k.sb(es, "pm_ct", [128, 8, S], F32)
            sc = k.sb(es, "pm_sc", [128, 8, S], F32)
            adab = k.sb(es, "pm_adab", [128, DEPTH * 2 * 3 * 8], F32)
            wsl = [k.sb(es, "pm_w%d" % i, [128, 8, 1024], F32) for i in range(2)]
            k.dma(ct[:], self.cT)
            k.dma(adab[:], self.ada_b)
            k.act(sc[:], ct[:], AF.Silu)
            i = 0
            for l in range(DEPTH):
                for sub in range(2):
                    for j in range(3):
                        w = wsl[i % 2]
                        i += 1
                        src = self.ada_w[l, sub, :, j * 1024:(j + 1) * 1024].rearrange("(kc p) f -> p kc f", p=128)
                        for h in range(2):
                            k.dma(w[:, h * 4:(h + 1) * 4, :], src[:, h * 4:(h + 1) * 4, :], q="sp" if h == 0 else "pool")
                        pst = self.ps[i % 2]
                        for cc in range(8):
                            for kc in range(8):
                                k.mm(pst[:, cc * S:(cc + 1) * S], w[:, kc, cc * 128:(cc + 1) * 128], sc[:, kc, :],
                                     start=(kc == 0), stop=(kc == 7))
                        base = ((l * 2 + sub) * 3 + j) * 8
                        for cc in range(8):
                            o = self.modv[:, (base + cc) * S:(base + cc + 1) * S]
                            if j == 2:
                                k.ts(o, pst[:, cc * S:(cc + 1) * S], adab[:, base + cc:base + cc + 1], ALU.add,
                                     s2=1.0 / ALPHA, op1=ALU.mult)
                            else:
                                k.ts(o, pst[:, cc * S:(cc + 1) * S], adab[:, base + cc:base + cc + 1], ALU.add,
                                     s2=(1.0 if j == 1 else 0.0), op1=ALU.add)
            k.barrier()

    def ln_tile(self, zb, sqb, outb, msb, vsb, l, sub, pM, pQ):
        k = self.k
        for kc in range(8):
            k.act(sqb[:, kc, :], zb[:, kc, :], AF.Square)
        for kc in range(8):
            k.mm(pM[:], self.onesD[:], zb[:, kc, :], start=(kc == 0), stop=(kc == 7))
        for kc in range(8):
            k.mm(pQ[:], self.onesD[:], sqb[:, kc, :], start=(kc == 0), stop=(kc == 7))
        k.copy(msb[:], pM[:], eng="act")
        k.act(vsb[:], pM[:], AF.Square)
        k.tt(vsb[:], pQ[:], vsb[:], ALU.subtract)
        k.act(vsb[:], vsb[:], AF.Sqrt, bias=self.epsln[:])
        k.recip(vsb[:], vsb[:])
        for kc in range(8):
            k.tt(zb[:, kc, :], zb[:, kc, :], msb[:], ALU.subtract, eng="pool")
            k.tt(zb[:, kc, :], zb[:, kc, :], vsb[:], ALU.mult)
            k.act(outb[:, kc, :], zb[:, kc, :], AF.Identity, bias=self.lcol(self.lnb, l, sub, kc),
                  scale=self.lcol(self.lng, l, sub, kc))

    def phase_moe(self, l, src, dst):
        nc, k, S = self.nc, self.k, self.nseq
        ST = 1024
        NT = ST // 512
        ps = self.ps
        with ExitStack() as es:
            hT = k.sb(es, "mo_hT", [128, 8, ST], BF16)
            cTt = k.sb(es, "mo_cT", [16, ST], F32)
            hT2 = k.sb(es, "mo_hT2", [128, 8, ST], BF16)
            cTt2 = k.sb(es, "mo_cT2", [16, ST], F32)
            rt4 = [k.sb(es, "mo_rt4_%d" % i, [128, 12, NE], F32) for i in range(4)]
            rs4 = [k.sb(es, "mo_rs4_%d" % i, [128, 16], F32) for i in range(4)]
            yacc = k.sb(es, "mo_yacc", [128, 8, ST], F32)
            w1b = [k.sb(es, "mo_w1_%d" % i, [128, 8, DE], BF16) for i in range(2)]
            w3b = [k.sb(es, "mo_w3_%d" % i, [128, 8, DE], BF16) for i in range(2)]
            w2b = [k.sb(es, "mo_w2_%d" % i, [128, 4, D], BF16) for i in range(2)]
            xb = k.sb(es, "mo_xb", [128, 8, 512], F32)
            zb = k.sb(es, "mo_zb", [128, 8, 512], F32)
            t1 = [k.sb(es, "mo_t1_%d" % i, [128, 512], F32) for i in range(2)]
            t2 = [k.sb(es, "mo_t2_%d" % i, [128, 512], F32) for i in range(2)]
            hid = [k.sb(es, "mo_hid%d" % i, [128, 4, 512], BF16) for i in range(2)]
            bcS = [k.sb(es, "mo_bc%d" % i, [128, 512], F32) for i in range(2)]
            msb = k.sb(es, "mo_msb", [128, 512], F32)
            vsb = k.sb(es, "mo_vsb", [128, 512], F32)
            rw = k.sb(es, "mo_rw", [128, 8, NE], F32)
            rb = k.sb(es, "mo_rb", [128, NE], F32)
            sel = k.sb(es, "mo_sel", [16, NE * 128], F32)
            rt = k.sb(es, "mo_rt", [128, 12, NE], F32)
            rs = k.sb(es, "mo_rs", [128, 16], F32)
            k.dma(rw[:], self.router_w)
            k.dma(rb[:], self.router_b.broadcast_to([128, NE]))
            k.dma(sel[:], self.c_sel16)

            def load_w(e, buf):
                k.dma(w1b[buf][:], self.exp_w1[l, e].rearrange("(kc p) f -> p kc f", p=128), q="pool")
                k.dma(w3b[buf][:], self.exp_w3[l, e].rearrange("(kc p) f -> p kc f", p=128), q="pool")
                k.dma(w2b[buf][:], self.exp_w2[l, e].rearrange("(fc p) d -> p fc d", p=128), q="pool")

            load_w(0, 0)
            load_w(1, 1)
            hTb = [hT, hT2]
            cTb = [cTt, cTt2]
            stiles = [(s, st) for s in range(S) for st in range(T // ST)]

            def emit_prologue(idx):
                s, st = stiles[idx]
                hT_, cT_ = hTb[idx % 2], cTb[idx % 2]
                for tt in range(NT):
                    t0 = st * ST + tt * 512
                    k.dma(xb[:], src[s][:, t0:t0 + 512].rearrange("(kc p) t -> p kc t", p=128))
                    for kc in range(8):
                        k.ts(zb[:, kc, :], xb[:, kc, :], self.mcol(l, 1, 1, kc, s), ALU.mult,
                             s2=self.mcol(l, 1, 0, kc, s), op1=ALU.add)
                        k.copy(hT_[:, kc, tt * 512:(tt + 1) * 512], zb[:, kc, :], eng="pool")
                    pr = ps[0]
                    for q4 in range(4):
                        for kc in range(8):
                            k.mm(pr[:, q4 * 16:(q4 + 1) * 16], zb[:, kc, q4 * 128:(q4 + 1) * 128], rw[:, kc, :],
                                 start=(kc == 0), stop=(kc == 7))
                    pt = ps[0]
                    for q4 in range(4):
                        self.route(pr[:, q4 * 16:(q4 + 1) * 16], rb, rt4[q4], rs4[q4])
                    for q4 in range(4):
                        k.tr(pt[0:16, q4 * 128:(q4 + 1) * 128], rt4[q4][:, 0, :], self.ident[:])
                    k.copy(cT_[:, tt * 512:(tt + 1) * 512], pt[0:16, :], eng="act")

            def emit_epilogue(idx):
                s, st = stiles[idx]
                for tt in range(NT):
                    t0 = st * ST + tt * 512
                    k.dma(xb[:], src[s][:, t0:t0 + 512].rearrange("(kc p) t -> p kc t", p=128))
                    for kc in range(8):
                        k.stt(zb[:, kc, :], yacc[:, kc, tt * 512:(tt + 1) * 512], self.mcol(l, 1, 2, kc, s),
                              xb[:, kc, :], ALU.mult, ALU.add)
                    self.ln_tile(zb, xb, xb, msb, vsb, l, 1, ps[6], ps[7])
                    k.dma(dst[s][:, t0:t0 + 512].rearrange("(kc p) t -> p kc t", p=128), xb[:])

            units = [(e, tt) for e in range(NE) for tt in range(NT)]

            def emit_H(idx, u):
                e, tt = units[u]
                buf = e % 2
                hT_, cT_ = hTb[idx % 2], cTb[idx % 2]
                tok = slice(tt * 512, (tt + 1) * 512)
                j = u % 2
                k.mm(ps[0][:], sel[:, e * 128:(e + 1) * 128], cT_[:, tok])
                k.copy(bcS[j][:], ps[0][:], eng="act")
                for fc in range(4):
                    jj = fc % 2
                    p1, p3 = ps[1 + jj], ps[3 + jj]
                    for kc in range(8):
                        k.mm(p1[:], w1b[buf][:, kc, fc * 128:(fc + 1) * 128], hT_[:, kc, tok],
                             start=(kc == 0), stop=(kc == 7))
                    for kc in range(8):
                        k.mm(p3[:], w3b[buf][:, kc, fc * 128:(fc + 1) * 128], hT_[:, kc, tok],
                             start=(kc == 0), stop=(kc == 7))
                    k.act(t1[jj][:], p1[:], AF.Silu)
                    k.tt(t2[jj][:], t1[jj][:], bcS[j][:], ALU.mult, eng="pool")
                    k.tt(hid[j][:, fc, :], t2[jj][:], p3[:], ALU.mult)

            def emit_Y(idx, u):
                e, tt = units[u]
                buf = e % 2
                tok = slice(tt * 512, (tt + 1) * 512)
                j = u % 2
                for dc in range(8):
                    py = ps[5 + (u * 8 + dc) % 3]
                    for fc in range(4):
                        k.mm(py[:], w2b[buf][:, fc, dc * 128:(dc + 1) * 128], hid[j][:, fc, :],
                             start=(fc == 0), stop=(fc == 3))
                    if e == 0:
                        k.copy(yacc[:, dc, tok], py[:])
                    else:
                        k.tt(yacc[:, dc, tok], yacc[:, dc, tok], py[:], ALU.add)

            emit_prologue(0)
            for idx in range(len(stiles)):
                last_st = (idx == len(stiles) - 1)
                emit_H(idx, 0)
                for u in range(len(units)):
                    if u + 1 < len(units):
                        emit_H(idx, u + 1)
                    emit_Y(idx, u)
                    e_done, tt_done = units[u]
                    if tt_done == NT - 1:
                        if not (last_st and e_done + 2 >= NE):
                            load_w((e_done + 2) % NE, e_done % 2)
                    if u == len(units) // 2 and not last_st:
                        emit_prologue(idx + 1)
                emit_epilogue(idx)
            k.barrier()

    def route(self, logits, rb, rt, rs):
        k = self.k
        s_ = rt[:, 1, :]
        a = rt[:, 2, :]
        k.act(s_, logits, AF.Sigmoid)
        k.tt(a, s_, rb[:], ALU.add)
        a3 = a.rearrange("p (g e) -> p g e", e=4)
        m1 = rs[:, 0:4]
        m2 = rs[:, 4:8]
        k.red(m1, a3, ALU.max)
        oh = rt[:, 3, :].rearrange("p (g e) -> p g e", e=4)
        k.tt(oh, a3, m1.unsqueeze(2).broadcast_to([128, 4, 4]), ALU.is_equal)
        a2 = rt[:, 4, :].rearrange("p (g e) -> p g e", e=4)
        k.stt(a2, oh, -1.0e9, a3, ALU.mult, ALU.add)
        k.red(m2, a2, ALU.max)
        gs = rs[:, 8:12]
        k.tt(gs, m1, m2, ALU.add)
        gm = rs[:, 12:13]
        k.red(gm, gs, ALU.max)
        gse = rt[:, 5, 0:4]
        k.ts(gse, gs, gm, ALU.is_equal)
        selm = rt[:, 6, :].rearrange("p (g e) -> p g e", e=4)
        k.tt(selm, a3, m2.unsqueeze(2).broadcast_to([128, 4, 4]), ALU.is_ge)
        k.tt(selm, selm, gse.unsqueeze(2).broadcast_to([128, 4, 4]), ALU.mult)
        w = rt[:, 7, :]
        k.tt(w, rt[:, 6, :], s_, ALU.mult)
        ws = rs[:, 13:14]
        k.red(ws, w, ALU.add)
        k.recip(rs[:, 14:15], ws)
        k.ts(rt[:, 0, :], w, rs[:, 14:15], ALU.mult)


FM_GQ, FM_GK, FM_GV, FM_GZ, FM_RG, FM_NQ, FM_FQ, FM_FK, FM_NC, FM_NK, FM_SM = 0, 2, 4, 6, 8, 10, 12, 14, 16, 17, 18
NFM = 19
TM_RQ, TM_RK, TM_RV, TM_FV, TM_NVS, TM_NVW = 0, 256, 512, 768, 1024, 1088
NTM = 1152
WIN_COLS = NFM * 128 + NTM


def _mixer_methods():
    def phase_proj(self, l, s, src):
        nc, k = self.nc, self.k
        ps = self.ps
        with ExitStack() as es:
            win = k.sb(es, "pj_win", [128, 8, WIN_COLS], BF16)
            xb = [k.sb(es, "pj_xb%d" % i, [128, 8, 512], F32) for i in range(2)]
            hT = [k.sb(es, "pj_hT%d" % i, [128, 8, 512], BF16) for i in range(2)]
            stF = [k.sb(es, "pj_stF%d" % i, [128, 512], BF16) for i in range(4)]
            stS = k.sb(es, "pj_stS", [128, 512], F32)
            stT = [k.sb(es, "pj_stT%d" % i, [128, NTM], BF16) for i in range(2)]
            for kc in range(8):
                k.dma(win[:, kc, :], self.w_in[l, kc * 128:(kc + 1) * 128, :], q="pool")
            NTT = T // 512
            k.dma(xb[0][:], src[:, 0:512].rearrange("(kc p) t -> p kc t", p=128))
            ev = 0
            for tt in range(NTT):
                t0 = tt * 512
                b = tt % 2
                if tt + 1 < NTT:
                    k.dma(xb[1 - b][:], src[:, t0 + 512:t0 + 1024].rearrange("(kc p) t -> p kc t", p=128))
                for kc in range(8):
                    k.ts(hT[b][:, kc, :], xb[b][:, kc, :], self.mcol(l, 0, 1, kc, s), ALU.mult,
                         s2=self.mcol(l, 0, 0, kc, s), op1=ALU.add)
                for ch in range(NFM):
                    pp = ps[ch % 4]
                    for kc in range(8):
                        k.mm(pp[:], win[:, kc, ch * 128:(ch + 1) * 128], hT[b][:, kc, :], start=(kc == 0), stop=(kc == 7))
                    if ch == FM_SM:
                        k.copy(stS[:], pp[:], eng="act")
                        k.dma(self.projS[:, t0:t0 + 512], stS[:])
                    else:
                        st = stF[ev % 4]
                        k.copy(st[:], pp[:], eng=("act" if ev % 2 == 0 else "dve"))
                        ev += 1
                        k.dma(self.projF[ch * 128:(ch + 1) * 128, t0:t0 + 512], st[:])
                for q4 in range(4):
                    st = stT[q4 % 2]
                    for gi, (c0, cw) in enumerate(((0, 512), (512, 512), (1024, 128))):
                        pp = ps[4 + (q4 * 3 + gi) % 4]
                        for kc in range(8):
                            k.mm(pp[:, 0:cw], hT[b][:, kc, q4 * 128:(q4 + 1) * 128],
                                 win[:, kc, NFM * 128 + c0:NFM * 128 + c0 + cw], start=(kc == 0), stop=(kc == 7))
                        k.copy(st[:, c0:c0 + cw], pp[:, 0:cw], eng=("act" if gi % 2 == 0 else "dve"))
                    k.dma(self.projT[t0 + q4 * 128:t0 + (q4 + 1) * 128, :], st[:])
            k.barrier()

    def attn_finalize(self, pO, rr, pB, osb, outsb, gate_row=None):
        k = self.k
        k.ts(rr[64:65, :], pO[64:65, :], 1e-30, ALU.max)
        k.recip(rr[64:65, :], rr[64:65, :])
        if gate_row is not None:
            k.tt(rr[64:65, :], rr[64:65, :], gate_row, ALU.mult)
        k.mm(pB[0:64, :], self.ones[64:65, 0:64], rr[64:65, :])
        k.copy(osb[0:64, :], pO[0:64, :], eng="act")
        k.tt(outsb, osb[0:64, :], pB[0:64, :], ALU.mult)

    def run_attn_loops(self, loops, look=2):
        flat = []
        for li, L in enumerate(loops):
            for j in range(L["n"]):
                flat.append((li, j))
        n = len(flat)
        pend = []
        for t in range(n + look):
            if t < n:
                li, j = flat[t]
                loops[li]["qk"](j, t)
            g = t - look
            if g >= 0:
                li, j = flat[g]
                loops[li]["pv"](j, g)
                if j == loops[li]["n"] - 1:
                    pend.append(loops[li]["fin"])
                elif j == 1 and pend:
                    for f in pend:
                        f()
                    pend.clear()
        for f in pend:
            f()

    def phase_fox(self, l, s):
        nc, k = self.nc, self.k
        ps = self.ps
        with ExitStack() as es:
            ff = k.sb(es, "fx_ff", [4, T], F32)
            cs = k.sb(es, "fx_cs", [4, T], F32)
            fb = k.sb(es, "fx_fb", [4, DEPTH], F32)
            nfb = k.sb(es, "fx_nfb", [4, 1], F32)
            ckT = k.sb(es, "fx_ckT", [128, 32, 4], F32)
            rhsd = k.sb(es, "fx_rhsd", [4, 8, 4], F32)
            crefB = k.sb(es, "fx_cref", [128, 8, 4], F32)
            biasall = k.sb(es, "fx_bias", [128, 8, 4, 32], F32)
            qT2 = k.sb(es, "fx_qT", [128, T], BF16)
            kT2 = k.sb(es, "fx_kT", [128, T], BF16)
            vaug = k.sb(es, "fx_v", [128, 32, 4, 128], BF16)
            kTz = [k.sb(es, "fx_kTz%d" % i, [128, T], BF16) for i in range(2)]
            cm = k.sb(es, "fx_cm", [128, 4, 512], BF16)
            Pt = [k.sb(es, "fx_P%d" % i, [128, 512], BF16) for i in range(3)]
            rr = k.sb(es, "fx_rr", [65, 512], F32)
            osb = k.sb(es, "fx_osb", [64, 512], F32)
            outsb = [k.sb(es, "fx_out%d" % i, [64, 512], BF16) for i in range(2)]
            k.dma(cm[:], self.c_cmask, q="pool")
            k.dma(ff[:], self.projS[20:24, :])
            k.dma(fb[:], self.fox_fb)
            k.ts(nfb[:], fb[0:4, l:l + 1], -1.0, ALU.mult)
            k.act(ff[:], ff[:], AF.Exp, bias=nfb[:], scale=-1.0)
            k.act(ff[:], ff[:], AF.Ln, bias=self.onec[0:4, :])
            k.op("dve", lambda e: e.tensor_tensor_scan(out=cs[:], data0=ff[:], data1=ff[:], initial=0.0,
                                                        op0=ALU.add, op1=ALU.max), [ff[:]], [cs[:]])
            pt = ps[0]
            for j in range(32):
                k.tr(pt[:, j * 4:(j + 1) * 4], cs[0:4, j * 128:(j + 1) * 128], self.ident[0:4, 0:4])
            k.copy(ckT[:].rearrange("p a b -> p (a b)"), pt[:, 0:128])
            mids = cs[0:4, :].rearrange("p (i t) -> p i t", t=512)[:, :, 255:256]
            k.tt(rhsd[:], mids.broadcast_to([4, 8, 4]), self.ident[0:4, 0:4].unsqueeze(1).broadcast_to([4, 8, 4]), ALU.mult)
            k.mm(ps[1][:, 0:32], self.ones[0:4, :], rhsd[:].rearrange("p a b -> p (a b)"))
            k.copy(crefB[:].rearrange("p a b -> p (a b)"), ps[1][:, 0:32])
            for i in range(8):
                for h in range(4):
                    k.ts(biasall[:, i, h, :], ckT[:, :, h], crefB[:, i, h:h + 1], ALU.subtract)
            k.memset(vaug[:, :, :, 64:128], 0.0)
            k.memset(vaug[:, :, :, 64:65], 1.0)
            k.memset(kTz[0][64:128, :], 0.0)
            k.memset(kTz[1][0:64, :], 0.0)
            for h in range(4):
                k.dma(vaug[:, :, h, 0:64],
                      self.projT[:, TM_FV + h * 64:TM_FV + (h + 1) * 64].rearrange("(j p) d -> p j d", p=128))
            ob = 0
            for hp in range(2):
                k.dma(qT2[:], self.projF[(FM_FQ + hp) * 128:(FM_FQ + hp + 1) * 128, :])
                k.dma(kT2[:], self.projF[(FM_FK + hp) * 128:(FM_FK + hp + 1) * 128, :])
                k.copy(kTz[0][0:64, :], kT2[0:64, :], eng="pool")
                k.copy(kTz[1][64:128, :], kT2[64:128, :], eng="pool")
                loops = []
                for h2 in range(2):
                    hh = hp * 2 + h2
                    po = 64 * h2
                    for i in range(8):
                        njt = 4 * (i + 1)
                        pO = ps[4 + len(loops) % 2]

                        def qk(j, t, i=i, hh=hh, po=po):
                            pS = ps[1 + t % 3]
                            diag = j >= 4 * i
                            k.mm(pS[:], kTz[po // 64][:, j * 128:(j + 1) * 128], qT2[:, i * 512:(i + 1) * 512],
                                 start=True, stop=(not diag))
                            if diag:
                                k.mm(pS[:], self.identb[:], cm[:, j - 4 * i, :], start=False, stop=True)
                            k.act(Pt[t % 3][:], pS[:], AF.Exp, bias=biasall[:, i, hh, j:j + 1], scale=0.125)

                        def pv(j, t, hh=hh, pO=pO, njt=njt):
                            k.mm(pO[:, :], vaug[:, j, hh, :], Pt[t % 3][:], start=(j == 0), stop=(j == njt - 1))

                        def fin(pO=pO, i=i, hh=hh, o=outsb[ob % 2]):
                            self.attn_finalize(pO, rr, ps[6], osb, o[:])
                            k.dma(self.obrT[3, hh * 64:(hh + 1) * 64, i * 512:(i + 1) * 512], o[:])
                        ob += 1
                        loops.append(dict(n=njt, qk=qk, pv=pv, fin=fin))
                self.run_attn_loops(loops)
            k.barrier()

    def phase_merge(self, l, s, src, dst):
        nc, k = self.nc, self.k
        ps = self.ps
        brs = self.branches
        with ExitStack() as es:
            wg = k.sb(es, "mg_wg", [128, 4, 8, D], BF16)
            bp = k.sb(es, "mg_bp", [128, 4, 2, D], BF16)
            wo = k.sb(es, "mg_wo", [128, 8, D], BF16)
            xb = k.sb(es, "mg_xb", [128, 8, 512], F32)
            zb = k.sb(es, "mg_zb", [128, 8, 512], F32)
            hT = k.sb(es, "mg_hT", [128, 8, 512], BF16)
            oTt = k.sb(es, "mg_oT", [128, 4, 2, 512], BF16)
            sig = [k.sb(es, "mg_sig%d" % i, [128, 512], F32) for i in range(2)]
            tmp = [k.sb(es, "mg_tmp%d" % i, [128, 512], F32) for i in range(2)]
            mer = [k.sb(es, "mg_mer%d" % i, [128, 512], F32) for i in range(2)]
            merged = k.sb(es, "mg_merged", [128, 8, 512], BF16)
            msb = k.sb(es, "mg_msb", [128, 512], F32)
            vsb = k.sb(es, "mg_vsb", [128, 512], F32)
            for br in range(4):
                for half in range(2):
                    k.dma(wg[:, br, half * 4:(half + 1) * 4, :],
                          self.w_gate[l, br, half * 512:(half + 1) * 512, :].rearrange("(kc p) f -> p kc f", p=128), q="pool")
                k.dma(bp[:, br, :, :], self.branch_proj[l, br].rearrange("(c p) f -> p c f", p=128), q="pool")
            k.dma(wo[:], self.w_out[l].rearrange("(kc p) f -> p kc f", p=128), q="pool")
            cnt = 0
            for tt in range(T // 512):
                t0 = tt * 512
                k.dma(xb[:], src[:, t0:t0 + 512].rearrange("(kc p) t -> p kc t", p=128))
                for br in brs:
                    k.dma(oTt[:, br, :, :], self.obrT[br, :, t0:t0 + 512].rearrange("(c p) t -> p c t", p=128))
                for kc in range(8):
                    k.ts(hT[:, kc, :], xb[:, kc, :], self.mcol(l, 0, 1, kc, s), ALU.mult,
                         s2=self.mcol(l, 0, 0, kc, s), op1=ALU.add)
                for dc in range(8):
                    m = mer[dc % 2]
                    for bi, br in enumerate(brs):
                        pG = ps[cnt % 2]
                        pBp = ps[2 + cnt % 2]
                        sg = sig[cnt % 2]
                        cnt += 1
                        for kc in range(8):
                            k.mm(pG[:], wg[:, br, kc, dc * 128:(dc + 1) * 128], hT[:, kc, :], start=(kc == 0), stop=(kc == 7))
                        for c in range(2):
                            k.mm(pBp[:], bp[:, br, c, dc * 128:(dc + 1) * 128], oTt[:, br, c, :], start=(c == 0), stop=(c == 1))
                        k.act(sg[:], pG[:], AF.Sigmoid)
                        if bi == 0:
                            k.tt(m[:], sg[:], pBp[:], ALU.mult)
                        else:
                            tp = tmp[bi % 2]
                            k.tt(tp[:], sg[:], pBp[:], ALU.mult)
                            k.tt(m[:], m[:], tp[:], ALU.add, eng="pool")
                    k.copy(merged[:, dc, :], m[:], eng="pool")
                for d2 in range(8):
                    pY = ps[4 + d2 % 2]
                    for dc in range(8):
                        k.mm(pY[:], wo[:, dc, d2 * 128:(d2 + 1) * 128], merged[:, dc, :], start=(dc == 0), stop=(dc == 7))
                    k.stt(zb[:, d2, :], pY[:], self.mcol(l, 0, 2, d2, s), xb[:, d2, :], ALU.mult, ALU.add)
                self.ln_tile(zb, xb, xb, msb, vsb, l, 0, ps[6], ps[7])
                k.dma(dst[:, t0:t0 + 512].rearrange("(kc p) t -> p kc t", p=128), xb[:])
            k.barrier()

    def phase_mixer(self, l, s, src, dst):
        if getattr(self, "probe_conv", 0):
            self.phase_gdn_conv(l, s)
            return
        self.phase_proj(l, s, src)
        if 0 in self.branches:
            self.phase_gdn(l, s)
        if 1 in self.branches:
            self.phase_ret(l, s)
        if 2 in self.branches:
            self.phase_nsa(l, s)
        if 3 in self.branches:
            self.phase_fox(l, s)
        self.phase_merge(l, s, src, dst)

    def phase_ret(self, l, s):
        nc, k = self.nc, self.k
        ps = self.ps
        psb = [p_[:].bitcast(BF16) for p_ in ps]
        TWO_PI = 2.0 * math.pi
        with ExitStack() as es:
            invf = k.sb(es, "rt_invf", [128, 32], F32)
            decT = k.sb(es, "rt_decT", [128, 4, 128], F32)
            xiT = k.sb(es, "rt_xiT", [64, 4, 128], F32)
            zt = k.sb(es, "rt_zt", [128, 4], F32)
            cdt = k.sb(es, "rt_cd", [64, 4, 64], F32)
            gnw = k.sb(es, "rt_gnw", [128, DEPTH * 2], F32)
            posi = k.sb(es, "rt_posi", [128, 32], I32)
            posf = k.sb(es, "rt_posf", [128, 32], F32)
            ang = k.sb(es, "rt_ang", [128, 32, 32], F32)
            tmpa = k.sb(es, "rt_tmpa", [128, 32, 32], F32)
            tmpi = k.sb(es, "rt_tmpi", [128, 32, 32], I32)
            cosT = k.sb(es, "rt_cos", [128, 32, 32], F32)
            sinT = k.sb(es, "rt_sin", [128, 32, 32], F32)
            tq = [k.sb(es, "rt_tq%d" % i, [128, 768], BF16) for i in range(2)]
            gt = [k.sb(es, "rt_gt%d" % i, [128, 2, 128], BF16) for i in range(2)]
            sg = k.sb(es, "rt_sg", [128, 2, 128], F32)
            ra = [k.sb(es, "rt_ra%d" % i, [128, 4, 32], F32) for i in range(4)]
            qr = k.sb(es, "rt_qr", [128, 4, 64], BF16)
            kr = k.sb(es, "rt_kr", [128, 4, 64], BF16)
            kz = k.sb(es, "rt_kz", [128, 4, 64], BF16)
            qrT = k.sb(es, "rt_qrT", [64, 4, 128], BF16)
            qxT = k.sb(es, "rt_qxT", [64, 4, 128], BF16)
            krT = k.sb(es, "rt_krT", [64, 4, 128], BF16)
            Sd = [k.sb(es, "rt_Sd%d" % i, [128, 128], BF16) for i in range(2)]
            st32 = k.sb(es, "rt_st32", [64, 4, 64], F32)
            stb = k.sb(es, "rt_stb", [64, 4, 64], BF16)
            o32 = k.sb(es, "rt_o32", [128, 4, 64], F32)
            st6 = k.sb(es, "rt_st6", [128, 4, 6], F32)
            mv = k.sb(es, "rt_mv", [128, 4, 2], F32)
            rstd = k.sb(es, "rt_rstd", [128, 4], F32)
            on = k.sb(es, "rt_on", [128, 4, 64], BF16)
            oT = k.sb(es, "rt_oT", [128, 2, 512], BF16)
            k.dma(invf[:], self.c_invf)
            k.dma(decT[:], self.c_decT)
            k.dma(xiT[:], self.c_xiT)
            k.dma(zt[:], self.c_zt)
            k.dma(cdt[:], self.c_cd)
            k.dma(gnw[:], self.ret_gnw)
            k.dma(posi[:], self.posT[s])
            k.copy(posf[:], posi[:])
            k.tt(ang[:], posf[:].unsqueeze(2).broadcast_to([128, 32, 32]), invf[:].unsqueeze(1).broadcast_to([128, 32, 32]), ALU.mult)

            def sin_of(dst, shift):
                k.ts(tmpa[:], ang[:], 1.0 / TWO_PI, ALU.mult, s2=shift / TWO_PI + 0.5, op1=ALU.add)
                k.copy(tmpi[:], tmpa[:])
                k.copy(tmpa[:], tmpi[:])
                k.stt(tmpa[:], tmpa[:], -TWO_PI, ang[:], ALU.mult, ALU.add)
                if shift != 0.0:
                    k.ts(tmpa[:], tmpa[:], shift, ALU.add)
                k.ts(dst, tmpa[:], -math.pi, ALU.is_lt, s2=TWO_PI, op1=ALU.mult)
                k.tt(tmpa[:], tmpa[:], dst, ALU.add)
                k.ts(dst, tmpa[:], math.pi, ALU.is_gt, s2=-TWO_PI, op1=ALU.mult)
                k.tt(tmpa[:], tmpa[:], dst, ALU.add)
                k.ts(tmpa[:], tmpa[:], math.pi, ALU.min, s2=-math.pi, op1=ALU.max)
                k.act(dst, tmpa[:], AF.Sin)

            sin_of(sinT[:], 0.0)
            sin_of(cosT[:], math.pi / 2.0)
            k.memset(st32[:], 0.0)
            k.memset(stb[:], 0.0)

            def rope(dst, src4, j):
                cb = cosT[:, j, :].unsqueeze(1).broadcast_to([128, 4, 32])
                sb_ = sinT[:, j, :].unsqueeze(1).broadcast_to([128, 4, 32])
                x1 = src4[:, :, 0:32]
                x2 = src4[:, :, 32:64]
                k.tt(ra[0][:], x1, cb, ALU.mult, eng="pool")
                k.tt(ra[1][:], x2, sb_, ALU.mult, eng="pool")
                k.tt(dst[:, :, 0:32], ra[0][:], ra[1][:], ALU.subtract)
                k.tt(ra[2][:], x1, sb_, ALU.mult, eng="pool")
                k.tt(ra[3][:], x2, cb, ALU.mult, eng="pool")
                k.tt(dst[:, :, 32:64], ra[2][:], ra[3][:], ALU.add)

            NJ = T // 128
            k.dma(tq[0][:], self.projT[0:128, 0:768])
            for j in range(NJ):
                b = j % 2
                if j + 1 < NJ:
                    k.dma(tq[1 - b][:], self.projT[(j + 1) * 128:(j + 2) * 128, 0:768])
                k.dma(gt[b][:], self.projF[FM_RG * 128:(FM_RG + 2) * 128, j * 128:(j + 1) * 128].rearrange("(c p) t -> p c t", p=128))
                q4 = tq[b][:, 0:256].rearrange("p (h d) -> p h d", d=64)
                k4 = tq[b][:, 256:512].rearrange("p (h d) -> p h d", d=64)
                v4 = tq[b][:, 512:768].rearrange("p (h d) -> p h d", d=64)
                rope(qr, q4, j)
                rope(kr, k4, j)
                k.tt(kz[:], kr[:], zt[:].unsqueeze(2).broadcast_to([128, 4, 64]), ALU.mult, eng="pool")
                pq = psb[0]
                pk = psb[1]
                for h in range(4):
                    k.tr(pq[0:64, h * 128:(h + 1) * 128], qr[:, h, :], self.identb[:])
                for h in range(4):
                    k.tr(pk[0:64, h * 128:(h + 1) * 128], kr[:, h, :], self.identb[:])
                k.copy(qrT[:].rearrange("p h t -> p (h t)"), pq[0:64, 0:512], eng="act")
                k.tt(qxT[:].rearrange("p h t -> p (h t)"), pq[0:64, 0:512], xiT[:].rearrange("p h t -> p (h t)"), ALU.mult)
                k.act(krT[:].rearrange("p h t -> p (h t)"), pk[0:64, 0:512], AF.Copy, scale=0.125)
                pO = ps[2 + b]
                pKV = ps[4]
                for h in range(4):
                    pS = ps[5 + h % 2]
                    k.mm(pS[:, 0:128], krT[:, h, :], qrT[:, h, :])
                    sd = Sd[h % 2]
                    k.tt(sd[:], pS[:, 0:128], decT[:, h, :], ALU.mult)
                    k.mm(pO[:, h * 64:(h + 1) * 64], sd[:], v4[:, h, :], start=True, stop=False)
                    k.mm(pO[:, h * 64:(h + 1) * 64], qxT[:, h, :], stb[:, h, :], start=False, stop=True)
                    k.mm(pKV[0:64, h * 64:(h + 1) * 64], kz[:, h, :], v4[:, h, :])
                k.tt(st32[:], st32[:], cdt[:], ALU.mult, eng="pool")
                k.tt(st32[:].rearrange("p h d -> p (h d)"), st32[:].rearrange("p h d -> p (h d)"), pKV[0:64, 0:256], ALU.add)
                k.copy(stb[:], st32[:], eng="pool")
                k.copy(o32[:].rearrange("p h d -> p (h d)"), pO[:, 0:256], eng="act")
                for h in range(4):
                    k.op("dve", lambda e, h=h: e.bn_stats(out=st6[:, h, :], in_=o32[:, h, :]), [o32[:]], [st6[:]])
                for h in range(4):
                    k.op("dve", lambda e, h=h: e.bn_aggr(out=mv[:, h, :], in_=st6[:, h, :]), [st6[:]], [mv[:]])
                k.act(rstd[:], mv[:, :, 1], AF.Sqrt, bias=self.epsc[:])
                k.recip(rstd[:], rstd[:])
                k.tt(o32[:], o32[:], mv[:, :, 0:1].broadcast_to([128, 4, 64]), ALU.subtract)
                k.tt(on[:], o32[:], rstd[:].unsqueeze(2).broadcast_to([128, 4, 64]), ALU.mult)
                k.act(sg[:], gt[b][:], AF.Silu)
                po = psb[7]
                for c in range(2):
                    k.tr(po[:, c * 128:(c + 1) * 128], on[:, 2 * c:2 * c + 2, :].rearrange("p h d -> p (h d)"), self.identb[:])
                jj = j % 4
                for c in range(2):
                    k.stt(oT[:, c, jj * 128:(jj + 1) * 128], po[:, c * 128:(c + 1) * 128], gnw[:, l * 2 + c:l * 2 + c + 1],
                          sg[:, c, :], ALU.mult, ALU.mult)
                if jj == 3:
                    t0 = (j - 3) * 128
                    k.dma(self.obrT[1, :, t0:t0 + 512].rearrange("(c p) t -> p c t", p=128), oT[:])
            k.barrier()

    def phase_nsa(self, l, s):
        nc, k = self.nc, self.k
        ps = self.ps
        psb = [p_[:].bitcast(BF16) for p_ in ps]
        with ExitStack() as es:
            W1 = k.sb(es, "ns_W1", [128, 32, 256], BF16)
            w2k = k.sb(es, "ns_w2k", [128, 2, 64], BF16)
            w2v = k.sb(es, "ns_w2v", [128, 2, 64], BF16)
            pe32 = k.sb(es, "ns_pe32", [128, DEPTH, 32], F32)
            peb = k.sb(es, "ns_peb", [128, 32], BF16)
            nc16 = k.sb(es, "ns_nc16", [128, T], BF16)
            ksT = k.sb(es, "ns_ksT", [128, T], BF16)
            kwT = k.sb(es, "ns_kwT", [128, T], BF16)
            qt = [k.sb(es, "ns_qt%d" % i, [128, 4, 512], BF16) for i in range(2)]
            vsa = k.sb(es, "ns_vsa", [128, 32, 128], BF16)
            vwa = k.sb(es, "ns_vwa", [128, 32, 128], BF16)
            cmpm = k.sb(es, "ns_cmpm", [128, 5, 512], BF16)
            cm = k.sb(es, "ns_cm", [128, 4, 512], BF16)
            bm = k.sb(es, "ns_bm", [128, 4, 512], BF16)
            E2 = k.sb(es, "ns_E2", [128, 32, 128], BF16)
            ovl = k.sb(es, "ns_ovl", [128, 2, 64], BF16)
            selA = k.sb(es, "ns_selA", [128, 32, 64], F32)
            selB = k.sb(es, "ns_selB", [128, 32, 64], F32)
            onesb = k.sb(es, "ns_onesb", [128, 128], BF16)
            hs = k.sb(es, "ns_hs", [128, 2, 2, 256], BF16)
            bias_h = k.sb(es, "ns_bh", [128, 4], F32)
            kcmpT = k.sb(es, "ns_kcmpT", [128, 256], BF16)
            vcmp = k.sb(es, "ns_vcmp", [128, 2, 128], BF16)
            gsp = k.sb(es, "ns_gsp", [128, 1024], F32)
            gt4 = k.sb(es, "ns_gt4", [65, 4, 3, 512], F32)
            PT = [k.sb(es, "ns_PT%d" % i, [128, 512], BF16) for i in range(3)]
            Pn = k.sb(es, "ns_Pn", [128, 4, 2, 512], BF16)
            rD = k.sb(es, "ns_rD", [128, 512], F32)
            sc = k.sb(es, "ns_sc", [128, 64], F32)
            sc2 = k.sb(es, "ns_sc2", [128, 64], F32)
            m8 = k.sb(es, "ns_m8", [128, 16], F32)
            mb = k.sb(es, "ns_mb", [128, 64], BF16)
            MbT = k.sb(es, "ns_MbT", [128, 512], BF16)
            rr = k.sb(es, "ns_rr", [65, 512], F32)
            osb = k.sb(es, "ns_osb", [64, 512], F32)
            tmpo = k.sb(es, "ns_tmpo", [64, 512], F32)
            acc = k.sb(es, "ns_acc", [64, 4, 512], F32)
            outsb = [k.sb(es, "ns_out%d" % i, [64, 512], BF16) for i in range(2)]
            k.dma(W1[0:64, :, :], self.nsa_ck_w1[l].rearrange("(l d) h -> d l h", d=64), q="pool")
            k.dma(W1[64:128, :, :], self.nsa_cv_w1[l].rearrange("(l d) h -> d l h", d=64), q="pool")
            k.dma(w2k[:], self.nsa_ck_w2[l].rearrange("(c p) d -> p c d", p=128), q="pool")
            k.dma(w2v[:], self.nsa_cv_w2[l].rearrange("(c p) d -> p c d", p=128), q="pool")
            k.dma(pe32[:], self.nsa_pe)
            k.copy(peb[:], pe32[:, l, :])
            k.dma(cmpm[:], self.c_cmpmask, q="pool")
            k.dma(cm[:], self.c_cmask, q="pool")
            k.dma(bm[:], self.c_bmask, q="pool")
            k.dma(E2[0:64, :, :], self.c_E2, q="pool")
            k.memset(E2[64:128, :, :], 0.0)
            k.memset(MbT[64:128, :], 0.0)
            k.memset(kcmpT[:], 0.0)
            k.memset(vcmp[:], 0.0)
            for q__ in qt:
                k.memset(q__[64:128, :, :], 0.0)
            k.dma(ovl[:], self.c_ovl, q="pool")
            k.dma(selA[:], self.c_selA)
            k.dma(selB[:], self.c_selB)
            k.memset(onesb[:], 1.0)
            k.dma(nc16[:], self.projF[FM_NC * 128:(FM_NC + 1) * 128, :])
            k.memset(ksT[64:128, :], 0.0)
            k.memset(kwT[64:128, :], 0.0)
            k.dma(ksT[0:64, :], self.projF[FM_NK * 128:FM_NK * 128 + 64, :])
            k.dma(kwT[0:64, :], self.projF[FM_NK * 128 + 64:FM_NK * 128 + 128, :])
            k.memset(vsa[:, :, 64:128], 0.0)
            k.memset(vsa[:, :, 64:65], 1.0)
            k.memset(vwa[:, :, 64:128], 0.0)
            k.memset(vwa[:, :, 64:65], 1.0)
            k.dma(vsa[:, :, 0:64], self.projT[:, TM_NVS:TM_NVS + 64].rearrange("(j p) d -> p j d", p=128))
            k.dma(vwa[:, :, 0:64], self.projT[:, TM_NVW:TM_NVW + 64].rearrange("(j p) d -> p j d", p=128))
            for pc in range(4):
                sl = slice(pc * 1024, (pc + 1) * 1024)
                k.dma(gsp[64:76, :], self.projS[8:20, sl])
                k.act(gsp[64:76, :], gsp[64:76, :], AF.Sigmoid)
                k.dma((self.gsD[:, sl], "gsD"), gsp[64:76, :])
            ncv = nc16[:].rearrange("p (n s) -> p n s", s=16)
            for kv in range(2):
                po = 64 * kv
                for hc in range(2):
                    pb = ps[0][:, kv * 2 + hc:kv * 2 + hc + 1]
                    for l_ in range(32):
                        k.mm(pb, W1[po:po + 64, l_, hc * 128:(hc + 1) * 128], peb[po:po + 64, l_:l_ + 1],
                             start=(l_ == 0), stop=(l_ == 31))
            k.copy(bias_h[:], ps[0][:, 0:4])
            for kv in range(2):
                po = 64 * kv
                for hc in range(2):
                    ph = ps[1 + hc]
                    for l_ in range(32):
                        k.mm(ph[:, 0:255], W1[po:po + 64, l_, hc * 128:(hc + 1) * 128],
                             ncv[po:po + 64, (l_ // 16):(l_ // 16) + 255, l_ % 16], start=(l_ == 0), stop=(l_ == 31))
                    k.act(hs[:, kv, hc, 0:255], ph[:, 0:255], AF.Silu, bias=bias_h[:, kv * 2 + hc:kv * 2 + hc + 1])
            for hc in range(2):
                k.mm(ps[3][0:64, 0:255], w2k[:, hc, :], hs[:, 0, hc, 0:255], start=(hc == 0), stop=(hc == 1))
            k.copy(kcmpT[0:64, 0:255], ps[3][0:64, 0:255], eng="act")
            MC = (128, 127)
            for c in range(2):
                M = MC[c]
                for hc in range(2):
                    k.mm(ps[4][0:M, c * 64:(c + 1) * 64], hs[:, 1, hc, c * 128:c * 128 + M], w2v[:, hc, :],
                         start=(hc == 0), stop=(hc == 1))
                k.copy(vcmp[0:M, c, 0:64], ps[4][0:M, c * 64:(c + 1) * 64])
            ob = 0
            pending = []
            for i in range(8):
                tok = slice(i * 512, (i + 1) * 512)
                q_ = qt[i % 2]
                k.dma(q_[0:64, :, :], self.projF[FM_NQ * 128:(FM_NQ + 2) * 128, tok].rearrange("(h d) t -> d h t", d=64))
                k.dma(gt4[64:65, :, :, :].rearrange("p h b t -> p (h b) t"),
                      (self.gsD[:, tok].rearrange("(o r) t -> o r t", o=1), "gsD"))
                ncs = [0] if i <= 3 else [0, 1]
                for h in range(4):
                    for c in ncs:
                        M = MC[c]
                        pS = ps[c]
                        d_ = 512 * i - 2048 * c
                        need = d_ < 2063
                        k.mm(pS[0:M, :], kcmpT[:, c * 128:c * 128 + M], q_[:, h, :], start=True, stop=(not need))
                        if need:
                            k.mm(pS[0:M, :], self.identb[:, 0:M], cmpm[:, d_ // 512, :], start=False, stop=True)
                        k.act(PT[c][0:M, :], pS[0:M, :], AF.Exp, scale=0.125)
                    for ci, c in enumerate(ncs):
                        M = MC[c]
                        k.mm(ps[2][:], onesb[0:M, :], PT[c][0:M, :], start=(ci == 0), stop=(ci == len(ncs) - 1))
                    k.ts(rD[:], ps[2][:], 1e-18, ALU.max)
                    k.act(rD[:], rD[:], AF.Ln)
                    k.act(rD[:], rD[:], AF.Exp, scale=-1.0)
                    for c in ncs:
                        M = MC[c]
                        k.tt(Pn[0:M, h, c, :], PT[c][0:M, :], rD[0:M, :], ALU.mult, eng="pool")
                    for ci, c in enumerate(ncs):
                        M = MC[c]
                        k.mm(ps[3][:, :], vcmp[0:M, c, :], Pn[0:M, h, c, :], start=(ci == 0), stop=(ci == len(ncs) - 1))
                    k.mm(ps[7][0:64, :], self.ones[64:65, 0:64], gt4[64:65, h, 0, :])
                    k.copy(osb[:], ps[3][0:64, :], eng="act")
                    k.tt(acc[:, h, :], osb[:], ps[7][0:64, :], ALU.mult)
                pT = psb[2]
                pI = ps[4]
                for q4 in range(4):
                    n_mm = 4 * len(ncs)
                    ii = 0
                    for h in range(4):
                        for c in ncs:
                            M = MC[c]
                            k.mm(pI[:, q4 * 64:(q4 + 1) * 64], Pn[0:M, h, c, q4 * 128:(q4 + 1) * 128], ovl[0:M, c, :],
                                 start=(ii == 0), stop=(ii == n_mm - 1))
                            ii += 1
                for q4 in range(4):
                    u = i * 4 + q4
                    k.tt(sc[:], pI[:, q4 * 64:(q4 + 1) * 64], selA[:, u, :], ALU.mult)
                    k.tt(sc[:], sc[:], selB[:, u, :], ALU.add)
                    k.op("dve", lambda e: e.max(out=m8[:, 0:8], in_=sc[:]), [sc[:]], [m8[:]])
                    k.op("dve", lambda e: e.match_replace(out=sc2[:], in_to_replace=m8[:, 0:8], in_values=sc[:], imm_value=-1.0e9),
                         [sc[:], m8[:]], [sc2[:]])
                    k.op("dve", lambda e: e.max(out=m8[:, 8:16], in_=sc2[:]), [sc2[:]], [m8[:]])
                    k.ts(mb[:], sc[:], m8[:, 15:16], ALU.is_ge, s2=1.0, op1=ALU.subtract)
                    k.tr(pT[0:64, q4 * 128:(q4 + 1) * 128], mb[:], self.identb[:])
                k.copy(MbT[0:64, :], pT[0:64, 0:512], eng="act")
                loops = []
                for h in range(4):
                    njt = 4 * (i + 1)

                    def qk_s(j, t, h=h, i=i, q_=q_):
                        pS = ps[t % 4]
                        diag = j >= 4 * i
                        k.mm(pS[:], ksT[:, j * 128:(j + 1) * 128], q_[:, h, :], start=True, stop=False)
                        k.mm(pS[:], E2[:, j, :], MbT[:], start=False, stop=(not diag))
                        if diag:
                            k.mm(pS[:], self.identb[:], cm[:, j - 4 * i, :], start=False, stop=True)
                        k.act(PT[t % 3][:], pS[:], AF.Exp, scale=0.125)

                    def pv_s(j, t, njt=njt):
                        k.mm(ps[5][:, :], vsa[:, j, :], PT[t % 3][:], start=(j == 0), stop=(j == njt - 1))

                    def fin_s(h=h):
                        self.attn_finalize(ps[5], rr, ps[7], osb, tmpo[:], gate_row=gt4[64:65, h, 1, :])
                        k.tt(acc[:, h, :], acc[:, h, :], tmpo[:], ALU.add, eng="pool")
                    loops.append(dict(n=njt, qk=qk_s, pv=pv_s, fin=fin_s))
                    kts = list(range(max(0, 4 * i - 4), 4 * i + 4))

                    def qk_w(j, t, h=h, i=i, q_=q_, kts=kts):
                        kt = kts[j]
                        pS = ps[t % 4]
                        diag = kt >= 4 * i
                        mk = cm[:, kt - 4 * i, :] if diag else bm[:, kt - 4 * i + 4, :]
                        k.mm(pS[:], kwT[:, kt * 128:(kt + 1) * 128], q_[:, h, :], start=True, stop=False)
                        k.mm(pS[:], self.identb[:], mk, start=False, stop=True)
                        k.act(PT[t % 3][:], pS[:], AF.Exp, scale=0.125)

                    def pv_w(j, t, kts=kts):
                        k.mm(ps[6][:, :], vwa[:, kts[j], :], PT[t % 3][:], start=(j == 0), stop=(j == len(kts) - 1))

                    def fin_w(h=h, o=outsb[ob % 2], tok=tok):
                        self.attn_finalize(ps[6], rr, ps[7], osb, tmpo[:], gate_row=gt4[64:65, h, 2, :])
                        k.tt(o[:], acc[:, h, :], tmpo[:], ALU.add)
                        k.dma(self.obrT[2, h * 64:(h + 1) * 64, tok], o[:])
                    ob += 1
                    loops.append(dict(n=len(kts), qk=qk_w, pv=pv_w, fin=fin_w))
                self.run_attn_loops(loops)
            k.barrier()

    def phase_gdn_conv(self, l, s):
        nc, k = self.nc, self.k
        ps = self.ps
        with ExitStack() as es:
            cw = k.sb(es, "gc_cw", [128, DEPTH, 6, 4], F32)
            blk = k.sb(es, "gc_blk", [128, 128], BF16)
            gcol = k.sb(es, "gc_gcol", [128, 8], F32)
            xpad = [k.sb(es, "gc_xp%d" % i, [128, 4 + T], F32) for i in range(2)]
            acc = k.sb(es, "gc_acc", [128, T], F32)
            ys = k.sb(es, "gc_ys", [128, T], F32)
            sq = k.sb(es, "gc_sq", [128, T], BF16)
            rin = [k.sb(es, "gc_rin%d" % i, [128, 512], F32) for i in range(2)]
            outb = [k.sb(es, "gc_out%d" % i, [128, T], BF16) for i in range(2)]
            k.dma(cw[:], self.gdn_convw)
            k.dma(blk[:], self.c_blk, q="pool")
            k.dma(gcol[:], self.c_gcols)
            lvl = getattr(self, "probe_conv", 9)
            for ch in range(6):
                xp = xpad[ch % 2]
                ob = outb[ch % 2]
                if lvl == 1 and ch > 0:
                    break
                k.memset(xp[:, 0:4], 0.0)
                k.dma(xp[:, 4:4 + T], self.projF[ch * 128:(ch + 1) * 128, :], q="pool")
                k.ts(acc[:], xp[:, 4:4 + T], cw[:, l, ch, 3:4], ALU.mult)
                for kk in (2, 1, 0):
                    k.stt(acc[:], xp[:, 1 + kk:1 + kk + T], cw[:, l, ch, kk:kk + 1], acc[:], ALU.mult, ALU.add)
                if lvl <= 2:
                    continue
                if ch >= 4:
                    k.act(ob[:], acc[:], AF.Silu)
                    k.dma(self.gvs[(ch - 4) * 128:(ch - 3) * 128, :], ob[:])
                else:
                    k.act(ys[:], acc[:], AF.Silu)
                    k.act(sq[:], ys[:], AF.Square)
                    if lvl <= 3:
                        continue
                    for tt in range(8):
                        sl = slice(tt * 512, (tt + 1) * 512)
                        pss = ps[tt % 2]
                        r = rin[tt % 2]
                        k.mm(pss[:], blk[:], sq[:, sl])
                        k.act(r[:], pss[:], AF.Ln, bias=gcol[:, 6:7])
                        k.act(r[:], r[:], AF.Exp, scale=-0.5)
                        if ch < 2:
                            k.stt(ob[:, sl], ys[:, sl], 0.125, r[:], ALU.mult, ALU.mult)
                        else:
                            k.tt(ob[:, sl], ys[:, sl], r[:], ALU.mult, eng="pool")
                    dst = self.gqn if ch < 2 else self.gkn
                    k.dma(dst[(ch % 2) * 128:(ch % 2 + 1) * 128, :], ob[:])
            k.barrier()

    def phase_gdn(self, l, s):
        self.phase_gdn_conv(l, s)
        if getattr(self, "gdn_conv_only", False):
            return
        nc, k = self.nc, self.k
        ps = self.ps
        psb = [p_[:].bitcast(BF16) for p_ in ps]
        TP = 1024
        NCH = TP // 64
        with ExitStack() as es:
            gcol = k.sb(es, "gd_gcol", [128, 8], F32)
            gmsk = k.sb(es, "gd_gmsk", [128, 4], F32)
            mreset = k.sb(es, "gd_mreset", [128, TP], F32)
            maskS = k.sb(es, "gd_maskS", [64, 256], F32)
            maskIT = k.sb(es, "gd_maskIT", [64, 256], F32)
            pA = k.sb(es, "gd_pA", [128, DEPTH], F32)
            pDt = k.sb(es, "gd_pDt", [128, DEPTH], F32)
            nA = k.sb(es, "gd_nA", [128, 1], F32)
            nw = k.sb(es, "gd_nw", [64, DEPTH, 64], F32)
            raw = k.sb(es, "gd_raw", [128, TP], F32)
            gg = k.sb(es, "gd_gg", [128, TP], F32)
            bc = k.sb(es, "gd_bc", [128, TP], F32)
            At = k.sb(es, "gd_A", [128, TP], F32)
            Bt = k.sb(es, "gd_B", [128, TP], F32)
            AtH = k.sb(es, "gd_AH", [2, 4, TP], F32)
            BtH = k.sb(es, "gd_BH", [2, 4, TP], F32)
            R1 = k.sb(es, "gd_R1", [128, TP], F32)
            R2 = k.sb(es, "gd_R2", [128, TP], F32)
            rhsd = k.sb(es, "gd_rhsd", [128, NCH, 4], F32)
            Gs = k.sb(es, "gd_Gs", [64, NCH, 4], F32)
            qn = k.sb(es, "gd_qn", [64, 4, TP], BF16)
            kn = k.sb(es, "gd_kn", [64, 4, TP], BF16)
            vv = k.sb(es, "gd_vv", [64, 4, TP], BF16)
            zT = k.sb(es, "gd_zT", [128, 2, TP], BF16)
            sz = k.sb(es, "gd_sz", [128, 2, 64], F32)
            tm1 = k.sb(es, "gd_tm1", [64, 128], F32)
            tm2 = k.sb(es, "gd_tm2", [64, 128], F32)
            smS = [k.sb(es, "gd_sm%d" % i, [64, 8, 4], F32) for i in range(2)]
            y32S = [k.sb(es, "gd_y32_%d" % i, [64, 4, 128], F32) for i in range(2)]
            ybS = [k.sb(es, "gd_yb_%d" % i, [64, 4, 128], BF16) for i in range(2)]
            kdecS = [k.sb(es, "gd_kdec_%d" % i, [64, 4, 64], BF16) for i in range(2)]
            attnTS = [k.sb(es, "gd_attnT_%d" % i, [64, 4, 64], BF16) for i in range(2)]
            NbS = [[k.sb(es, "gd_N%d_%d" % (j, i), [64, 4, 64], BF16) for i in range(2)] for j in range(2)]
            PbS = [[k.sb(es, "gd_P%d_%d" % (j, i), [64, 4, 64], BF16) for i in range(2)] for j in range(2)]
            kvtm = k.sb(es, "gd_kvtm", [64, 512], BF16)
            kdec = k.sb(es, "gd_kdec", [64, 4, 64], BF16)
            Ds = k.sb(es, "gd_Ds", [64, 256], F32)
            DTs = k.sb(es, "gd_DTs", [64, 256], F32)
            tmpN = k.sb(es, "gd_tmpN", [64, 256], F32)
            Nb = [k.sb(es, "gd_N%d" % i, [64, 4, 64], BF16) for i in range(2)]
            Pb = [k.sb(es, "gd_P%d" % i, [64, 4, 64], BF16) for i in range(2)]
            attnT = k.sb(es, "gd_attnT", [64, 4, 64], BF16)
            y32 = k.sb(es, "gd_y32", [64, 4, 128], F32)
            yb = k.sb(es, "gd_yb", [64, 4, 128], BF16)
            wT = k.sb(es, "gd_wT", [64, 4, 64], BF16)
            ub = k.sb(es, "gd_ub", [64, 4, 64], BF16)
            S32 = k.sb(es, "gd_S32", [64, 4, 64], F32)
            Sb = k.sb(es, "gd_Sb", [64, 4, 64], BF16)
            t1 = k.sb(es, "gd_t1", [64, 4, 64], F32)
            o32 = k.sb(es, "gd_o32", [64, 4, 64], F32)
            osq = k.sb(es, "gd_osq", [64, 4, 64], F32)
            ss = k.sb(es, "gd_ss", [64, 8], F32)
            on = k.sb(es, "gd_on", [64, 4, 64], BF16)
            oT = k.sb(es, "gd_oT", [128, 2, 512], BF16)
            k.dma(gcol[:], self.c_gcols)
            k.dma(gmsk[:], self.c_gmsk)
            k.dma(mreset[:], self.c_mreset)
            k.dma(maskS[:], self.c_gmaskS)
            k.dma(maskIT[:], self.c_gmaskIT)
            k.dma(pA[:], self.gdn_pA)
            k.dma(pDt[:], self.gdn_pDt)
            k.dma(nw[:], self.gdn_nw)
            k.act(nA[:], pA[:, l:l + 1], AF.Exp)
            k.ts(nA[:], nA[:], -1.0, ALU.mult)
            k.memset(S32[:], 0.0)
            k.memset(Sb[:], 0.0)
            id64 = self.ident[0:64, 0:64]
            idb64 = self.identb[0:64, 0:64]
            for pc in range(T // TP):
                p0 = pc * TP
                k.dma(raw[:], self.projS[:, p0:p0 + TP])
                k.dma(qn[:], self.gqn[:, p0:p0 + TP].rearrange("(h d) t -> d h t", d=64))
                k.dma(kn[:], self.gkn[:, p0:p0 + TP].rearrange("(h d) t -> d h t", d=64))
                k.dma(vv[:], self.gvs[:, p0:p0 + TP].rearrange("(h d) t -> d h t", d=64))
                k.dma(zT[:], self.projF[FM_GZ * 128:(FM_GZ + 2) * 128, p0:p0 + TP].rearrange("(c p) t -> p c t", p=128))
                k.act(gg[:], raw[:], AF.Exp, bias=pDt[:, l:l + 1])
                k.act(gg[:], gg[:], AF.Ln, bias=self.onec[:])
                k.ts(gg[:], gg[:], nA[:], ALU.mult)
                k.op("dve", lambda e: e.tensor_tensor_scan(out=bc[:], data0=mreset[:], data1=gg[:], initial=0.0,
                                                            op0=ALU.mult, op1=ALU.add), [mreset[:], gg[:]], [bc[:]])
                k.ts(At[:], bc[:], gcol[:, 0:1], ALU.mult, s2=gcol[:, 1:2], op1=ALU.add)
                k.ts(Bt[:], bc[:], gcol[:, 2:3], ALU.mult, s2=gcol[:, 3:4], op1=ALU.add)
                for h in range(4):
                    k.dma(AtH[:, h, :], At[32 * h:32 * h + 2, :])
                    k.dma(BtH[:, h, :], Bt[32 * h:32 * h + 2, :])
                k.act(raw[:], raw[:], AF.Sigmoid)
                k.ts(raw[:], raw[:], gcol[:, 4:5], ALU.mult)
                k.stt(R1[:], bc[:], gcol[:, 5:6], raw[:], ALU.mult, ALU.add)
                bl = bc[:].rearrange("p (n c) -> p n c", c=64)[:, :, 63:64]
                k.tt(R2[:].rearrange("p (n c) -> p n c", c=64), bl.broadcast_to([128, NCH, 64]),
                     bc[:].rearrange("p (n c) -> p n c", c=64), ALU.subtract)
                k.act(R2[:], R2[:], AF.Exp)
                k.tt(rhsd[:], bl.broadcast_to([128, NCH, 4]), gmsk[:].unsqueeze(1).broadcast_to([128, NCH, 4]), ALU.mult)
                k.mm(ps[7][0:64, 0:NCH * 4], self.ones[:, 0:64], rhsd[:].rearrange("p n h -> p (n h)"))
                k.act(Gs[:].rearrange("p n h -> p (n h)"), ps[7][0:64, 0:NCH * 4], AF.Exp)
                def chunk(n):
                    sl_ = n % 2
                    sm_, y32_, yb_, kdec_, attnT_ = smS[sl_], y32S[sl_], ybS[sl_], kdecS[sl_], attnTS[sl_]
                    Nb_, Pb_ = NbS[sl_], PbS[sl_]
                    c0 = n * 64
                    cs_ = slice(c0, c0 + 64)
                    gn = pc * NCH + n
                    k.tr(ps[0][0:64, 0:128], R1[:, cs_], self.ident[:])
                    k.tr(ps[0][0:64, 128:256], R2[:, cs_], self.ident[:])
                    k.copy(tm1[:], ps[0][0:64, 0:128], eng="act")
                    k.copy(tm2[:], ps[0][0:64, 128:256], eng="act")
                    tv1 = tm1[:].rearrange("p (h r) -> p h r", r=32)
                    tv2 = tm2[:].rearrange("p (h r) -> p h r", r=32)
                    b_tm = tv1[:, :, 0]
                    beta_tm = tv1[:, :, 2]
                    ekd_tm = tv2[:, :, 0]
                    nbeta, eb, beb = sm_[:, 0, :], sm_[:, 1, :], sm_[:, 2, :]
                    k.ts(nbeta, beta_tm, -1.0, ALU.mult)
                    k.act(eb, b_tm, AF.Exp)
                    k.tt(beb, eb, beta_tm, ALU.mult)
                    yield "prep"
                    pkv = psb[1]
                    for h in range(4):
                        k.tr(pkv[0:64, h * 64:(h + 1) * 64], kn[:, h, cs_], idb64)
                    for h in range(4):
                        k.tr(pkv[0:64, 256 + h * 64:256 + (h + 1) * 64], vv[:, h, cs_], idb64)
                    k.copy(kvtm[:], pkv[0:64, 0:512], eng="act")
                    kt3 = kvtm[:, 0:256].rearrange("p (h d) -> p h d", d=64)
                    vt3 = kvtm[:, 256:512].rearrange("p (h d) -> p h d", d=64)
                    k.tt(y32_[:, :, 0:64], vt3, beta_tm.unsqueeze(2).broadcast_to([64, 4, 64]), ALU.mult)
                    k.tt(y32_[:, :, 64:128], kt3, beb.unsqueeze(2).broadcast_to([64, 4, 64]), ALU.mult)
                    k.tt(kdec_[:], kt3, ekd_tm.unsqueeze(2).broadcast_to([64, 4, 64]), ALU.mult)
                    k.copy(yb_[:], y32_[:], eng="act")
                    yield "prep"
                    pD = ps[2]
                    k.mm(pD[0:64, 0:256], id64, maskS[:], start=True, stop=False)
                    def AB(h):
                        return AtH[0:2, h, cs_], BtH[0:2, h, cs_]
                    import os
                    hs_ = [int(c) for c in os.environ.get("GH", "0123")]
                    for h in hs_:
                        a_, b_ = AB(h)
                        k.mm(pD[0:64, h * 64:(h + 1) * 64], a_, b_, start=False, stop=True)
                    k.mm(pD[0:64, 256:512], id64, maskIT[:], start=True, stop=False)
                    for h in hs_:
                        a_, b_ = AB(h)
                        k.mm(pD[0:64, 256 + h * 64:256 + (h + 1) * 64], b_, a_, start=False, stop=True)
                    k.act(Ds[:], pD[0:64, 0:256], AF.Exp)
                    k.act(DTs[:], pD[0:64, 256:512], AF.Exp)
                    yield "prep"
                    pKK = ps[3]
                    for h in range(4):
                        k.mm(pKK[0:64, h * 64:(h + 1) * 64], kn[:, h, cs_], kn[:, h, cs_])
                    for h in range(4):
                        k.mm(pKK[0:64, 256 + h * 64:256 + (h + 1) * 64], kn[:, h, cs_], qn[:, h, cs_])
                    k.tt(tmpN[:], pKK[0:64, 0:256], Ds[:], ALU.mult)
                    N0, P0 = Nb_[0], Pb_[0]
                    k.tt(N0[:], tmpN[:].rearrange("p (h j) -> p h j", j=64), nbeta.unsqueeze(2).broadcast_to([64, 4, 64]), ALU.mult)
                    k.tt(attnT_[:].rearrange("p h i -> p (h i)"), pKK[0:64, 256:512], DTs[:], ALU.mult)
                    pP = psb[4]
                    for h in range(4):
                        k.tr(pP[0:64, h * 64:(h + 1) * 64], N0[:, h, :], idb64)
                    k.copy(P0[:].rearrange("p h i -> p (h i)"), pP[0:64, 0:256], eng="act")
                    yield "prepdone"
                    cur = 0
                    for lev in range(6):
                        Nc, Pc = Nb_[cur], Pb_[cur]
                        pY = ps[6]
                        for h in range(4):
                            k.mm(pY[0:64, h * 128:(h + 1) * 128], Pc[:, h, :], yb_[:, h, :])
                        if lev < 5:
                            Nn, Pn_ = Nb_[1 - cur], Pb_[1 - cur]
                            pN = ps[5]
                            for h in range(4):
                                k.mm(pN[0:64, h * 64:(h + 1) * 64], Pc[:, h, :], Nc[:, h, :])
                            for h in range(4):
                                k.mm(pN[0:64, 256 + h * 64:256 + (h + 1) * 64], Nc[:, h, :], Pc[:, h, :])
                        k.tt(y32_[:].rearrange("p h c -> p (h c)"), y32_[:].rearrange("p h c -> p (h c)"), pY[0:64, :], ALU.add)
                        k.copy(yb_[:], y32_[:], eng="act")
                        if lev < 5:
                            k.copy(Nn[:].rearrange("p h i -> p (h i)"), pN[0:64, 0:256], eng="act")
                            k.copy(Pn_[:].rearrange("p h i -> p (h i)"), pN[0:64, 256:512], eng="act")
                            cur = 1 - cur
                        yield "lev"
                    pW = psb[4]
                    for h in range(4):
                        k.tr(pW[0:64, 256 + h * 64:256 + (h + 1) * 64], yb_[:, h, 64:128], idb64)
                    k.copy(wT[:].rearrange("p h i -> p (h i)"), pW[0:64, 256:512], eng="act")
                    pU = ps[7]
                    for h in range(4):
                        k.mm(pU[0:64, h * 64:(h + 1) * 64], wT[:, h, :], Sb[:, h, :])
                    for h in range(4):
                        k.mm(pU[0:64, 256 + h * 64:256 + (h + 1) * 64], qn[:, h, cs_], Sb[:, h, :])
                    k.tt(ub[:], y32_[:, :, 0:64], pU[0:64, 0:256].rearrange("p (h d) -> p h d", d=64), ALU.subtract)
                    pAU = ps[4]
                    for h in range(4):
                        k.mm(pAU[0:64, 256 + h * 64:256 + (h + 1) * 64], attnT_[:, h, :], ub[:, h, :])
                    pSn = ps[1]
                    for h in range(4):
                        k.mm(pSn[0:64, 256 + h * 64:256 + (h + 1) * 64], kdec_[:, h, :], ub[:, h, :])
                    k.tt(t1[:], pU[0:64, 256:512].rearrange("p (h d) -> p h d", d=64), eb.unsqueeze(2).broadcast_to([64, 4, 64]), ALU.mult)
                    k.tt(o32[:].rearrange("p h d -> p (h d)"), t1[:].rearrange("p h d -> p (h d)"), pAU[0:64, 256:512], ALU.add)
                    k.tt(S32[:], S32[:], Gs[:, n, :].unsqueeze(2).broadcast_to([64, 4, 64]), ALU.mult, eng="pool")
                    k.tt(S32[:].rearrange("p h d -> p (h d)"), S32[:].rearrange("p h d -> p (h d)"), pSn[0:64, 256:512], ALU.add)
                    k.copy(Sb[:], S32[:], eng="pool")
                    k.act(osq[:], o32[:], AF.Square)
                    k.red(ss[:, 0:4], osq[:], ALU.add)
                    k.ts(ss[:, 0:4], ss[:, 0:4], 1.0 / 64.0, ALU.mult, s2=1.0e-6, op1=ALU.add)
                    k.act(ss[:, 0:4], ss[:, 0:4], AF.Sqrt)
                    k.recip(ss[:, 4:8], ss[:, 0:4])
                    k.tt(o32[:], o32[:], ss[:, 4:8].unsqueeze(2).broadcast_to([64, 4, 64]), ALU.mult)
                    k.tt(on[:], o32[:], nw[:, l, :].unsqueeze(1).broadcast_to([64, 4, 64]), ALU.mult)
                    pOT = psb[0]
                    for c in range(2):
                        k.tr(pOT[:, 512 + c * 64:512 + (c + 1) * 64], on[:, 2 * c:2 * c + 2, :].rearrange("p h d -> p (h d)"), idb64)
                    k.act(sz[:], zT[:, :, cs_], AF.Silu)
                    jj = gn % 8
                    for c in range(2):
                        k.tt(oT[:, c, jj * 64:(jj + 1) * 64], pOT[:, 512 + c * 64:512 + (c + 1) * 64], sz[:, c, :], ALU.mult)
                    if jj == 7:
                        t0 = (gn - 7) * 64
                        k.dma(self.obrT[0, :, t0:t0 + 512].rearrange("(c p) t -> p c t", p=128), oT[:])

                gens = [chunk(n) for n in range(NCH)]

                def run_until(g, tag):
                    for t_ in g:
                        if t_ == tag:
                            return True
                    return False

                run_until(gens[0], "prepdone")
                for n in range(NCH):
                    for lev in range(6):
                        next(gens[n])
                        if lev < 4 and n + 1 < NCH:
                            next(gens[n + 1])
                    for _ in gens[n]:
                        pass

            k.barrier()

    for f in (run_attn_loops, phase_proj, attn_finalize, phase_fox, phase_merge, phase_mixer, phase_ret, phase_nsa, phase_gdn, phase_gdn_conv):
        setattr(Prog, f.__name__, f)


_mixer_methods()

def host_consts():
    c = {}
    sel = np.zeros((16, NE * 128), np.float32)
    for e in range(NE):
        sel[e, e * 128:(e + 1) * 128] = 1.0
    c["c_sel16"] = sel
    c["c_ident"] = np.eye(128, dtype=np.float32)
    kk = np.arange(128)[:, None, None]
    oo = np.arange(4)[None, :, None]
    qq = np.arange(512)[None, None, :]
    c["c_cmask"] = np.where(oo * 128 + kk <= qq, 0.0, NEG).astype(np.float32)
    gc = np.zeros((128, 8), np.float32)
    gm = np.zeros((128, 4), np.float32)
    for h in range(4):
        gc[32 * h, 0] = 1.0; gc[32 * h + 1, 1] = 1.0; gc[32 * h + 1, 2] = -1.0; gc[32 * h, 3] = 1.0
        gc[32 * h + 2, 4] = 1.0; gc[32 * h, 5] = 1.0
        gm[32 * h, h] = 1.0
    gc[:, 6] = 1.0e-6
    c["c_gcols"] = gc
    c["c_gmsk"] = gm
    c["c_mreset"] = np.ascontiguousarray(np.broadcast_to((np.arange(1024) % 64 != 0).astype(np.float32)[None, :], (128, 1024)))
    ii = np.arange(64)
    mS = np.where(ii[:, None] > ii[None, :], 0.0, NEG).astype(np.float32)
    mIT = np.where(ii[None, :] >= ii[:, None], 0.0, NEG).astype(np.float32)
    c["c_gmaskS"] = np.ascontiguousarray(np.tile(mS, (1, 4)))
    c["c_gmaskIT"] = np.ascontiguousarray(np.tile(mIT, (1, 4)))
    blk = np.zeros((128, 128), np.float32); blk[0:64, 0:64] = 1.0; blk[64:128, 64:128] = 1.0
    c["c_blk"] = blk
    nl = np.arange(128)[:, None, None]
    di = np.arange(5)[None, :, None]
    c["c_cmpmask"] = np.where(16 * nl + 31 - qq <= 512 * di, 0.0, NEG).astype(np.float32)
    c["c_bmask"] = np.where(oo * 128 + kk > qq, 0.0, NEG).astype(np.float32)
    jj = np.arange(64)
    E2 = np.zeros((64, 32, 128), np.float32)
    for kt in range(32):
        for m_ in range(128):
            E2[2 * kt + m_ // 64, kt, m_] = -NEG
    c["c_E2"] = E2
    n_all = np.arange(256)
    ov = np.clip(np.minimum(n_all[:, None] * 16 + 32, jj[None, :] * 64 + 64) - np.maximum(n_all[:, None] * 16, jj[None, :] * 64), 0, 32) / 32.0
    ov[255:] = 0.0
    c["c_ovl"] = np.ascontiguousarray(ov.reshape(2, 128, 64).transpose(1, 0, 2)).astype(np.float32)
    tpos = (np.arange(32)[None, :] * 128 + np.arange(128)[:, None])
    cur = (tpos // 64)[:, :, None]
    jb = jj[None, None, :]
    forced = (jb == 0) | (jb == cur) | (jb == cur - 1)
    causal = jb <= cur
    c["c_selA"] = (causal & ~forced).astype(np.float32)
    c["c_selB"] = (1.0e4 * forced - 1.0 * ((~causal) & (~forced))).astype(np.float32)
    half = 32
    invf = (10000.0 ** (-np.arange(half, dtype=np.float32) / half)).astype(np.float32)
    c["c_invf"] = np.ascontiguousarray(np.broadcast_to(invf[None, :], (128, 32))).astype(np.float32)
    lg = np.log1p(-(2.0 ** (-5.0 - np.arange(4, dtype=np.float64))))
    idx = np.arange(128, dtype=np.float64)
    rel = idx[None, :] - idx[:, None]
    dec = np.where(rel[None] >= 0, np.exp(np.maximum(rel[None], 0.0) * lg[:, None, None]), 0.0)
    c["c_decT"] = np.ascontiguousarray(dec.transpose(1, 0, 2)).astype(np.float32)
    xi = np.exp((idx[None, :] + 1.0) * lg[:, None])
    c["c_xiT"] = np.ascontiguousarray(np.broadcast_to(xi[None], (64, 4, 128))).astype(np.float32)
    zeta = np.exp((127.0 - idx[None, :]) * lg[:, None]) / 8.0
    c["c_zt"] = np.ascontiguousarray(zeta.T).astype(np.float32)
    cd = np.exp(128.0 * lg)
    c["c_cd"] = np.ascontiguousarray(np.broadcast_to(cd[None, :, None], (64, 4, 64))).astype(np.float32)
    return c


_O = dict(gq=0, gk=256, gv=512, ga=768, gb=772, gz=776, rq=1032, rk=1288, rv=1544, rg=1800, nq=2056, nkc=2312,
          nvc=2376, nks=2440, nvs=2504, nkw=2568, nvw=2632, ngate=2696, fq=2708, fk=2964, fv=3220, ff=3476)


def permute_w_in(w_in):
    out = np.zeros((w_in.shape[0], D, WIN_COLS), np.float32)

    def put(dst, name, width):
        out[:, :, dst:dst + width] = w_in[:, :, _O[name]:_O[name] + width]

    put(FM_GQ * 128, "gq", 256); put(FM_GK * 128, "gk", 256); put(FM_GV * 128, "gv", 256); put(FM_GZ * 128, "gz", 256)
    put(FM_RG * 128, "rg", 256); put(FM_NQ * 128, "nq", 256); put(FM_FQ * 128, "fq", 256); put(FM_FK * 128, "fk", 256)
    put(FM_NC * 128, "nkc", 64); put(FM_NC * 128 + 64, "nvc", 64)
    put(FM_NK * 128, "nks", 64); put(FM_NK * 128 + 64, "nkw", 64)
    for h in range(4):
        for r, nm in ((0, "ga"), (1, "ga"), (2, "gb")):
            out[:, :, FM_SM * 128 + 32 * h + r] = w_in[:, :, _O[nm] + h]
    put(FM_SM * 128 + 8, "ngate", 12); put(FM_SM * 128 + 20, "ff", 4)
    b = NFM * 128
    put(b + TM_RQ, "rq", 256); put(b + TM_RK, "rk", 256); put(b + TM_RV, "rv", 256); put(b + TM_FV, "fv", 256)
    put(b + TM_NVS, "nvs", 64); put(b + TM_NVW, "nvw", 64)
    return out


def pcol(v):
    v = np.asarray(v)
    sh = v.shape[:-1]
    n = v.shape[-1] // 128
    v = v.reshape(sh + (n, 128))
    v = np.moveaxis(v, -1, 0)
    return np.ascontiguousarray(v)


def prep_core(inp, seqs):
    S = len(seqs)
    m = {}
    m["xT"] = np.ascontiguousarray(np.transpose(inp["x"][seqs], (0, 2, 1)))
    m["cT"] = np.ascontiguousarray(np.transpose(pcol(inp["c"][seqs]), (0, 2, 1)))
    m["ada_w"] = np.ascontiguousarray(inp["ada_w"])
    m["ada_b"] = pcol(inp["ada_b"].reshape(DEPTH, 2, 3, D)).reshape(128, -1)
    m["ln_g"] = pcol(inp["ln_g"]).reshape(128, -1)
    m["ln_b"] = pcol(inp["ln_b"]).reshape(128, -1)
    m["router_w"] = np.ascontiguousarray(inp["router_w"].reshape(8, 128, NE).transpose(1, 0, 2))
    m["router_b"] = np.ascontiguousarray(inp["router_b"].reshape(1, NE))
    for n in ("exp_w1", "exp_w3", "exp_w2", "w_gate", "branch_proj", "w_out"):
        m[n] = np.ascontiguousarray(inp[n])
    m["w_in"] = permute_w_in(inp["w_in"])
    m["fox_fb"] = np.ascontiguousarray(inp["fox_f_bias"].T)
    m["gdn_convw"] = np.ascontiguousarray(inp["gdn_conv_w"].reshape(DEPTH, 4, 6, 128).transpose(3, 0, 2, 1))
    pA = np.zeros((128, DEPTH), np.float32); pDt = np.zeros((128, DEPTH), np.float32)
    for h in range(4):
        for r in (0, 1):
            pA[32 * h + r, :] = inp["gdn_a_log"][:, h]
            pDt[32 * h + r, :] = inp["gdn_dt_bias"][:, h]
    m["gdn_pA"] = pA
    m["gdn_pDt"] = pDt
    m["gdn_nw"] = np.ascontiguousarray(np.broadcast_to(inp["gdn_norm_w"][None], (64, DEPTH, 64))).astype(np.float32)
    pe = np.transpose(inp["nsa_cmp_pe"], (2, 0, 1))
    m["nsa_pe"] = np.ascontiguousarray(np.concatenate([pe, pe], axis=0))
    for n in ("nsa_ck_w1", "nsa_cv_w1", "nsa_ck_w2", "nsa_cv_w2"):
        m[n] = np.ascontiguousarray(inp[n])
    m["ret_gnw"] = pcol(inp["ret_gn_w"]).reshape(128, -1)
    m["posT"] = np.ascontiguousarray(inp["positions"][seqs].reshape(S, 32, 128).transpose(0, 2, 1)).astype(np.int32)
    return m


_CACHE = {}


def kernel(**inputs):
    inp = {k_: np.asarray(v) for k_, v in inputs.items()}
    ncores = 8
    S = inp["x"].shape[0] // ncores
    if "prog" not in _CACHE:
        p = Prog(nseq=S)
        p.build()
        _CACHE["prog"] = p
    p = _CACHE["prog"]
    consts = host_consts()
    in_maps = []
    for c in range(ncores):
        m = prep_core(inp, list(range(c * S, (c + 1) * S)))
        m.update(consts)
        in_maps.append({n: m[n] for n in p.inputs})
    res = run_bass_kernel_spmd(p.nc, in_maps, core_ids=list(range(ncores)))
    outs = [np.transpose(r["outT"], (0, 2, 1)) for r in res.results]
    return np.ascontiguousarray(np.concatenate(outs, axis=0)).astype(np.float32)
```
